# Optimizing a Trainium2 kernel written in Bass

```python
import math
import jax, jax.numpy as jnp
from jax import lax
import numpy as np

D_MODEL = 1024
BATCH = 16
SEQ = 2048
DEPTH = 1

EPS = 1e-6
HG_HEADS = 8
HG_KDIM = 128
HG_VDIM = 128
HG_CHUNK = 64
HG_WIDTH_K = HG_HEADS * HG_KDIM
HG_WIDTH_V = HG_HEADS * HG_VDIM
MLA_HEADS = 8
MLA_Q_RANK = 384
MLA_KV_RANK = 256
MLA_NOPE = 128
MLA_ROPE = 64
MLA_VDIM = 128
MLA_QK_DIM = MLA_NOPE + MLA_ROPE
ROPE_THETA = 10000.0
Q_BLOCK = 128
N_BRANCH = 2
IN_SPLITS = (HG_WIDTH_K, HG_WIDTH_K, HG_WIDTH_V, HG_WIDTH_V, MLA_Q_RANK, MLA_KV_RANK, MLA_ROPE, D_MODEL, D_MODEL)
IN_COLS = sum(IN_SPLITS)
N_GROUPS = 8
EXPERTS_PER_GROUP = 8
N_EXPERTS = N_GROUPS * EXPERTS_PER_GROUP
TOP_K_IN_GROUP = 2
EXPERT_FF = 512
MOE_BLOCK = 128

kernel_name = "hybrid_hgrn2_mla_hmoe_block"


def rmsnorm(x, w):
    xf = x.astype(jnp.float32)
    y = xf * lax.rsqrt(jnp.mean(xf * xf, axis=-1, keepdims=True) + EPS)
    return (y * w.astype(jnp.float32)).astype(x.dtype)


def apply_rope(x, cos, sin):
    x1, x2 = jnp.split(x.astype(jnp.float32), 2, axis=-1)
    return jnp.concatenate([x1 * cos - x2 * sin, x2 * cos + x1 * sin], axis=-1).astype(x.dtype)


def hgrn2_mixer(q_raw, f_raw, i_raw, g_raw, lb, out_norm_w):
    B, S, _ = q_raw.shape
    n = S // HG_CHUNK
    f32 = jnp.float32
    q = jax.nn.silu(q_raw.astype(f32)) * (HG_KDIM ** -0.5)
    lbf = lb.astype(f32)
    fg = lbf + (1.0 - lbf) * jax.nn.sigmoid(f_raw.astype(f32))
    k = 1.0 - fg
    logf = jnp.log(fg)
    v = i_raw.astype(f32)

    def to_chunks(t, d):
        return t.reshape(B, n, HG_CHUNK, HG_HEADS, d).transpose(1, 0, 3, 2, 4)

    qc, kc, lfc = to_chunks(q, HG_KDIM), to_chunks(k, HG_KDIM), to_chunks(logf, HG_KDIM)
    vc = to_chunks(v, HG_VDIM)
    causal = jnp.tril(jnp.ones((HG_CHUNK, HG_CHUNK), dtype=bool))

    def step(state, inp):
        qt, kt, lft, vt = inp
        bcum = jnp.cumsum(lft, axis=2)
        diff = bcum[:, :, :, None, :] - bcum[:, :, None, :, :]
        decay = jnp.exp(jnp.where(causal[None, None, :, :, None], diff, -jnp.inf))
        attn = jnp.sum(qt[:, :, :, None, :] * kt[:, :, None, :, :] * decay, axis=-1)
        o = jnp.einsum('bhts,bhsv->bhtv', attn, vt) + \
            jnp.einsum('bhtk,bhkv->bhtv', qt * jnp.exp(bcum), state)
        b_last = bcum[:, :, -1]
        k_dec = kt * jnp.exp(b_last[:, :, None, :] - bcum)
        state = jnp.exp(b_last)[..., None] * state + jnp.einsum('bhsk,bhsv->bhkv', k_dec, vt)
        return state, o

    s0 = jnp.zeros((B, HG_HEADS, HG_KDIM, HG_VDIM), f32)
    _, o = lax.scan(step, s0, (qc, kc, lfc, vc))
    o = o.transpose(1, 0, 3, 2, 4).reshape(B, S, HG_HEADS, HG_VDIM)
    o = rmsnorm(o, out_norm_w).reshape(B, S, HG_WIDTH_V)
    g = g_raw.astype(f32)
    return o * jax.nn.silu(g)


def mla_mixer(c_q, c_kv, k_rope_raw, positions, q_norm_w, w_uq, kv_norm_w, w_ukv):
    B, S, _ = c_q.shape
    half = MLA_ROPE // 2
    inv_freq = ROPE_THETA ** (-jnp.arange(half, dtype=jnp.float32) / half)
    ang = positions.astype(jnp.float32)[..., None] * inv_freq
    cos, sin = jnp.cos(ang), jnp.sin(ang)

    q = (rmsnorm(c_q, q_norm_w) @ w_uq).reshape(B, S, MLA_HEADS, MLA_QK_DIM)
    q_nope, q_pe = q[..., :MLA_NOPE], q[..., MLA_NOPE:]
    q_pe = apply_rope(q_pe, cos[:, :, None, :], sin[:, :, None, :])
    kv = (rmsnorm(c_kv, kv_norm_w) @ w_ukv).reshape(B, S, MLA_HEADS, MLA_NOPE + MLA_VDIM)
    k_nope, v = kv[..., :MLA_NOPE], kv[..., MLA_NOPE:]
    k_pe = apply_rope(k_rope_raw, cos, sin)
    k = jnp.concatenate([k_nope, jnp.broadcast_to(k_pe[:, :, None, :], (B, S, MLA_HEADS, MLA_ROPE))], axis=-1)
    qf = jnp.concatenate([q_nope, q_pe], axis=-1) * (MLA_QK_DIM ** -0.5)

    nq = S // Q_BLOCK
    qb = qf.reshape(B, nq, Q_BLOCK, MLA_HEADS, MLA_QK_DIM).transpose(1, 0, 3, 2, 4)
    kpos = jnp.arange(S)

    def attend(args):
        qblk, blk = args
        s = jnp.einsum('bhqd,bkhd->bhqk', qblk, k, preferred_element_type=jnp.float32)
        qpos = blk * Q_BLOCK + jnp.arange(Q_BLOCK)
        s = jnp.where(kpos[None, :] <= qpos[:, None], s, -jnp.inf)
        p = jax.nn.softmax(s, axis=-1)
        return jnp.einsum('bhqk,bkhd->bqhd', p.astype(v.dtype), v)

    o = lax.map(attend, (qb, jnp.arange(nq)))
    return o.transpose(1, 0, 2, 3, 4).reshape(B, S, MLA_HEADS * MLA_VDIM)


def hierarchical_moe(h, w_group, b_group, w_expert, b_expert, w1, w3, w2):
    B, S, D = h.shape
    T = B * S
    xt = h.reshape(T, D)
    g_prob = jax.nn.softmax((xt @ w_group).astype(jnp.float32) + b_group.astype(jnp.float32), axis=-1)
    g_w, g_idx = lax.top_k(g_prob, 1)
    e_logits = ((xt @ w_expert).astype(jnp.float32) + b_expert.astype(jnp.float32)).reshape(T, N_GROUPS, EXPERTS_PER_GROUP)
    e_sel = jnp.take_along_axis(e_logits, g_idx[:, :, None], axis=1)[:, 0]
    e_prob = jax.nn.softmax(e_sel, axis=-1)
    e_w, e_idx = lax.top_k(e_prob, TOP_K_IN_GROUP)
    e_w = e_w / jnp.sum(e_w, axis=-1, keepdims=True)
    weights = g_w * e_w
    expert_ids = g_idx * EXPERTS_PER_GROUP + e_idx

    A = T * TOP_K_IN_GROUP
    flat_e = expert_ids.reshape(A)
    flat_t = jnp.repeat(jnp.arange(T, dtype=jnp.int32), TOP_K_IN_GROUP)
    flat_w = weights.reshape(A)
    order = jnp.argsort(flat_e)
    se, st, sw = flat_e[order], flat_t[order], flat_w[order]
    counts = jax.ops.segment_sum(jnp.ones_like(se), se, num_segments=N_EXPERTS)
    starts = jnp.cumsum(counts) - counts
    padded = (counts + MOE_BLOCK - 1) // MOE_BLOCK * MOE_BLOCK
    pends = jnp.cumsum(padded)
    pstarts = pends - padded
    dest = pstarts[se] + (jnp.arange(A, dtype=se.dtype) - starts[se])
    n_blocks = A // MOE_BLOCK + N_EXPERTS
    cap = n_blocks * MOE_BLOCK
    tok_buf = jnp.full((cap,), T, dtype=jnp.int32).at[dest].set(st)
    w_buf = jnp.zeros((cap,), jnp.float32).at[dest].set(sw)
    block_start = jnp.arange(n_blocks, dtype=pends.dtype) * MOE_BLOCK
    block_expert = jnp.minimum(jnp.searchsorted(pends, block_start, side='right'), N_EXPERTS - 1)
    x_pad = jnp.concatenate([xt, jnp.zeros((1, D), xt.dtype)], axis=0)

    def run_block(args):
        toks, wts, e = args
        xb = x_pad[toks]
        hid = jax.nn.silu(xb @ w1[e]) * (xb @ w3[e])
        return (hid @ w2[e]) * wts[:, None].astype(xb.dtype)

    yb = lax.map(run_block, (tok_buf.reshape(n_blocks, MOE_BLOCK), w_buf.reshape(n_blocks, MOE_BLOCK), block_expert))
    out = jax.ops.segment_sum(yb.reshape(cap, D), tok_buf, num_segments=T + 1)[:T]
    return out.reshape(B, S, D)


def setup_inputs(seed: int = 0) -> dict:
    key = jax.random.key(seed)
    ks = jax.random.split(key, 24)
    f32 = jnp.float32
    nrm = lambda k, shape, scale: jax.random.normal(k, shape, f32) * scale
    gain = lambda k, shape: 1.0 + 0.02 * jax.random.normal(k, shape, f32)
    x = jax.random.normal(ks[0], (BATCH, SEQ, D_MODEL), f32)
    offsets = jax.random.randint(ks[1], (BATCH, 1), 0, 4096, dtype=jnp.int32)
    positions = offsets + jnp.arange(SEQ, dtype=jnp.int32)[None, :]
    return {
        "x": x,
        "positions": positions,
        "attn_norm_w": gain(ks[2], (DEPTH, D_MODEL)),
        "w_in": nrm(ks[3], (DEPTH, D_MODEL, IN_COLS), D_MODEL ** -0.5),
        "hg_lower_bound": nrm(ks[4], (DEPTH + 1, HG_WIDTH_K), 0.5),
        "hg_out_norm_w": gain(ks[5], (DEPTH, HG_VDIM)),
        "mla_q_norm_w": gain(ks[6], (DEPTH, MLA_Q_RANK)),
        "mla_w_uq": nrm(ks[7], (DEPTH, MLA_Q_RANK, MLA_HEADS * MLA_QK_DIM), MLA_Q_RANK ** -0.5),
        "mla_kv_norm_w": gain(ks[8], (DEPTH, MLA_KV_RANK)),
        "mla_w_ukv": nrm(ks[9], (DEPTH, MLA_KV_RANK, MLA_HEADS * (MLA_NOPE + MLA_VDIM)), MLA_KV_RANK ** -0.5),
        "w_branch_hgrn": nrm(ks[10], (DEPTH, HG_WIDTH_V, D_MODEL), HG_WIDTH_V ** -0.5),
        "w_branch_mla": nrm(ks[11], (DEPTH, MLA_HEADS * MLA_VDIM, D_MODEL), (MLA_HEADS * MLA_VDIM) ** -0.5),
        "w_out": nrm(ks[12], (DEPTH, D_MODEL, D_MODEL), D_MODEL ** -0.5),
        "ffn_norm_w": gain(ks[13], (DEPTH, D_MODEL)),
        "router_group_w": nrm(ks[14], (DEPTH, D_MODEL, N_GROUPS), D_MODEL ** -0.5),
        "router_group_b": nrm(ks[15], (DEPTH, N_GROUPS), 0.01),
        "router_expert_w": nrm(ks[16], (DEPTH, D_MODEL, N_EXPERTS), D_MODEL ** -0.5),
        "router_expert_b": nrm(ks[17], (DEPTH, N_EXPERTS), 0.01),
        "expert_w1": nrm(ks[18], (DEPTH, N_EXPERTS, D_MODEL, EXPERT_FF), D_MODEL ** -0.5),
        "expert_w3": nrm(ks[19], (DEPTH, N_EXPERTS, D_MODEL, EXPERT_FF), D_MODEL ** -0.5),
        "expert_w2": nrm(ks[20], (DEPTH, N_EXPERTS, EXPERT_FF, D_MODEL), EXPERT_FF ** -0.5),
        "final_norm_w": gain(ks[21], (D_MODEL,)),
    }


def reference(x, positions, attn_norm_w, w_in, hg_lower_bound, hg_out_norm_w, mla_q_norm_w, mla_w_uq,
              mla_kv_norm_w, mla_w_ukv, w_branch_hgrn, w_branch_mla, w_out, ffn_norm_w, router_group_w,
              router_group_b, router_expert_w, router_expert_b, expert_w1, expert_w3, expert_w2, final_norm_w):
    split_idx = [int(v) for v in np.cumsum(IN_SPLITS)[:-1]]
    lb_all = jnp.cumsum(jax.nn.softmax(hg_lower_bound.astype(jnp.float32), axis=0), axis=0)
    for l in range(DEPTH):
        h = rmsnorm(x, attn_norm_w[l])
        z = h @ w_in[l]
        hq, hf, hi, hg, cq, ckv, kr, gate_a, gate_b = jnp.split(z, split_idx, axis=-1)
        o_a = hgrn2_mixer(hq, hf, hi, hg, lb_all[l], hg_out_norm_w[l]).astype(x.dtype)
        o_b = mla_mixer(cq, ckv, kr, positions, mla_q_norm_w[l], mla_w_uq[l], mla_kv_norm_w[l], mla_w_ukv[l])
        y = jax.nn.sigmoid(gate_a) * (o_a @ w_branch_hgrn[l]) + jax.nn.sigmoid(gate_b) * (o_b @ w_branch_mla[l])
        x = x + y @ w_out[l]
        h = rmsnorm(x, ffn_norm_w[l])
        x = x + hierarchical_moe(h, router_group_w[l], router_group_b[l], router_expert_w[l], router_expert_b[l],
                                 expert_w1[l], expert_w3[l], expert_w2[l])
    return rmsnorm(x, final_norm_w)
```

```python
import math
import contextlib
import numpy as np
import concourse.bass as bass
import concourse.mybir as mybir
from concourse.bass_utils import run_bass_kernel_spmd

F32 = mybir.dt.float32
BF16 = mybir.dt.bfloat16
I32 = mybir.dt.int32
U32 = mybir.dt.uint32
AF = mybir.ActivationFunctionType
ALU = mybir.AluOpType

NCORES = 8
T = 4096
SEQ = 2048
D = 1024
EPS = 1e-6
CAP = 256
NEXP = 64
NROWS = NEXP * CAP
SAME_ENG_SYNC = True
N_DMA_SEMS = 40
SCRATCH_INTERNAL = True

C_IDENT = 0
C_TRILE = 128
C_TRIST = 256
C_MASK256 = 384
C_RESET = 640
C_IOTA = 1152
C_INVF = 1216
C_SGN = 1217
C_ONES = 1218
C_AMASK = 1346
NCONST = C_AMASK + 2048


def make_consts():
    c = np.zeros((128, NCONST), np.float32)
    p = np.arange(128)[:, None]
    f = np.arange(128)[None, :]
    c[:, C_IDENT:C_IDENT + 128] = (p == f)
    c[:, C_TRILE:C_TRILE + 128] = (p <= f)
    c[:, C_TRIST:C_TRIST + 128] = (p < f)
    f256 = np.arange(256)[None, :]
    c[:, C_MASK256:C_MASK256 + 256] = ((p % 64) <= (f256 % 64))
    f512 = np.arange(512)[None, :]
    c[:, C_RESET:C_RESET + 512] = ((f512 % 64) != 0)
    c[:, C_IOTA:C_IOTA + 64] = np.arange(64)[None, :]
    half = 32
    inv_freq = (10000.0 ** (-np.arange(half, dtype=np.float32) / half)).astype(np.float32)
    c[:, C_INVF] = inv_freq[np.arange(128) % 32]
    c[:, C_SGN] = np.where((np.arange(128) % 64) < 32, -1.0, 1.0)
    c[:, C_ONES:C_ONES + 128] = 1.0
    for r in range(4):
        c[:, C_AMASK + r * 512:C_AMASK + (r + 1) * 512] = ((p + r * 128) <= f512)
    return c


class Cell:
    __slots__ = ("name", "w", "r")

    def __init__(self, name):
        self.name = name
        self.w = None
        self.r = []


class TB:
    def __init__(self, t, c):
        self.t = t
        self.c = c


class Emitter:
    def __init__(self, nc, es):
        self.nc = nc
        self.es = es
        self.eng = {"pe": nc.tensor, "act": nc.scalar, "dve": nc.vector, "pool": nc.gpsimd, "sp": nc.sync}
        self.ops = []
        self.last = {}
        self.dmas_since = []

    def cell(self, name="c"):
        return Cell(name)

    def op(self, eng, fn, reads=(), writes=(), kind="c"):
        oid = len(self.ops)
        deps = set()
        for c in reads:
            if c.w is not None:
                deps.add(c.w)
        for c in writes:
            if c.w is not None:
                deps.add(c.w)
            deps.update(c.r)
        self.ops.append(dict(eng=eng, fn=fn, deps=deps, kind=kind, sig=False))
        for c in reads:
            if kind == "c":
                c.r = [q for q in c.r if not (self.ops[q]["kind"] == "c" and self.ops[q]["eng"] == eng)]
            c.r.append(oid)
        for c in writes:
            c.w = oid
            c.r = []
        if kind == "d":
            self.dmas_since.append(oid)
        else:
            self.last[eng] = oid
        return oid

    def barrier(self):
        deps = set(self.last.values()) | set(self.dmas_since)
        for e in self.eng:
            self.ops.append(dict(eng=e, fn=None, deps=set(deps), kind="b", sig=False))
        self.dmas_since = []

    def emit(self):
        nc = self.nc
        ops = self.ops

        def skip(po, o):
            if po["kind"] != "c" or o["kind"] != "c":
                return False
            if po["eng"] == "pe" and o["eng"] == "pe":
                return True
            if (not SAME_ENG_SYNC) and po["eng"] == o["eng"]:
                return True
            return False

        for o in ops:
            for d in o["deps"]:
                po = ops[d]
                if po["kind"] == "c" and not skip(po, o):
                    po["sig"] = True
        sems = {e: self.es.enter_context(nc.semaphore("s_" + e)) for e in self.eng}
        dpool = {}
        for q in ("sp", "pool", "act"):
            n = N_DMA_SEMS if q != "act" else 4
            dpool[q] = dict(sems=[self.es.enter_context(nc.semaphore(f"s_d{q}{i}")) for i in range(n)],
                            cnt=[0] * n, last=[None] * n, nxt=0, n=n)
        cnt = {e: 0 for e in self.eng}
        waited = {e: {} for e in self.eng}
        tok = [None] * len(ops)
        nw = 0

        def wait(e, key, sem, val):
            nonlocal nw
            if waited[e].get(key, 0) >= val:
                return
            self.eng[e].wait_ge(sem, val)
            waited[e][key] = val
            nw += 1

        for oid, o in enumerate(ops):
            e = o["eng"]
            for d in sorted(o["deps"]):
                po = ops[d]
                if skip(po, o):
                    continue
                if tok[d] is None:
                    continue
                key, sem, val = tok[d]
                wait(e, key, sem, val)
            if o["kind"] == "b":
                continue
            if o["kind"] == "d":
                P = dpool[e]
                i = P["nxt"]
                P["nxt"] = (i + 1) % P["n"]
                if P["last"][i] is not None:
                    wait(e, ("d", e, i), P["sems"][i], P["last"][i])
                inst = o["fn"](self.eng[e])
                P["cnt"][i] += 16
                inst.then_inc(P["sems"][i], 16)
                P["last"][i] = P["cnt"][i]
                tok[oid] = (("d", e, i), P["sems"][i], P["cnt"][i])
            else:
                inst = o["fn"](self.eng[e])
                if o["sig"]:
                    cnt[e] += 1
                    inst.then_inc(sems[e], 1)
                    tok[oid] = (e, sems[e], cnt[e])
        for q, P in dpool.items():
            for i in range(P["n"]):
                if P["last"][i] is not None:
                    wait("sp", ("d", q, i), P["sems"][i], P["last"][i])
        return dict(n_ops=len(ops), n_waits=nw, cnt=cnt)


def build(stage_limit=99, dbg=False):
    nc = bass.Bass("TRN2", target_bir_lowering=False)

    def DI(name, shape, dt):
        return nc.dram_tensor(name, list(shape), dt, kind="ExternalInput").ap()

    def DS(name, shape, dt):
        kind = "ExternalOutput" if (dbg or not SCRATCH_INTERNAL) else "Internal"
        return nc.dram_tensor(name, list(shape), dt, kind=kind).ap()

    x = DI("x", [T, D], F32)
    pos = DI("pos", [1, T], I32)
    cst = DI("cst", [128, NCONST], F32)
    w_in = DI("w_in", [D, 6848], F32)
    w_krsw = DI("w_krsw", [D, 64], F32)
    g_attn = DI("g_attn", [1, D], F32)
    g_ffn = DI("g_ffn", [1, D], F32)
    g_fin = DI("g_fin", [1, D], F32)
    lbT = DI("lbT", [128, 16], F32)
    g_hg = DI("g_hg", [128, 1], F32)
    g_q = DI("g_q", [128, 3], F32)
    g_kv = DI("g_kv", [128, 2], F32)
    w_uq = DI("w_uq", [384, 1536], F32)
    w_uqsw = DI("w_uqsw", [384, 512], F32)
    w_ukv = DI("w_ukv", [256, 2048], F32)
    w_bh = DI("w_bh", [D, D], F32)
    w_bm = DI("w_bm", [D, D], F32)
    w_out = DI("w_out", [D, D], F32)
    w_r = DI("w_r", [D, 72], F32)
    b_r = DI("b_r", [1, 72], F32)
    ew1 = DI("ew1", [NEXP, D, 512], F32)
    ew3 = DI("ew3", [NEXP, D, 512], F32)
    ew2 = DI("ew2", [NEXP, 512, D], F32)
    out = nc.dram_tensor("out", [T, D], F32, kind="ExternalOutput").ap()

    oaT = DS("oaT", [D, T], BF16)
    obT = DS("obT", [D, T], BF16)
    x1d = DS("x1d", [T, D], F32)
    xg = DS("xg", [NROWS + 128, D], BF16)
    yb = DS("yb", [NROWS + 128, D], F32)

    w_in_v = w_in.rearrange("(c p) n -> p c n", p=128)

    with contextlib.ExitStack() as es:
        em = Emitter(nc, es)

        def sbt(st, name, shape, dt):
            t = st.enter_context(nc.sbuf_tensor(name, list(shape), dt))
            return TB(t, em.cell(name))

        def ACT(out_, in_, func, r, w, **kw):
            em.op("act", lambda e: e.activation(out=out_, in_=in_, func=func, **kw), r, w)

        def TT(eng, out_, in0, in1, op, r, w):
            em.op(eng, lambda e: e.tensor_tensor(out=out_, in0=in0, in1=in1, op=op), r, w)

        def TS(eng, out_, in0, s1, s2, op0, op1, r, w):
            if op1 is None:
                em.op(eng, lambda e: e.tensor_scalar(out=out_, in0=in0, scalar1=s1, scalar2=None, op0=op0), r, w)
            else:
                em.op(eng, lambda e: e.tensor_scalar(out=out_, in0=in0, scalar1=s1, scalar2=s2, op0=op0, op1=op1), r, w)

        def STT(out_, in0, scalar, in1, op0, op1, r, w):
            em.op("dve", lambda e: e.scalar_tensor_tensor(out=out_, in0=in0, scalar=scalar, in1=in1, op0=op0, op1=op1), r, w)

        def MM(out_, lhsT, rhs, start, stop, r, w):
            em.op("pe", lambda e: e.matmul(out_, lhsT, rhs, start=start, stop=stop), r, w)

        def TR(out_, in_, ident, r, w):
            em.op("pe", lambda e: e.transpose(out_, in_, ident), r, w)

        def CP(eng, out_, in_, r, w):
            if eng == "act":
                em.op("act", lambda e: e.activation(out=out_, in_=in_, func=AF.Copy), r, w)
            else:
                em.op(eng, lambda e: e.tensor_copy(out=out_, in_=in_), r, w)

        def MSET(eng, ap, val, w):
            em.op(eng, lambda e: e.memset(ap, val), [], w)

        def DMA(q, out_, in_, r, w):
            em.op(q, lambda e: e.dma_start(out=out_, in_=in_), r, w, kind="d")

        PS = []
        for i in range(8):
            t = es.enter_context(nc.psum_tensor(f"ps{i}", [128, 512], F32))
            PS.append(TB(t, em.cell(f"ps{i}")))
        rot = [0]

        def nb(n=6):
            b = PS[rot[0] % n]
            rot[0] += 1
            return b

        xnT = sbt(es, "xnT", [128, 8, T], BF16)
        xnT_c = [em.cell(f"xnT{b}") for b in range(T // 512)]
        cb = sbt(es, "cb", [128, 1346 + 2048], BF16)
        cf = sbt(es, "cf", [128, 1346], F32)
        epsT = sbt(es, "epsT", [128, 1], F32)
        dst = sbt(es, "dst", [128, 64], U32)
        wts = sbt(es, "wts", [128, 64], F32)

        DMA("pool", cb.t[:, :], cst[:, :], [], [cb.c])
        DMA("sp", cf.t[:, :], cst[:, 0:1346], [], [cf.c])
        MSET("dve", epsT.t[:, :], EPS, [epsT.c])
        ident_bf = cb.t[:, C_IDENT:C_IDENT + 128]
        ones_bf = cb.t[:, C_ONES:C_ONES + 128]

        def rstd_from(ssq_ap, npart, ncol, scale, tmp, outt, reads):
            ACT(tmp.t[0:npart, 0:ncol], ssq_ap, AF.Ln, reads + [epsT.c], [tmp.c], scale=scale, bias=epsT.t[0:npart, 0:1])
            ACT(outt.t[0:npart, 0:ncol], tmp.t[0:npart, 0:ncol], AF.Exp, [tmp.c], [outt.c], scale=-0.5)

        with contextlib.ExitStack() as st:
            xts = [sbt(st, f"xt{i}", [128, D], F32) for i in range(2)]
            xns = [sbt(st, f"xn{i}", [128, D], BF16) for i in range(2)]
            junk = sbt(st, "junk", [128, D], BF16)
            gA = sbt(st, "gA", [128, D], F32)
            ssq = sbt(st, "ssq", [128, 2], F32)
            lnv = sbt(st, "lnv", [128, 2], F32)
            rstd = sbt(st, "rstd", [128, 2], F32)
            DMA("sp", gA.t[:, :], g_attn[0:1, :].to_broadcast([128, D]), [], [gA.c])
            for i in range(T // 128):
                xt = xts[i % 2]
                xn = xns[i % 2]
                DMA("sp", xt.t[:, :], x[i * 128:(i + 1) * 128, :], [], [xt.c])
                ACT(junk.t[:, :], xt.t[:, :], AF.Square, [xt.c], [junk.c, ssq.c], accum_out=ssq.t[:, 0:1])
                rstd_from(ssq.t[:, 0:1], 128, 1, 1.0 / D, lnv, rstd, [ssq.c])
                STT(xn.t[:, :], xt.t[:, :], rstd.t[:, 0:1], gA.t[:, :], ALU.mult, ALU.mult, [xt.c, rstd.c, gA.c], [xn.c])
                bk = nb()
                bkb = bk.t[:, :].bitcast(BF16)
                for c in range(8):
                    TR(bkb[:, c * 128:(c + 1) * 128], xn.t[:, c * 128:(c + 1) * 128], ident_bf, [xn.c, cb.c], [bk.c])
                CP("act", xnT.t[:, :, i * 128:(i + 1) * 128], bkb.rearrange("p (c t) -> p c t", c=8), [bk.c], [xnT_c[i // 4]])
        em.barrier()

        if stage_limit >= 1:
            with contextlib.ExitStack() as st:
                whs = [sbt(st, f"wh{i}", [128, 8, 4, 128], BF16) for i in range(2)]
                lb_sb = sbt(st, "lb_sb", [128, 16], F32)
                oml = sbt(st, "oml", [128, 8], F32)
                ghg = sbt(st, "ghg", [128, 1], F32)
                class NS:
                    pass
                bufs = []
                for bi_ in range(2):
                    B = NS()
                    for nm in ("sq", "kk", "logf", "bcum", "dd", "ek", "eq", "sg", "osb", "lnv", "rs"):
                        setattr(B, nm, sbt(st, f"{nm}{bi_}", [128, 512], F32))
                    for nm in ("kdec", "qx", "osq", "oa"):
                        setattr(B, nm, sbt(st, f"{nm}{bi_}", [128, 512], BF16))
                    B.eb = sbt(st, f"eb{bi_}", [128, 8], F32)
                    B.vtok = sbt(st, f"vtok{bi_}", [128, 4, 128], BF16)
                    B.kdT = sbt(st, f"kdT{bi_}", [128, 4, 128], BF16)
                    B.A = sbt(st, f"A{bi_}", [128, 256], BF16)
                    bufs.append(B)
                S = sbt(st, "S", [128, 128], F32)
                Sds = [sbt(st, f"Sd{i}", [128, 128], BF16) for i in range(8)]
                zt = sbt(st, "zt", [128, 8192], BF16)
                zf = sbt(st, "zf", [128, D], F32)
                MSET("pool", zt.t[:, :], 0.0, [zt.c])
                MSET("pool", zf.t[:, :], 0.0, [zf.c])
                xg_flat = xg.rearrange("(p r) d -> p (r d)", p=128)
                nper = (NROWS + 128) // 128 * D
                for k0 in range(0, nper, 8192):
                    k1 = min(nper, k0 + 8192)
                    DMA("sp", xg_flat[:, k0:k1], zt.t[:, 0:k1 - k0], [zt.c], [])
                DMA("sp", yb[NROWS:NROWS + 128, :], zf.t[:, :], [zf.c], [])
                DMA("sp", lb_sb.t[:, :], lbT[:, :], [], [lb_sb.c])
                DMA("sp", ghg.t[:, :], g_hg[:, :], [], [ghg.c])
                TT("dve", oml.t[:, :], lb_sb.t[:, 8:16], lb_sb.t[:, 0:8], ALU.subtract, [lb_sb.c], [oml.c])
                ACT(oml.t[:, :], oml.t[:, :], AF.Sigmoid, [oml.c], [oml.c])
                PQ, PF_, PG_, PV, PTA, PUe, PUo, PO = PS
                pt_c = PTA.c
                pa_c = PTA.c
                lnc = math.log(128 ** -0.5)
                lncT = sbt(st, "lncT", [128, 1], F32)
                MSET("dve", lncT.t[:, :], lnc, [lncT.c])
                batches = [(h, s_, j) for h in range(8) for s_ in range(2) for j in range(4)]

                def load_w(h):
                    wh = whs[h % 2]
                    for fam in range(4):
                        col = fam * 1024 + h * 128
                        DMA("pool", wh.t[:, :, fam, :], w_in_v[:, :, col:col + 128], [], [wh.c])

                def geo(bi):
                    h, s_, j = batches[bi]
                    return h, s_, j, bufs[bi % 2], whs[h % 2], s_ * SEQ + j * 512

                def front_proj(bi):
                    h, s_, j, B, wh, T0 = geo(bi)
                    xc = xnT_c[T0 // 512]
                    for c in range(8):
                        MM(PQ.t[:, :], wh.t[:, c, 0, :], xnT.t[:, c, T0:T0 + 512], c == 0, c == 7, [wh.c, xc], [PQ.c])
                    for c in range(8):
                        MM(PF_.t[:, :], wh.t[:, c, 1, :], xnT.t[:, c, T0:T0 + 512], c == 0, c == 7, [wh.c, xc], [PF_.c])
                    for c in range(8):
                        MM(PG_.t[:, :], wh.t[:, c, 3, :], xnT.t[:, c, T0:T0 + 512], c == 0, c == 7, [wh.c, xc], [PG_.c])
                    for ti in range(4):
                        for c in range(8):
                            MM(PV.t[:, ti * 128:(ti + 1) * 128], xnT.t[:, c, T0 + ti * 128:T0 + (ti + 1) * 128],
                               wh.t[:, c, 2, :], c == 0, c == 7, [wh.c, xc], [PV.c])
                    ACT(B.kk.t[:, :], PF_.t[:, :], AF.Sigmoid, [PF_.c], [B.kk.c], scale=-1.0)
                    ACT(B.sq.t[:, :], PQ.t[:, :], AF.Silu, [PQ.c], [B.sq.c])
                    ACT(B.sg.t[:, :], PG_.t[:, :], AF.Silu, [PG_.c], [B.sg.c])
                    CP("act", B.vtok.t[:, :, :], PV.t[:, :].rearrange("p (a b) -> p a b", a=4), [PV.c], [B.vtok.c])

                def front_mid(bi):
                    h, s_, j, B, wh, T0 = geo(bi)
                    TS("dve", B.kk.t[:, :], B.kk.t[:, :], oml.t[:, h:h + 1], None, ALU.mult, None, [B.kk.c, oml.c], [B.kk.c])
                    ACT(B.logf.t[:, :], B.kk.t[:, :], AF.Ln, [B.kk.c, cf.c], [B.logf.c], scale=-1.0, bias=cf.t[:, C_ONES:C_ONES + 1])
                    em.op("dve", lambda e: e.tensor_tensor_scan(
                        out=B.bcum.t[:, :], data0=cf.t[:, C_RESET:C_RESET + 512], data1=B.logf.t[:, :],
                        initial=0.0, op0=ALU.mult, op1=ALU.add), [cf.c, B.logf.c], [B.bcum.c])
                    b3 = B.bcum.t[:, :].rearrange("p (a b) -> p a b", a=8)
                    d3 = B.dd.t[:, :].rearrange("p (a b) -> p a b", a=8)
                    TT("dve", d3, b3[:, :, 63:64].to_broadcast([128, 8, 64]), b3, ALU.subtract, [B.bcum.c], [B.dd.c])
                    ACT(B.ek.t[:, :], B.dd.t[:, :], AF.Exp, [B.dd.c], [B.ek.c])
                    ACT(B.eq.t[:, :], B.dd.t[:, :], AF.Exp, [B.dd.c, lncT.c], [B.eq.c], scale=-1.0, bias=lncT.t[:, 0:1])
                    ACT(B.eb.t[:, :], b3[:, :, 63], AF.Exp, [B.bcum.c], [B.eb.c])
                    TT("pool", B.kdec.t[:, :], B.kk.t[:, :], B.ek.t[:, :], ALU.mult, [B.kk.c, B.ek.c], [B.kdec.c])
                    TT("pool", B.qx.t[:, :], B.sq.t[:, :], B.eq.t[:, :], ALU.mult, [B.sq.c, B.eq.c], [B.qx.c])

                def front_end(bi):
                    h, s_, j, B, wh, T0 = geo(bi)
                    ptb = PTA.t[:, :].bitcast(BF16)
                    for ti in range(4):
                        TR(ptb[:, ti * 128:(ti + 1) * 128], B.kdec.t[:, ti * 128:(ti + 1) * 128], ident_bf, [B.kdec.c, cb.c], [pt_c])
                    CP("act", B.kdT.t[:, :, :], ptb[:, 0:512].rearrange("p (a b) -> p a b", a=4), [pt_c], [B.kdT.c])
                    for c8 in range(8):
                        ti, hf = c8 // 2, c8 % 2
                        MM(PTA.t[hf * 64:(hf + 1) * 64, 256 + ti * 64:256 + (ti + 1) * 64], B.kdec.t[:, c8 * 64:(c8 + 1) * 64],
                           B.qx.t[:, c8 * 64:(c8 + 1) * 64], True, True, [B.kdec.c, B.qx.c], [pa_c])
                    TT("dve", B.A.t[:, :], PTA.t[:, 256:512], cb.t[:, C_MASK256:C_MASK256 + 256], ALU.mult, [pa_c, cb.c], [B.A.c])

                def back_u(bi):
                    h, s_, j, B, wh, T0 = geo(bi)
                    for c8 in range(8):
                        ti, hf = c8 // 2, c8 % 2
                        hs = slice(hf * 64, (hf + 1) * 64)
                        ub = PUe if hf == 0 else PUo
                        MM(ub.t[:, ti * 128:(ti + 1) * 128], B.kdT.t[hs, ti, :], B.vtok.t[hs, ti, :], True, True,
                           [B.kdT.c, B.vtok.c], [ub.c])

                def back_chain(bi):
                    h, s_, j, B, wh, T0 = geo(bi)
                    if j == 0:
                        MSET("dve", S.t[:, :], 0.0, [S.c])
                        MSET("dve", Sds[0].t[:, :], 0.0, [Sds[0].c])
                    for c8 in range(8):
                        ti, hf = c8 // 2, c8 % 2
                        first = (j == 0 and c8 == 0)
                        ub = PUe if hf == 0 else PUo
                        if not first:
                            TS("dve", Sds[c8].t[:, :], S.t[:, :], B.eb.t[:, c8:c8 + 1], None, ALU.mult, None, [S.c, B.eb.c], [Sds[c8].c])
                        STT(S.t[:, :], S.t[:, :], B.eb.t[:, c8:c8 + 1], ub.t[:, ti * 128:(ti + 1) * 128], ALU.mult, ALU.add,
                            [S.c, B.eb.c, ub.c], [S.c])
                    for c8 in range(8):
                        ti, hf = c8 // 2, c8 % 2
                        hs = slice(hf * 64, (hf + 1) * 64)
                        MM(PO.t[:, c8 * 64:(c8 + 1) * 64], B.vtok.t[hs, ti, :], B.A.t[hs, ti * 64:(ti + 1) * 64], True, False,
                           [B.vtok.c, B.A.c], [PO.c])
                        MM(PO.t[:, c8 * 64:(c8 + 1) * 64], Sds[c8].t[:, :], B.qx.t[:, c8 * 64:(c8 + 1) * 64], False, True,
                           [Sds[c8].c, B.qx.c], [PO.c])
                    CP("act", B.osb.t[:, :], PO.t[:, :], [PO.c], [B.osb.c])
                    TT("pool", B.osq.t[:, :], B.osb.t[:, :], B.osb.t[:, :], ALU.mult, [B.osb.c], [B.osq.c])
                    MM(PO.t[:, :], ones_bf, B.osq.t[:, :], True, True, [cb.c, B.osq.c], [PO.c])

                def back_norm(bi):
                    h, s_, j, B, wh, T0 = geo(bi)
                    rstd_from(PO.t[:, :], 128, 512, 1.0 / 128, B.lnv, B.rs, [PO.c])
                    TT("dve", B.osb.t[:, :], B.osb.t[:, :], B.rs.t[:, :], ALU.mult, [B.osb.c, B.rs.c], [B.osb.c])
                    STT(B.oa.t[:, :], B.osb.t[:, :], ghg.t[:, 0:1], B.sg.t[:, :], ALU.mult, ALU.mult, [B.osb.c, ghg.c, B.sg.c], [B.oa.c])
                    DMA("sp", oaT[h * 128:(h + 1) * 128, T0:T0 + 512], B.oa.t[:, :], [B.oa.c], [])

                load_w(0)
                front_proj(0)
                front_mid(0)
                front_end(0)
                nb_ = len(batches)
                for bi in range(nb_):
                    h, s_, j = batches[bi]
                    if s_ == 0 and j == 0 and h + 1 < 8:
                        load_w(h + 1)
                    nx = bi + 1 < nb_
                    back_u(bi)
                    if nx:
                        front_proj(bi + 1)
                    back_chain(bi)
                    if nx:
                        front_mid(bi + 1)
                    back_norm(bi)
                    if nx:
                        front_end(bi + 1)
            em.barrier()


        if stage_limit >= 2:
            with contextlib.ExitStack() as st:
                wm = sbt(st, "wm", [128, 8, 768], BF16)
                wuq = sbt(st, "wuq", [128, 3, 1536], BF16)
                wuqs = sbt(st, "wuqs", [128, 3, 512], BF16)
                wukv = sbt(st, "wukv", [128, 2, 2048], BF16)
                gq = sbt(st, "gq", [128, 3], F32)
                gkv = sbt(st, "gkv", [128, 2], F32)
                cos2 = sbt(st, "cos2", [64, T], BF16)
                sin2 = sbt(st, "sin2", [64, T], BF16)
                scl = sbt(st, "scl", [64, 1], F32)
                rope_st = contextlib.ExitStack()
                posi = sbt(rope_st, "posi", [64, 1024], I32)
                ang = sbt(rope_st, "ang", [64, 1024], F32)
                uu = sbt(rope_st, "uu", [64, 1024], F32)
                ui = sbt(rope_st, "ui", [64, 1024], I32)
                uf = sbt(rope_st, "uf", [64, 1024], F32)
                PO, PL = PS[6], PS[7]
                TWO_PI = 2.0 * math.pi
                DMA("pool", wm.t[:, :, 0:704], w_in_v[:, :, 4096:4800], [], [wm.c])
                DMA("pool", wm.t[:, :, 704:768], w_krsw.rearrange("(c p) n -> p c n", p=128), [], [wm.c])
                DMA("pool", wuq.t[:, :, :], w_uq.rearrange("(c p) n -> p c n", p=128), [], [wuq.c])
                DMA("pool", wuqs.t[:, :, :], w_uqsw.rearrange("(c p) n -> p c n", p=128), [], [wuqs.c])
                DMA("pool", wukv.t[:, :, :], w_ukv.rearrange("(c p) n -> p c n", p=128), [], [wukv.c])
                DMA("sp", gq.t[:, :], g_q[:, :], [], [gq.c])
                DMA("sp", gkv.t[:, :], g_kv[:, :], [], [gkv.c])
                TS("dve", scl.t[:, :], cf.t[0:64, C_SGN:C_SGN + 1], TWO_PI * (1.0 - 1e-6), None, ALU.mult, None, [cf.c], [scl.c])
                for blk in range(4):
                    cs = slice(blk * 1024, (blk + 1) * 1024)
                    DMA("sp", posi.t[:, :], pos[0:1, cs].to_broadcast([64, 1024]), [], [posi.c])
                    CP("dve", ang.t[:, :], posi.t[:, :], [posi.c], [ang.c])
                    TS("dve", ang.t[:, :], ang.t[:, :], cf.t[0:64, C_INVF:C_INVF + 1], 1.0 / TWO_PI, ALU.mult, ALU.mult, [ang.c, cf.c], [ang.c])
                    for kind, off in (("sin", 0.0), ("cos", 0.25)):
                        TS("dve", uu.t[:, :], ang.t[:, :], off, None, ALU.add, None, [ang.c], [uu.c])
                        CP("dve", ui.t[:, :], uu.t[:, :], [uu.c], [ui.c])
                        CP("dve", uf.t[:, :], ui.t[:, :], [ui.c], [uf.c])
                        TT("dve", uu.t[:, :], uu.t[:, :], uf.t[:, :], ALU.subtract, [uu.c, uf.c], [uu.c])
                        TS("dve", uf.t[:, :], uu.t[:, :], 0.5, None, ALU.is_gt, None, [uu.c], [uf.c])
                        TT("dve", uu.t[:, :], uu.t[:, :], uf.t[:, :], ALU.subtract, [uu.c, uf.c], [uu.c])
                        TS("dve", uf.t[:, :], uu.t[:, :], -0.5, None, ALU.is_lt, None, [uu.c], [uf.c])
                        TT("dve", uu.t[:, :], uu.t[:, :], uf.t[:, :], ALU.add, [uu.c, uf.c], [uu.c])
                        if kind == "sin":
                            ACT(sin2.t[:, cs], uu.t[:, :], AF.Sin, [uu.c, scl.c], [sin2.c], scale=scl.t[:, 0:1])
                        else:
                            ACT(cos2.t[:, cs], uu.t[:, :], AF.Sin, [uu.c], [cos2.c], scale=TWO_PI * (1.0 - 1e-6))
                em.barrier()
                rope_st.close()
                sqc = [sbt(st, f"sqc{i}", [128, 512], BF16) for i in range(3)]
                lnv = sbt(st, "lnv2", [128, 512], F32)
                rs = sbt(st, "rs2", [128, 512], F32)
                cqn = sbt(st, "cqn", [128, 3, SEQ], BF16)
                ckvn = sbt(st, "ckvn", [128, 2, SEQ], BF16)
                krT = sbt(st, "krT", [64, SEQ], BF16)
                t1 = sbt(st, "t1", [64, 512], F32)
                t2 = sbt(st, "t2", [64, 512], F32)
                KnTs = [sbt(st, f"KnT{i}", [128, SEQ], BF16) for i in range(2)]
                Vhs = [sbt(st, f"Vh{i}", [128, 16, 128], BF16) for i in range(2)]
                qn = sbt(st, "qn", [128, 512], BF16)
                qr = sbt(st, "qr", [64, 512], BF16)
                pts = [sbt(st, f"pt{i}", [128, 512], BF16) for i in range(4)]
                lnl = sbt(st, "lnl", [128, 512], F32)
                rl = sbt(st, "rl", [128, 512], F32)
                obs = [sbt(st, f"ob{i}", [128, 512], BF16) for i in range(2)]
                nbat = 0
                for s in range(2):
                    for j in range(4):
                        T0 = s * SEQ + j * 512
                        L0 = j * 512
                        xc = xnT_c[T0 // 512]
                        for (dst_t, ncc, col0, gt, dim) in ((cqn, 3, 0, gq, 384), (ckvn, 2, 384, gkv, 256)):
                            banks = [nb() for _ in range(ncc)]
                            for cc in range(ncc):
                                for c in range(8):
                                    MM(banks[cc].t[:, :], wm.t[:, c, col0 + cc * 128:col0 + (cc + 1) * 128], xnT.t[:, c, T0:T0 + 512],
                                       c == 0, c == 7, [wm.c, xc], [banks[cc].c])
                            for cc in range(ncc):
                                ACT(sqc[cc].t[:, :], banks[cc].t[:, :], AF.Square, [banks[cc].c], [sqc[cc].c])
                            bs = nb()
                            for cc in range(ncc):
                                MM(bs.t[:, :], ones_bf, sqc[cc].t[:, :], cc == 0, cc == ncc - 1, [cb.c, sqc[cc].c], [bs.c])
                            rstd_from(bs.t[:, :], 128, 512, 1.0 / dim, lnv, rs, [bs.c])
                            for cc in range(ncc):
                                STT(dst_t.t[:, cc, L0:L0 + 512], banks[cc].t[:, :], gt.t[:, cc:cc + 1], rs.t[:, :], ALU.mult, ALU.mult,
                                    [banks[cc].c, gt.c, rs.c], [dst_t.c])
                        bk1, bk2 = nb(), nb()
                        for c in range(8):
                            MM(bk1.t[0:64, :], wm.t[:, c, 640:704], xnT.t[:, c, T0:T0 + 512], c == 0, c == 7, [wm.c, xc], [bk1.c])
                        for c in range(8):
                            MM(bk2.t[0:64, :], wm.t[:, c, 704:768], xnT.t[:, c, T0:T0 + 512], c == 0, c == 7, [wm.c, xc], [bk2.c])
                        TT("dve", t1.t[:, :], bk1.t[0:64, :], cos2.t[:, T0:T0 + 512], ALU.mult, [bk1.c, cos2.c], [t1.c])
                        TT("dve", t2.t[:, :], bk2.t[0:64, :], sin2.t[:, T0:T0 + 512], ALU.mult, [bk2.c, sin2.c], [t2.c])
                        TT("pool", krT.t[:, L0:L0 + 512], t1.t[:, :], t2.t[:, :], ALU.add, [t1.c, t2.c], [krT.c])
                    for h in range(8):
                        KnT = KnTs[h % 2]
                        Vh = Vhs[h % 2]
                        for j in range(4):
                            T0 = s * SEQ + j * 512
                            L0 = j * 512
                            bkk = nb()
                            for c in range(2):
                                MM(bkk.t[:, :], wukv.t[:, c, h * 256:h * 256 + 128], ckvn.t[:, c, L0:L0 + 512], c == 0, c == 1,
                                   [wukv.c, ckvn.c], [bkk.c])
                            CP("act", KnT.t[:, L0:L0 + 512], bkk.t[:, :], [bkk.c], [KnT.c])
                            bv = nb()
                            for ti in range(4):
                                for c in range(2):
                                    MM(bv.t[:, ti * 128:(ti + 1) * 128], ckvn.t[:, c, L0 + ti * 128:L0 + (ti + 1) * 128],
                                       wukv.t[:, c, h * 256 + 128:h * 256 + 256], c == 0, c == 1, [wukv.c, ckvn.c], [bv.c])
                            CP("dve", Vh.t[:, j * 4:(j + 1) * 4, :], bv.t[:, :].rearrange("p (a b) -> p a b", a=4), [bv.c], [Vh.c])
                            bq, bp, bps = nb(), nb(), nb()
                            for c in range(3):
                                MM(bq.t[:, :], wuq.t[:, c, h * 192:h * 192 + 128], cqn.t[:, c, L0:L0 + 512], c == 0, c == 2, [wuq.c, cqn.c], [bq.c])
                            for c in range(3):
                                MM(bp.t[0:64, :], wuq.t[:, c, h * 192 + 128:h * 192 + 192], cqn.t[:, c, L0:L0 + 512], c == 0, c == 2,
                                   [wuq.c, cqn.c], [bp.c])
                            for c in range(3):
                                MM(bps.t[0:64, :], wuqs.t[:, c, h * 64:(h + 1) * 64], cqn.t[:, c, L0:L0 + 512], c == 0, c == 2,
                                   [wuqs.c, cqn.c], [bps.c])
                            CP("act", qn.t[:, :], bq.t[:, :], [bq.c], [qn.c])
                            TT("dve", t1.t[:, :], bp.t[0:64, :], cos2.t[:, T0:T0 + 512], ALU.mult, [bp.c, cos2.c], [t1.c])
                            TT("dve", t2.t[:, :], bps.t[0:64, :], sin2.t[:, T0:T0 + 512], ALU.mult, [bps.c, sin2.c], [t2.c])
                            TT("pool", qr.t[:, :], t1.t[:, :], t2.t[:, :], ALU.add, [t1.c, t2.c], [qr.c])
                            nkt = 4 * j + 4
                            def s_mm(kt):
                                bst = nb()
                                MM(bst.t[:, :], KnT.t[:, kt * 128:(kt + 1) * 128], qn.t[:, :], True, False, [KnT.c, qn.c], [bst.c])
                                MM(bst.t[:, :], krT.t[:, kt * 128:(kt + 1) * 128], qr.t[:, :], False, True, [krT.c, qr.c], [bst.c])
                                return bst
                            pend = [s_mm(0), s_mm(1)]
                            for kt in range(nkt):
                                bst = pend.pop(0)
                                if kt + 2 < nkt:
                                    pend.append(s_mm(kt + 2))
                                pt = pts[kt % 4]
                                ACT(pt.t[:, :], bst.t[:, :], AF.Exp, [bst.c], [pt.c], scale=192.0 ** -0.5)
                                if kt >= 4 * j:
                                    r = kt - 4 * j
                                    TT("dve", pt.t[:, :], pt.t[:, :], cb.t[:, C_AMASK + r * 512:C_AMASK + (r + 1) * 512], ALU.mult,
                                       [pt.c, cb.c], [pt.c])
                                MM(PO.t[:, :], Vh.t[:, kt, :], pt.t[:, :], kt == 0, kt == nkt - 1, [Vh.c, pt.c], [PO.c])
                                MM(PL.t[:, :], ones_bf, pt.t[:, :], kt == 0, kt == nkt - 1, [cb.c, pt.c], [PL.c])
                            ACT(lnl.t[:, :], PL.t[:, :], AF.Ln, [PL.c], [lnl.c])
                            ACT(rl.t[:, :], lnl.t[:, :], AF.Exp, [lnl.c], [rl.c], scale=-1.0)
                            ob = obs[nbat % 2]
                            nbat += 1
                            TT("dve", ob.t[:, :], PO.t[:, :], rl.t[:, :], ALU.mult, [PO.c, rl.c], [ob.c])
                            DMA("sp", obT[h * 128:(h + 1) * 128, T0:T0 + 512], ob.t[:, :], [ob.c], [])
            em.barrier()

        if stage_limit >= 3:
            with contextlib.ExitStack() as st:
                wg = sbt(st, "wg", [128, 8, 2048], BF16)
                wbh = sbt(st, "wbh", [128, 8, D], BF16)
                wbm = sbt(st, "wbm", [128, 8, D], BF16)
                wo = sbt(st, "wo", [128, 8, D], BF16)
                wr = sbt(st, "wr", [128, 8, 72], F32)
                br = sbt(st, "br", [128, 72], F32)
                g2 = sbt(st, "g2", [128, D], F32)
                oabs = [sbt(st, f"oab{i}", [128, 8, 128], BF16) for i in range(2)]
                obbs = [sbt(st, f"obb{i}", [128, 8, 128], BF16) for i in range(2)]
                Ta = sbt(st, "Ta", [128, D], F32)
                Tb = sbt(st, "Tb", [128, D], F32)
                ybf = sbt(st, "ybf", [128, D], BF16)
                yT = sbt(st, "yT", [128, 8, 128], BF16)
                h2bfs = [sbt(st, f"h2bf{i}", [128, D], BF16) for i in range(2)]
                h2T = sbt(st, "h2T", [128, 8, 128], F32)
                ssq = sbt(st, "ssq3", [128, 2], F32)
                lnv = sbt(st, "lnv3", [128, 2], F32)
                rstd = sbt(st, "rstd3", [128, 2], F32)
                lg = sbt(st, "lg", [128, 72], F32)
                g8 = sbt(st, "g8", [128, 8], F32)
                ohg = sbt(st, "ohg", [128, 8], F32)
                ngm = sbt(st, "ngm", [128, 1], F32)
                ex = sbt(st, "ex", [128, 8], F32)
                gs = sbt(st, "gs", [128, 1], F32)
                gw = sbt(st, "gw", [128, 1], F32)
                pen = sbt(st, "pen", [128, 8], F32)
                msk = sbt(st, "msk", [128, 64], F32)
                m8 = sbt(st, "m8", [128, 8], F32)
                i8 = sbt(st, "i8", [128, 8], U32)
                idf = sbt(st, "idf", [128, 2], F32)
                dlt = sbt(st, "dlt", [128, 1], F32)
                ed = sbt(st, "ed", [128, 1], F32)
                den = sbt(st, "den", [128, 1], F32)
                e1 = sbt(st, "e1", [128, 1], F32)
                e2 = sbt(st, "e2", [128, 1], F32)
                oh1 = sbt(st, "oh1", [128, 64], F32)
                oh2 = sbt(st, "oh2", [128, 64], F32)
                oh = sbt(st, "oh", [128, 64], F32)
                tmp64 = sbt(st, "tmp64", [128, 64], F32)
                sl = sbt(st, "sl", [128, 2], F32)
                ovf = sbt(st, "ovf", [128, 2], F32)
                dsf = sbt(st, "dsf", [128, 2], F32)
                base = sbt(st, "base", [1, 64], F32)
                xg_c = em.cell("xg")
                DMA("pool", wg.t[:, :, :], w_in_v[:, :, 4800:6848], [], [wg.c])
                DMA("pool", wbh.t[:, :, :], w_bh.rearrange("(c p) n -> p c n", p=128), [], [wbh.c])
                DMA("pool", wbm.t[:, :, :], w_bm.rearrange("(c p) n -> p c n", p=128), [], [wbm.c])
                DMA("pool", wo.t[:, :, :], w_out.rearrange("(c p) n -> p c n", p=128), [], [wo.c])
                DMA("sp", wr.t[:, :, :], w_r.rearrange("(c p) n -> p c n", p=128), [], [wr.c])
                DMA("sp", br.t[:, :], b_r[0:1, :].to_broadcast([128, 72]), [], [br.c])
                DMA("sp", g2.t[:, :], g_ffn[0:1, :].to_broadcast([128, D]), [], [g2.c])
                MSET("dve", base.t[:, :], 0.0, [base.c])
                oaT_v = oaT.rearrange("(c p) t -> p c t", p=128)
                obT_v = obT.rearrange("(c p) t -> p c t", p=128)
                ident_f = cf.t[:, C_IDENT:C_IDENT + 128]
                iota = cf.t[:, C_IOTA:C_IOTA + 64]
                Tas = [Ta, sbt(st, "Ta1", [128, D], F32)]
                Tbs = [Tb, sbt(st, "Tb1", [128, D], F32)]
                ybfs = [ybf, sbt(st, "ybf1", [128, D], BF16)]
                NT = T // 128

                def f_load(i):
                    tok = slice(i * 128, (i + 1) * 128)
                    DMA("sp", oabs[i % 2].t[:, :, :], oaT_v[:, :, tok], [], [oabs[i % 2].c])
                    DMA("sp", obbs[i % 2].t[:, :, :], obT_v[:, :, tok], [], [obbs[i % 2].c])

                def f_gate(i, which):
                    tok = slice(i * 128, (i + 1) * 128)
                    xc = xnT_c[i // 4]
                    Tt = (Tas if which == 0 else Tbs)[i % 2]
                    goff = 0 if which == 0 else 1024
                    for hf in range(2):
                        b = nb()
                        for c in range(8):
                            MM(b.t[:, :], xnT.t[:, c, tok], wg.t[:, c, goff + hf * 512:goff + (hf + 1) * 512], c == 0, c == 7,
                               [xc, wg.c], [b.c])
                        ACT(Tt.t[:, hf * 512:(hf + 1) * 512], b.t[:, :], AF.Sigmoid, [b.c], [Tt.c])

                def f_branch(i, which):
                    Tt = (Tas if which == 0 else Tbs)[i % 2]
                    src = (oabs if which == 0 else obbs)[i % 2]
                    wb = wbh if which == 0 else wbm
                    for hf in range(2):
                        b = nb()
                        for c in range(8):
                            MM(b.t[:, :], src.t[:, c, :], wb.t[:, c, hf * 512:(hf + 1) * 512], c == 0, c == 7, [src.c, wb.c], [b.c])
                        TT("dve", Tt.t[:, hf * 512:(hf + 1) * 512], b.t[:, :], Tt.t[:, hf * 512:(hf + 1) * 512], ALU.mult, [b.c, Tt.c], [Tt.c])
                    if which == 1:
                        TT("pool", ybfs[i % 2].t[:, :], Tas[i % 2].t[:, :], Tbs[i % 2].t[:, :], ALU.add,
                           [Tas[i % 2].c, Tbs[i % 2].c], [ybfs[i % 2].c])

                def b1(i):
                    tok = slice(i * 128, (i + 1) * 128)
                    yb_, Tb_ = ybfs[i % 2], Tbs[i % 2]
                    bt = nb()
                    btb = bt.t[:, :].bitcast(BF16)
                    for c in range(8):
                        TR(btb[:, c * 128:(c + 1) * 128], yb_.t[:, c * 128:(c + 1) * 128], ident_bf, [yb_.c, cb.c], [bt.c])
                    CP("act", yT.t[:, :, :], btb.rearrange("p (c t) -> p c t", c=8), [bt.c], [yT.c])
                    DMA("sp", Tb_.t[:, :], x[tok, :], [], [Tb_.c])

                def b2(i):
                    tok = slice(i * 128, (i + 1) * 128)
                    Ta_, Tb_, yb_ = Tas[i % 2], Tbs[i % 2], ybfs[i % 2]
                    h2bf = h2bfs[i % 2]
                    for hf in range(2):
                        b = nb()
                        for c in range(8):
                            MM(b.t[:, :], yT.t[:, c, :], wo.t[:, c, hf * 512:(hf + 1) * 512], c == 0, c == 7, [yT.c, wo.c], [b.c])
                        TT("dve", Ta_.t[:, hf * 512:(hf + 1) * 512], b.t[:, :], Tb_.t[:, hf * 512:(hf + 1) * 512], ALU.add, [b.c, Tb_.c], [Ta_.c])
                    DMA("sp", x1d[tok, :], Ta_.t[:, :], [Ta_.c], [])
                    ACT(yb_.t[:, :], Ta_.t[:, :], AF.Square, [Ta_.c], [yb_.c, ssq.c], accum_out=ssq.t[:, 0:1])
                    rstd_from(ssq.t[:, 0:1], 128, 1, 1.0 / D, lnv, rstd, [ssq.c])
                    STT(Tb_.t[:, :], Ta_.t[:, :], rstd.t[:, 0:1], g2.t[:, :], ALU.mult, ALU.mult, [Ta_.c, rstd.c, g2.c], [Tb_.c])
                    CP("pool", h2bf.t[:, :], Tb_.t[:, :], [Tb_.c], [h2bf.c])

                def b3(i):
                    Tb_ = Tbs[i % 2]
                    p1, p2 = nb(), nb()
                    for c in range(8):
                        bb = p1 if c < 4 else p2
                        TR(bb.t[:, (c % 4) * 128:(c % 4 + 1) * 128], Tb_.t[:, c * 128:(c + 1) * 128], ident_f, [Tb_.c, cf.c], [bb.c])
                    CP("act", h2T.t[:, 0:4, :], p1.t[:, :].rearrange("p (c t) -> p c t", c=4), [p1.c], [h2T.c])
                    CP("act", h2T.t[:, 4:8, :], p2.t[:, :].rearrange("p (c t) -> p c t", c=4), [p2.c], [h2T.c])

                def b4(i):
                    bl = nb()
                    for c in range(8):
                        MM(bl.t[:, 0:72], h2T.t[:, c, :], wr.t[:, c, :], c == 0, c == 7, [h2T.c, wr.c], [bl.c])
                    TT("dve", lg.t[:, :], bl.t[:, 0:72], br.t[:, :], ALU.add, [bl.c, br.c], [lg.c])
                    em.op("dve", lambda e: e.max(out=g8.t[:, :], in_=lg.t[:, 0:8]), [lg.c], [g8.c])
                    TS("dve", ohg.t[:, :], lg.t[:, 0:8], g8.t[:, 0:1], None, ALU.is_equal, None, [lg.c, g8.c], [ohg.c])
                    TS("dve", ngm.t[:, :], g8.t[:, 0:1], -1.0, None, ALU.mult, None, [g8.c], [ngm.c])
                    ACT(ex.t[:, :], lg.t[:, 0:8], AF.Exp, [lg.c, ngm.c], [ex.c, gs.c], bias=ngm.t[:, 0:1], accum_out=gs.t[:, 0:1])
                    em.op("dve", lambda e: e.reciprocal(out=gw.t[:, :], in_=gs.t[:, :]), [gs.c], [gw.c])
                    TS("dve", pen.t[:, :], ohg.t[:, :], -1.0, 1e30, ALU.add, ALU.mult, [ohg.c], [pen.c])
                    TT("dve", msk.t[:, :].rearrange("p (a b) -> p a b", a=8), lg.t[:, 8:72].rearrange("p (a b) -> p a b", a=8),
                       pen.t[:, :].rearrange("p (a b) -> p a b", b=1).to_broadcast([128, 8, 8]), ALU.add, [lg.c, pen.c], [msk.c])
                    em.op("dve", lambda e: e.max(out=m8.t[:, :], in_=msk.t[:, :]), [msk.c], [m8.c])
                    em.op("dve", lambda e: e.max_index(out=i8.t[:, :], in_max=m8.t[:, :], in_values=msk.t[:, :]), [m8.c, msk.c], [i8.c])
                    CP("dve", idf.t[:, :], i8.t[:, 0:2], [i8.c], [idf.c])
                    TT("dve", dlt.t[:, :], m8.t[:, 1:2], m8.t[:, 0:1], ALU.subtract, [m8.c], [dlt.c])
                    ACT(ed.t[:, :], dlt.t[:, :], AF.Exp, [dlt.c], [ed.c])
                    TS("dve", den.t[:, :], ed.t[:, :], 1.0, None, ALU.add, None, [ed.c], [den.c])
                    em.op("dve", lambda e: e.reciprocal(out=e1.t[:, :], in_=den.t[:, :]), [den.c], [e1.c])
                    TT("dve", e2.t[:, :], ed.t[:, :], e1.t[:, :], ALU.mult, [ed.c, e1.c], [e2.c])
                    TT("dve", wts.t[:, 2 * i:2 * i + 1], e1.t[:, :], gw.t[:, :], ALU.mult, [e1.c, gw.c], [wts.c])
                    TT("dve", wts.t[:, 2 * i + 1:2 * i + 2], e2.t[:, :], gw.t[:, :], ALU.mult, [e2.c, gw.c], [wts.c])
                    TS("dve", oh1.t[:, :], iota, idf.t[:, 0:1], None, ALU.is_equal, None, [cf.c, idf.c], [oh1.c])
                    TS("dve", oh2.t[:, :], iota, idf.t[:, 1:2], None, ALU.is_equal, None, [cf.c, idf.c], [oh2.c])
                    TT("dve", oh.t[:, :], oh1.t[:, :], oh2.t[:, :], ALU.add, [oh1.c, oh2.c], [oh.c])

                def b5(i):
                    h2bf = h2bfs[i % 2]
                    bp_ = nb()
                    MM(bp_.t[:, 0:64], cf.t[:, C_TRIST:C_TRIST + 128], oh.t[:, :], True, False, [cf.c, oh.c], [bp_.c])
                    MM(bp_.t[:, 0:64], cf.t[0:1, C_ONES:C_ONES + 128], base.t[0:1, :], False, True, [cf.c, base.c], [bp_.c])
                    bc_ = nb()
                    MM(bc_.t[0:1, 0:64], cf.t[:, C_ONES:C_ONES + 1], oh.t[:, :], True, True, [cf.c, oh.c], [bc_.c])
                    TT("dve", tmp64.t[:, :], bp_.t[:, 0:64], oh1.t[:, :], ALU.mult, [bp_.c, oh1.c], [tmp64.c])
                    em.op("dve", lambda e: e.reduce_sum(out=sl.t[:, 0:1], in_=tmp64.t[:, :], axis=mybir.AxisListType.X), [tmp64.c], [sl.c])
                    TT("dve", tmp64.t[:, :], bp_.t[:, 0:64], oh2.t[:, :], ALU.mult, [bp_.c, oh2.c], [tmp64.c])
                    em.op("dve", lambda e: e.reduce_sum(out=sl.t[:, 1:2], in_=tmp64.t[:, :], axis=mybir.AxisListType.X), [tmp64.c], [sl.c])
                    TT("dve", base.t[0:1, :], base.t[0:1, :], bc_.t[0:1, 0:64], ALU.add, [base.c, bc_.c], [base.c])
                    TS("dve", ovf.t[:, :], sl.t[:, :], float(CAP), 1e6, ALU.is_ge, ALU.mult, [sl.c], [ovf.c])
                    STT(dsf.t[:, :], idf.t[:, :], float(CAP), sl.t[:, :], ALU.mult, ALU.add, [idf.c, sl.c], [dsf.c])
                    TT("dve", dsf.t[:, :], dsf.t[:, :], ovf.t[:, :], ALU.add, [dsf.c, ovf.c], [dsf.c])
                    TS("dve", dsf.t[:, :], dsf.t[:, :], float(NROWS), None, ALU.min, None, [dsf.c], [dsf.c])
                    CP("dve", dst.t[:, 2 * i:2 * i + 2], dsf.t[:, :], [dsf.c], [dst.c])
                    for k in range(2):
                        em.op("pool", lambda e, k=k, i=i, h2bf=h2bf: e.indirect_dma_start(
                            out=xg[:, :], out_offset=bass.IndirectOffsetOnAxis(ap=dst.t[:, 2 * i + k:2 * i + k + 1], axis=0),
                            in_=h2bf.t[:, :], in_offset=None), [dst.c, h2bf.c], [xg_c], kind="d")

                f_load(0)
                f_gate(0, 0)
                f_branch(0, 0)
                f_gate(0, 1)
                f_branch(0, 1)
                for i in range(NT):
                    nx = i + 1 < NT
                    if nx:
                        f_load(i + 1)
                    b1(i)
                    if nx:
                        f_gate(i + 1, 0)
                    b2(i)
                    if nx:
                        f_branch(i + 1, 0)
                    b3(i)
                    if nx:
                        f_gate(i + 1, 1)
                    b4(i)
                    if nx:
                        f_branch(i + 1, 1)
                    b5(i)
            em.barrier()

        if stage_limit >= 4:
            with contextlib.ExitStack() as st:
                NR = CAP // 128
                w1s = [sbt(st, f"w1s{i}", [128, 8, 512], BF16) for i in range(3)]
                w3s = [sbt(st, f"w3s{i}", [128, 8, 512], BF16) for i in range(3)]
                w2s = [sbt(st, f"w2s{i}", [128, 4, D], BF16) for i in range(3)]
                xgs = [sbt(st, f"xgs{i}", [128, NR, D], BF16) for i in range(2)]
                xgT = [sbt(st, f"xgT{i}", [128, 8, CAP], BF16) for i in range(2)]
                s1s = [sbt(st, f"s1s{i}", [128, CAP], F32) for i in range(2)]
                hid = [sbt(st, f"hid{i}", [128, 4, CAP], BF16) for i in range(2)]
                ysb = [sbt(st, f"ysb{i}", [128, D], F32) for i in range(2)]
                nsi = [0]

                def load_e(ex_):
                    sl_ = ex_ % 3
                    DMA("pool", w1s[sl_].t[:, :, :], ew1[ex_].rearrange("(c p) n -> p c n", p=128), [], [w1s[sl_].c])
                    DMA("pool", w3s[sl_].t[:, :, :], ew3[ex_].rearrange("(c p) n -> p c n", p=128), [], [w3s[sl_].c])
                    DMA("pool", w2s[sl_].t[:, :, :], ew2[ex_].rearrange("(c p) n -> p c n", p=128), [], [w2s[sl_].c])
                    xs = xgs[ex_ % 2]
                    DMA("sp", xs.t[:, :, :], xg[ex_ * CAP:(ex_ + 1) * CAP, :].rearrange("(r p) d -> p r d", p=128), [], [xs.c])

                def front_e(ex_):
                    sl_ = ex_ % 3
                    xs = xgs[ex_ % 2]
                    xT = xgT[ex_ % 2]
                    hd = hid[ex_ % 2]
                    for r in range(NR):
                        bt = nb()
                        btb = bt.t[:, :].bitcast(BF16)
                        for c in range(8):
                            TR(btb[:, c * 128:(c + 1) * 128], xs.t[:, r, c * 128:(c + 1) * 128], ident_bf, [xs.c, cb.c], [bt.c])
                        CP("act" if r == 0 else "dve", xT.t[:, :, r * 128:(r + 1) * 128], btb.rearrange("p (c t) -> p c t", c=8), [bt.c], [xT.c])
                    for fc in range(4):
                        b1, b3 = nb(), nb()
                        for c in range(8):
                            MM(b1.t[:, 0:CAP], w1s[sl_].t[:, c, fc * 128:(fc + 1) * 128], xT.t[:, c, :], c == 0, c == 7, [w1s[sl_].c, xT.c], [b1.c])
                        for c in range(8):
                            MM(b3.t[:, 0:CAP], w3s[sl_].t[:, c, fc * 128:(fc + 1) * 128], xT.t[:, c, :], c == 0, c == 7, [w3s[sl_].c, xT.c], [b3.c])
                        s1 = s1s[nsi[0] % 2]
                        nsi[0] += 1
                        ACT(s1.t[:, :], b1.t[:, 0:CAP], AF.Silu, [b1.c], [s1.c])
                        TT("dve", hd.t[:, fc, :], b3.t[:, 0:CAP], s1.t[:, :], ALU.mult, [b3.c, s1.c], [hd.c])

                def back_e(ex_):
                    sl_ = ex_ % 3
                    hd = hid[ex_ % 2]
                    for r in range(NR):
                        ys = ysb[r % 2]
                        for hf in range(2):
                            b = PS[6 + hf]
                            for fc in range(4):
                                MM(b.t[:, :], hd.t[:, fc, r * 128:(r + 1) * 128], w2s[sl_].t[:, fc, hf * 512:(hf + 1) * 512], fc == 0, fc == 3,
                                   [hd.c, w2s[sl_].c], [b.c])
                            CP("act" if hf == 0 else "dve", ys.t[:, hf * 512:(hf + 1) * 512], b.t[:, :], [b.c], [ys.c])
                        DMA("sp", yb[ex_ * CAP + r * 128:ex_ * CAP + (r + 1) * 128, :], ys.t[:, :], [ys.c], [])

                load_e(0)
                load_e(1)
                front_e(0)
                for ex_ in range(NEXP):
                    if ex_ + 2 < NEXP:
                        load_e(ex_ + 2)
                    if ex_ + 1 < NEXP:
                        front_e(ex_ + 1)
                    back_e(ex_)
            em.barrier()

        if stage_limit >= 5:
            with contextlib.ExitStack() as st:
                y0s = [sbt(st, f"y0s{i}", [128, D], F32) for i in range(2)]
                y1s = [sbt(st, f"y1s{i}", [128, D], F32) for i in range(2)]
                x1s = [sbt(st, f"x1s{i}", [128, D], F32) for i in range(2)]
                accs = [sbt(st, f"acc{i}", [128, D], F32) for i in range(2)]
                junk = sbt(st, "junk5", [128, D], BF16)
                gF = sbt(st, "gF", [128, D], F32)
                ssq = sbt(st, "ssq5", [128, 2], F32)
                lnv = sbt(st, "lnv5", [128, 2], F32)
                rstd = sbt(st, "rstd5", [128, 2], F32)
                DMA("sp", gF.t[:, :], g_fin[0:1, :].to_broadcast([128, D]), [], [gF.c])
                for i in range(T // 128):
                    tok = slice(i * 128, (i + 1) * 128)
                    y0, y1, x1t, acc = y0s[i % 2], y1s[i % 2], x1s[i % 2], accs[i % 2]
                    for k, yt in ((0, y0), (1, y1)):
                        em.op("pool", lambda e, k=k, i=i, yt=yt: e.indirect_dma_start(
                            out=yt.t[:, :], out_offset=None, in_=yb[:, :],
                            in_offset=bass.IndirectOffsetOnAxis(ap=dst.t[:, 2 * i + k:2 * i + k + 1], axis=0)), [dst.c], [yt.c], kind="d")
                    DMA("sp", x1t.t[:, :], x1d[tok, :], [], [x1t.c])
                    STT(acc.t[:, :], y0.t[:, :], wts.t[:, 2 * i:2 * i + 1], x1t.t[:, :], ALU.mult, ALU.add, [y0.c, wts.c, x1t.c], [acc.c])
                    STT(acc.t[:, :], y1.t[:, :], wts.t[:, 2 * i + 1:2 * i + 2], acc.t[:, :], ALU.mult, ALU.add, [y1.c, wts.c, acc.c], [acc.c])
                    ACT(junk.t[:, :], acc.t[:, :], AF.Square, [acc.c], [junk.c, ssq.c], accum_out=ssq.t[:, 0:1])
                    rstd_from(ssq.t[:, 0:1], 128, 1, 1.0 / D, lnv, rstd, [ssq.c])
                    STT(acc.t[:, :], acc.t[:, :], rstd.t[:, 0:1], gF.t[:, :], ALU.mult, ALU.mult, [acc.c, rstd.c, gF.c], [acc.c])
                    DMA("sp", out[tok, :], acc.t[:, :], [acc.c], [])

        stats = em.emit()
    return nc, stats


_CACHE = {}


def prep_inputs(inputs):
    f = lambda a: np.ascontiguousarray(np.asarray(a, dtype=np.float32))

    def pc(w, nch):
        e, r, n = w.shape
        return np.ascontiguousarray(w.reshape(e, nch, 128, n).transpose(0, 2, 1, 3)).reshape(e, 128, nch * n)
    x = f(inputs["x"])
    positions = np.asarray(inputs["positions"]).astype(np.int32)
    w_in = f(inputs["w_in"][0])
    kr0 = 4096 + 384 + 256
    w_krsw = np.ascontiguousarray(np.concatenate([w_in[:, kr0 + 32:kr0 + 64], w_in[:, kr0:kr0 + 32]], axis=1))
    hl = f(inputs["hg_lower_bound"])
    lbT = np.ascontiguousarray(hl.reshape(2, 8, 128).transpose(2, 0, 1).reshape(128, 16))
    w_uq = f(inputs["mla_w_uq"][0])
    uq3 = w_uq.reshape(384, 8, 192)
    w_uqsw = np.ascontiguousarray(np.concatenate([uq3[:, :, 160:192], uq3[:, :, 128:160]], axis=2).reshape(384, 512))
    shared = {
        "cst": make_consts(),
        "w_in": w_in,
        "w_krsw": w_krsw,
        "g_attn": f(inputs["attn_norm_w"]).reshape(1, D),
        "g_ffn": f(inputs["ffn_norm_w"]).reshape(1, D),
        "g_fin": f(inputs["final_norm_w"]).reshape(1, D),
        "lbT": lbT,
        "g_hg": f(inputs["hg_out_norm_w"]).reshape(128, 1),
        "g_q": np.ascontiguousarray(f(inputs["mla_q_norm_w"]).reshape(3, 128).T),
        "g_kv": np.ascontiguousarray(f(inputs["mla_kv_norm_w"]).reshape(2, 128).T),
        "w_uq": w_uq,
        "w_uqsw": w_uqsw,
        "w_ukv": f(inputs["mla_w_ukv"][0]),
        "w_bh": f(inputs["w_branch_hgrn"][0]),
        "w_bm": f(inputs["w_branch_mla"][0]),
        "w_out": f(inputs["w_out"][0]),
        "w_r": np.ascontiguousarray(np.concatenate([f(inputs["router_group_w"][0]), f(inputs["router_expert_w"][0])], axis=1)),
        "b_r": np.ascontiguousarray(np.concatenate([f(inputs["router_group_b"][0]), f(inputs["router_expert_b"][0])]).reshape(1, 72)),
        "ew1": f(inputs["expert_w1"][0]),
        "ew3": f(inputs["expert_w3"][0]),
        "ew2": f(inputs["expert_w2"][0]),
    }
    in_maps = []
    for c in range(NCORES):
        m = dict(shared)
        m["x"] = np.ascontiguousarray(x[2 * c:2 * c + 2].reshape(T, D))
        m["pos"] = np.ascontiguousarray(positions[2 * c:2 * c + 2].reshape(1, T))
        in_maps.append(m)
    return in_maps


def kernel(**inputs):
    if "nc" not in _CACHE:
        _CACHE["nc"] = build()[0]
    nc = _CACHE["nc"]
    in_maps = prep_inputs(inputs)
    res = run_bass_kernel_spmd(nc, in_maps, core_ids=list(range(NCORES)))
    outs = [np.asarray(r["out"]).reshape(2, SEQ, D) for r in res.results]
    return np.concatenate(outs, axis=0).astype(np.float32)
```

```python
import math
import contextlib
import numpy as np
import concourse.bass as bass
import concourse.mybir as mybir
from concourse.bass_utils import run_bass_kernel_spmd

F32 = mybir.dt.float32
BF16 = mybir.dt.bfloat16
I32 = mybir.dt.int32
U32 = mybir.dt.uint32
AF = mybir.ActivationFunctionType
ALU = mybir.AluOpType

NCORES = 8
T = 4096
SEQ = 2048
D = 1024
EPS = 1e-6
CAP = 256
NEXP = 64
NROWS = NEXP * CAP
SAME_ENG_SYNC = True
N_DMA_SEMS = 40
SCRATCH_INTERNAL = True

C_IDENT = 0
C_TRILE = 128
C_TRIST = 256
C_MASK256 = 384
C_RESET = 640
C_IOTA = 1152
C_INVF = 1216
C_SGN = 1217
C_ONES = 1218
C_AMASK = 1346
NCONST = C_AMASK + 2048


def make_consts():
    c = np.zeros((128, NCONST), np.float32)
    p = np.arange(128)[:, None]
    f = np.arange(128)[None, :]
    c[:, C_IDENT:C_IDENT + 128] = (p == f)
    c[:, C_TRILE:C_TRILE + 128] = (p <= f)
    c[:, C_TRIST:C_TRIST + 128] = (p < f)
    f256 = np.arange(256)[None, :]
    c[:, C_MASK256:C_MASK256 + 256] = ((p % 64) <= (f256 % 64))
    f512 = np.arange(512)[None, :]
    c[:, C_RESET:C_RESET + 512] = ((f512 % 64) != 0)
    c[:, C_IOTA:C_IOTA + 64] = np.arange(64)[None, :]
    half = 32
    inv_freq = (10000.0 ** (-np.arange(half, dtype=np.float32) / half)).astype(np.float32)
    c[:, C_INVF] = inv_freq[np.arange(128) % 32]
    c[:, C_SGN] = np.where((np.arange(128) % 64) < 32, -1.0, 1.0)
    c[:, C_ONES:C_ONES + 128] = 1.0
    for r in range(4):
        c[:, C_AMASK + r * 512:C_AMASK + (r + 1) * 512] = ((p + r * 128) <= f512)
    return c


class Cell:
    __slots__ = ("name", "w", "r")

    def __init__(self, name):
        self.name = name
        self.w = None
        self.r = []


class TB:
    def __init__(self, t, c):
        self.t = t
        self.c = c


class Emitter:
    def __init__(self, nc, es):
        self.nc = nc
        self.es = es
        self.eng = {"pe": nc.tensor, "act": nc.scalar, "dve": nc.vector, "pool": nc.gpsimd, "sp": nc.sync}
        self.ops = []
        self.last = {}
        self.dmas_since = []

    def cell(self, name="c"):
        return Cell(name)

    def op(self, eng, fn, reads=(), writes=(), kind="c"):
        oid = len(self.ops)
        deps = set()
        for c in reads:
            if c.w is not None:
                deps.add(c.w)
        for c in writes:
            if c.w is not None:
                deps.add(c.w)
            deps.update(c.r)
        self.ops.append(dict(eng=eng, fn=fn, deps=deps, kind=kind, sig=False))
        for c in reads:
            if kind == "c":
                c.r = [q for q in c.r if not (self.ops[q]["kind"] == "c" and self.ops[q]["eng"] == eng)]
            c.r.append(oid)
        for c in writes:
            c.w = oid
            c.r = []
        if kind == "d":
            self.dmas_since.append(oid)
        else:
            self.last[eng] = oid
        return oid

    def barrier(self):
        deps = set(self.last.values()) | set(self.dmas_since)
        for e in self.eng:
            self.ops.append(dict(eng=e, fn=None, deps=set(deps), kind="b", sig=False))
        self.dmas_since = []

    def emit(self):
        nc = self.nc
        ops = self.ops

        def skip(po, o):
            if po["kind"] != "c" or o["kind"] != "c":
                return False
            if po["eng"] == "pe" and o["eng"] == "pe":
                return True
            if (not SAME_ENG_SYNC) and po["eng"] == o["eng"]:
                return True
            return False

        for o in ops:
            for d in o["deps"]:
                po = ops[d]
                if po["kind"] == "c" and not skip(po, o):
                    po["sig"] = True
        sems = {e: self.es.enter_context(nc.semaphore("s_" + e)) for e in self.eng}
        dpool = {}
        for q in ("sp", "pool", "act"):
            n = N_DMA_SEMS if q != "act" else 4
            dpool[q] = dict(sems=[self.es.enter_context(nc.semaphore(f"s_d{q}{i}")) for i in range(n)],
                            cnt=[0] * n, last=[None] * n, nxt=0, n=n)
        cnt = {e: 0 for e in self.eng}
        waited = {e: {} for e in self.eng}
        tok = [None] * len(ops)
        nw = 0

        def wait(e, key, sem, val):
            nonlocal nw
            if waited[e].get(key, 0) >= val:
                return
            self.eng[e].wait_ge(sem, val)
            waited[e][key] = val
            nw += 1

        for oid, o in enumerate(ops):
            e = o["eng"]
            for d in sorted(o["deps"]):
                po = ops[d]
                if skip(po, o):
                    continue
                if tok[d] is None:
                    continue
                key, sem, val = tok[d]
                wait(e, key, sem, val)
            if o["kind"] == "b":
                continue
            if o["kind"] == "d":
                P = dpool[e]
                i = P["nxt"]
                P["nxt"] = (i + 1) % P["n"]
                if P["last"][i] is not None:
                    wait(e, ("d", e, i), P["sems"][i], P["last"][i])
                inst = o["fn"](self.eng[e])
                P["cnt"][i] += 16
                inst.then_inc(P["sems"][i], 16)
                P["last"][i] = P["cnt"][i]
                tok[oid] = (("d", e, i), P["sems"][i], P["cnt"][i])
            else:
                inst = o["fn"](self.eng[e])
                if o["sig"]:
                    cnt[e] += 1
                    inst.then_inc(sems[e], 1)
                    tok[oid] = (e, sems[e], cnt[e])
        for q, P in dpool.items():
            for i in range(P["n"]):
                if P["last"][i] is not None:
                    wait("sp", ("d", q, i), P["sems"][i], P["last"][i])
        return dict(n_ops=len(ops), n_waits=nw, cnt=cnt)


def build(stage_limit=99, dbg=False):
    nc = bass.Bass("TRN2", target_bir_lowering=False)

    def DI(name, shape, dt):
        return nc.dram_tensor(name, list(shape), dt, kind="ExternalInput").ap()

    def DS(name, shape, dt):
        kind = "ExternalOutput" if (dbg or not SCRATCH_INTERNAL) else "Internal"
        return nc.dram_tensor(name, list(shape), dt, kind=kind).ap()

    x = DI("x", [T, D], F32)
    pos = DI("pos", [1, T], I32)
    cst = DI("cst", [128, NCONST], F32)
    w_in = DI("w_in", [D, 6848], F32)
    w_krsw = DI("w_krsw", [D, 64], F32)
    g_attn = DI("g_attn", [1, D], F32)
    g_ffn = DI("g_ffn", [1, D], F32)
    g_fin = DI("g_fin", [1, D], F32)
    lbT = DI("lbT", [128, 16], F32)
    g_hg = DI("g_hg", [128, 1], F32)
    g_q = DI("g_q", [128, 3], F32)
    g_kv = DI("g_kv", [128, 2], F32)
    w_uq = DI("w_uq", [384, 1536], F32)
    w_uqsw = DI("w_uqsw", [384, 512], F32)
    w_ukv = DI("w_ukv", [256, 2048], F32)
    w_bh = DI("w_bh", [D, D], F32)
    w_bm = DI("w_bm", [D, D], F32)
    w_out = DI("w_out", [D, D], F32)
    w_r = DI("w_r", [D, 72], F32)
    b_r = DI("b_r", [1, 72], F32)
    ew1 = DI("ew1", [NEXP, D, 512], F32)
    ew3 = DI("ew3", [NEXP, D, 512], F32)
    ew2 = DI("ew2", [NEXP, 512, D], F32)
    out = nc.dram_tensor("out", [T, D], F32, kind="ExternalOutput").ap()

    oaT = DS("oaT", [D, T], BF16)
    obT = DS("obT", [D, T], BF16)
    x1d = DS("x1d", [T, D], F32)
    xg = DS("xg", [NROWS + 128, D], BF16)
    yb = DS("yb", [NROWS + 128, D], F32)

    w_in_v = w_in.rearrange("(c p) n -> p c n", p=128)

    with contextlib.ExitStack() as es:
        em = Emitter(nc, es)

        def sbt(st, name, shape, dt):
            t = st.enter_context(nc.sbuf_tensor(name, list(shape), dt))
            return TB(t, em.cell(name))

        def ACT(out_, in_, func, r, w, **kw):
            em.op("act", lambda e: e.activation(out=out_, in_=in_, func=func, **kw), r, w)

        def TT(eng, out_, in0, in1, op, r, w):
            em.op(eng, lambda e: e.tensor_tensor(out=out_, in0=in0, in1=in1, op=op), r, w)

        def TS(eng, out_, in0, s1, s2, op0, op1, r, w):
            if op1 is None:
                em.op(eng, lambda e: e.tensor_scalar(out=out_, in0=in0, scalar1=s1, scalar2=None, op0=op0), r, w)
            else:
                em.op(eng, lambda e: e.tensor_scalar(out=out_, in0=in0, scalar1=s1, scalar2=s2, op0=op0, op1=op1), r, w)

        def STT(out_, in0, scalar, in1, op0, op1, r, w):
            em.op("dve", lambda e: e.scalar_tensor_tensor(out=out_, in0=in0, scalar=scalar, in1=in1, op0=op0, op1=op1), r, w)

        def MM(out_, lhsT, rhs, start, stop, r, w):
            em.op("pe", lambda e: e.matmul(out_, lhsT, rhs, start=start, stop=stop), r, w)

        def TR(out_, in_, ident, r, w):
            em.op("pe", lambda e: e.transpose(out_, in_, ident), r, w)

        def CP(eng, out_, in_, r, w):
            if eng == "act":
                em.op("act", lambda e: e.activation(out=out_, in_=in_, func=AF.Copy), r, w)
            else:
                em.op(eng, lambda e: e.tensor_copy(out=out_, in_=in_), r, w)

        def MSET(eng, ap, val, w):
            em.op(eng, lambda e: e.memset(ap, val), [], w)

        def DMA(q, out_, in_, r, w):
            em.op(q, lambda e: e.dma_start(out=out_, in_=in_), r, w, kind="d")

        PS = []
        for i in range(8):
            t = es.enter_context(nc.psum_tensor(f"ps{i}", [128, 512], F32))
            PS.append(TB(t, em.cell(f"ps{i}")))
        rot = [0]

        def nb(n=6):
            b = PS[rot[0] % n]
            rot[0] += 1
            return b

        xnT = sbt(es, "xnT", [128, 8, T], BF16)
        xnT_c = [em.cell(f"xnT{b}") for b in range(T // 512)]
        cb = sbt(es, "cb", [128, 1346 + 2048], BF16)
        cf = sbt(es, "cf", [128, 1346], F32)
        epsT = sbt(es, "epsT", [128, 1], F32)
        dst = sbt(es, "dst", [128, 64], U32)
        wts = sbt(es, "wts", [128, 64], F32)

        DMA("pool", cb.t[:, :], cst[:, :], [], [cb.c])
        DMA("sp", cf.t[:, :], cst[:, 0:1346], [], [cf.c])
        MSET("dve", epsT.t[:, :], EPS, [epsT.c])
        ident_bf = cb.t[:, C_IDENT:C_IDENT + 128]
        ones_bf = cb.t[:, C_ONES:C_ONES + 128]

        def rstd_from(ssq_ap, npart, ncol, scale, tmp, outt, reads):
            ACT(tmp.t[0:npart, 0:ncol], ssq_ap, AF.Ln, reads + [epsT.c], [tmp.c], scale=scale, bias=epsT.t[0:npart, 0:1])
            ACT(outt.t[0:npart, 0:ncol], tmp.t[0:npart, 0:ncol], AF.Exp, [tmp.c], [outt.c], scale=-0.5)

        with contextlib.ExitStack() as st:
            xts = [sbt(st, f"xt{i}", [128, D], F32) for i in range(3)]
            xns = [sbt(st, f"xn{i}", [128, D], BF16) for i in range(2)]
            junk = sbt(st, "junk", [128, D], BF16)
            gA = sbt(st, "gA", [128, D], F32)
            ssq = sbt(st, "ssq", [128, 2], F32)
            lnv = sbt(st, "lnv", [128, 2], F32)
            rstd = sbt(st, "rstd", [128, 2], F32)
            DMA("sp", gA.t[:, :], g_attn[0:1, :].to_broadcast([128, D]), [], [gA.c])
            s0_banks = {}

            def s0_pre(i):
                xt = xts[i % 3]
                xn = xns[i % 2]
                DMA("sp", xt.t[:, :], x[i * 128:(i + 1) * 128, :], [], [xt.c])
                ACT(junk.t[:, :], xt.t[:, :], AF.Square, [xt.c], [junk.c, ssq.c], accum_out=ssq.t[:, 0:1])
                rstd_from(ssq.t[:, 0:1], 128, 1, 1.0 / D, lnv, rstd, [ssq.c])
                STT(xn.t[:, :], xt.t[:, :], rstd.t[:, 0:1], gA.t[:, :], ALU.mult, ALU.mult, [xt.c, rstd.c, gA.c], [xn.c])
                bk = nb()
                bkb = bk.t[:, :].bitcast(BF16)
                for c in range(8):
                    TR(bkb[:, c * 128:(c + 1) * 128], xn.t[:, c * 128:(c + 1) * 128], ident_bf, [xn.c, cb.c], [bk.c])
                s0_banks[i] = bk

            def s0_post(i):
                bk = s0_banks.pop(i)
                bkb = bk.t[:, :].bitcast(BF16)
                CP("act", xnT.t[:, :, i * 128:(i + 1) * 128], bkb.rearrange("p (c t) -> p c t", c=8), [bk.c], [xnT_c[i // 4]])

            s0_pre(0)
            for i in range(T // 128):
                if i + 1 < T // 128:
                    s0_pre(i + 1)
                s0_post(i)
        em.barrier()

        if stage_limit >= 1:
            with contextlib.ExitStack() as st:
                whs = [sbt(st, f"wh{i}", [128, 8, 4, 128], BF16) for i in range(2)]
                lb_sb = sbt(st, "lb_sb", [128, 16], F32)
                oml = sbt(st, "oml", [128, 8], F32)
                ghg = sbt(st, "ghg", [128, 1], F32)
                class NS:
                    pass
                bufs = []
                for bi_ in range(2):
                    B = NS()
                    for nm in ("sq", "kk", "logf", "bcum", "dd", "ek", "eq", "sg", "osb", "lnv", "rs"):
                        setattr(B, nm, sbt(st, f"{nm}{bi_}", [128, 512], F32))
                    for nm in ("kdec", "qx", "osq", "oa"):
                        setattr(B, nm, sbt(st, f"{nm}{bi_}", [128, 512], BF16))
                    B.eb = sbt(st, f"eb{bi_}", [128, 8], F32)
                    B.vtok = sbt(st, f"vtok{bi_}", [128, 4, 128], BF16)
                    B.kdT = sbt(st, f"kdT{bi_}", [128, 4, 128], BF16)
                    B.A = sbt(st, f"A{bi_}", [128, 256], BF16)
                    bufs.append(B)
                S = sbt(st, "S", [128, 128], F32)
                Sds = [sbt(st, f"Sd{i}", [128, 128], BF16) for i in range(8)]
                zt = sbt(st, "zt", [128, 8192], BF16)
                zf = sbt(st, "zf", [128, D], F32)
                MSET("pool", zt.t[:, :], 0.0, [zt.c])
                MSET("pool", zf.t[:, :], 0.0, [zf.c])
                xg_flat = xg.rearrange("(p r) d -> p (r d)", p=128)
                nper = (NROWS + 128) // 128 * D
                for k0 in range(0, nper, 8192):
                    k1 = min(nper, k0 + 8192)
                    DMA("sp", xg_flat[:, k0:k1], zt.t[:, 0:k1 - k0], [zt.c], [])
                DMA("sp", yb[NROWS:NROWS + 128, :], zf.t[:, :], [zf.c], [])
                DMA("sp", lb_sb.t[:, :], lbT[:, :], [], [lb_sb.c])
                DMA("sp", ghg.t[:, :], g_hg[:, :], [], [ghg.c])
                TT("dve", oml.t[:, :], lb_sb.t[:, 8:16], lb_sb.t[:, 0:8], ALU.subtract, [lb_sb.c], [oml.c])
                ACT(oml.t[:, :], oml.t[:, :], AF.Sigmoid, [oml.c], [oml.c])
                PQ, PF_, PG_, PV, PTA, PUe, PUo, PO = PS
                pt_c = PTA.c
                pa_c = PTA.c
                lnc = math.log(128 ** -0.5)
                lncT = sbt(st, "lncT", [128, 1], F32)
                MSET("dve", lncT.t[:, :], lnc, [lncT.c])
                batches = [(h, s_, j) for h in range(8) for s_ in range(2) for j in range(4)]

                def load_w(h):
                    wh = whs[h % 2]
                    for fam in range(4):
                        col = fam * 1024 + h * 128
                        DMA("pool", wh.t[:, :, fam, :], w_in_v[:, :, col:col + 128], [], [wh.c])

                def geo(bi):
                    h, s_, j = batches[bi]
                    return h, s_, j, bufs[bi % 2], whs[h % 2], s_ * SEQ + j * 512

                def front_proj(bi):
                    h, s_, j, B, wh, T0 = geo(bi)
                    xc = xnT_c[T0 // 512]
                    for c in range(8):
                        MM(PQ.t[:, :], wh.t[:, c, 0, :], xnT.t[:, c, T0:T0 + 512], c == 0, c == 7, [wh.c, xc], [PQ.c])
                    for c in range(8):
                        MM(PF_.t[:, :], wh.t[:, c, 1, :], xnT.t[:, c, T0:T0 + 512], c == 0, c == 7, [wh.c, xc], [PF_.c])
                    for c in range(8):
                        MM(PG_.t[:, :], wh.t[:, c, 3, :], xnT.t[:, c, T0:T0 + 512], c == 0, c == 7, [wh.c, xc], [PG_.c])
                    for ti in range(4):
                        for c in range(8):
                            MM(PV.t[:, ti * 128:(ti + 1) * 128], xnT.t[:, c, T0 + ti * 128:T0 + (ti + 1) * 128],
                               wh.t[:, c, 2, :], c == 0, c == 7, [wh.c, xc], [PV.c])
                    ACT(B.kk.t[:, :], PF_.t[:, :], AF.Sigmoid, [PF_.c], [B.kk.c], scale=-1.0)
                    ACT(B.sq.t[:, :], PQ.t[:, :], AF.Silu, [PQ.c], [B.sq.c])
                    ACT(B.sg.t[:, :], PG_.t[:, :], AF.Silu, [PG_.c], [B.sg.c])
                    CP("act", B.vtok.t[:, :, :], PV.t[:, :].rearrange("p (a b) -> p a b", a=4), [PV.c], [B.vtok.c])

                def front_mid(bi):
                    h, s_, j, B, wh, T0 = geo(bi)
                    TS("dve", B.kk.t[:, :], B.kk.t[:, :], oml.t[:, h:h + 1], None, ALU.mult, None, [B.kk.c, oml.c], [B.kk.c])
                    ACT(B.logf.t[:, :], B.kk.t[:, :], AF.Ln, [B.kk.c, cf.c], [B.logf.c], scale=-1.0, bias=cf.t[:, C_ONES:C_ONES + 1])
                    em.op("dve", lambda e: e.tensor_tensor_scan(
                        out=B.bcum.t[:, :], data0=cf.t[:, C_RESET:C_RESET + 512], data1=B.logf.t[:, :],
                        initial=0.0, op0=ALU.mult, op1=ALU.add), [cf.c, B.logf.c], [B.bcum.c])
                    b3 = B.bcum.t[:, :].rearrange("p (a b) -> p a b", a=8)
                    d3 = B.dd.t[:, :].rearrange("p (a b) -> p a b", a=8)
                    TT("dve", d3, b3[:, :, 63:64].to_broadcast([128, 8, 64]), b3, ALU.subtract, [B.bcum.c], [B.dd.c])
                    ACT(B.ek.t[:, :], B.dd.t[:, :], AF.Exp, [B.dd.c], [B.ek.c])
                    ACT(B.eq.t[:, :], B.dd.t[:, :], AF.Exp, [B.dd.c, lncT.c], [B.eq.c], scale=-1.0, bias=lncT.t[:, 0:1])
                    ACT(B.eb.t[:, :], b3[:, :, 63], AF.Exp, [B.bcum.c], [B.eb.c])
                    TT("pool", B.kdec.t[:, :], B.kk.t[:, :], B.ek.t[:, :], ALU.mult, [B.kk.c, B.ek.c], [B.kdec.c])
                    TT("pool", B.qx.t[:, :], B.sq.t[:, :], B.eq.t[:, :], ALU.mult, [B.sq.c, B.eq.c], [B.qx.c])

                def front_end(bi):
                    h, s_, j, B, wh, T0 = geo(bi)
                    ptb = PTA.t[:, :].bitcast(BF16)
                    for ti in range(4):
                        TR(ptb[:, ti * 128:(ti + 1) * 128], B.kdec.t[:, ti * 128:(ti + 1) * 128], ident_bf, [B.kdec.c, cb.c], [pt_c])
                    CP("act", B.kdT.t[:, :, :], ptb[:, 0:512].rearrange("p (a b) -> p a b", a=4), [pt_c], [B.kdT.c])
                    for c8 in range(8):
                        ti, hf = c8 // 2, c8 % 2
                        MM(PTA.t[hf * 64:(hf + 1) * 64, 256 + ti * 64:256 + (ti + 1) * 64], B.kdec.t[:, c8 * 64:(c8 + 1) * 64],
                           B.qx.t[:, c8 * 64:(c8 + 1) * 64], True, True, [B.kdec.c, B.qx.c], [pa_c])
                    TT("dve", B.A.t[:, :], PTA.t[:, 256:512], cb.t[:, C_MASK256:C_MASK256 + 256], ALU.mult, [pa_c, cb.c], [B.A.c])

                def back_u(bi):
                    h, s_, j, B, wh, T0 = geo(bi)
                    for c8 in range(8):
                        ti, hf = c8 // 2, c8 % 2
                        hs = slice(hf * 64, (hf + 1) * 64)
                        ub = PUe if hf == 0 else PUo
                        MM(ub.t[:, ti * 128:(ti + 1) * 128], B.kdT.t[hs, ti, :], B.vtok.t[hs, ti, :], True, True,
                           [B.kdT.c, B.vtok.c], [ub.c])

                def back_chain(bi):
                    h, s_, j, B, wh, T0 = geo(bi)
                    if j == 0:
                        MSET("dve", S.t[:, :], 0.0, [S.c])
                        MSET("dve", Sds[0].t[:, :], 0.0, [Sds[0].c])
                    for c8 in range(8):
                        ti, hf = c8 // 2, c8 % 2
                        first = (j == 0 and c8 == 0)
                        ub = PUe if hf == 0 else PUo
                        if not first:
                            TS("dve", Sds[c8].t[:, :], S.t[:, :], B.eb.t[:, c8:c8 + 1], None, ALU.mult, None, [S.c, B.eb.c], [Sds[c8].c])
                        STT(S.t[:, :], S.t[:, :], B.eb.t[:, c8:c8 + 1], ub.t[:, ti * 128:(ti + 1) * 128], ALU.mult, ALU.add,
                            [S.c, B.eb.c, ub.c], [S.c])
                    for c8 in range(8):
                        ti, hf = c8 // 2, c8 % 2
                        hs = slice(hf * 64, (hf + 1) * 64)
                        MM(PO.t[:, c8 * 64:(c8 + 1) * 64], B.vtok.t[hs, ti, :], B.A.t[hs, ti * 64:(ti + 1) * 64], True, False,
                           [B.vtok.c, B.A.c], [PO.c])
                        MM(PO.t[:, c8 * 64:(c8 + 1) * 64], Sds[c8].t[:, :], B.qx.t[:, c8 * 64:(c8 + 1) * 64], False, True,
                           [Sds[c8].c, B.qx.c], [PO.c])
                    CP("act", B.osb.t[:, :], PO.t[:, :], [PO.c], [B.osb.c])
                    TT("pool", B.osq.t[:, :], B.osb.t[:, :], B.osb.t[:, :], ALU.mult, [B.osb.c], [B.osq.c])
                    MM(PO.t[:, :], ones_bf, B.osq.t[:, :], True, True, [cb.c, B.osq.c], [PO.c])

                def back_norm(bi):
                    h, s_, j, B, wh, T0 = geo(bi)
                    rstd_from(PO.t[:, :], 128, 512, 1.0 / 128, B.lnv, B.rs, [PO.c])
                    TT("dve", B.osb.t[:, :], B.osb.t[:, :], B.rs.t[:, :], ALU.mult, [B.osb.c, B.rs.c], [B.osb.c])
                    STT(B.oa.t[:, :], B.osb.t[:, :], ghg.t[:, 0:1], B.sg.t[:, :], ALU.mult, ALU.mult, [B.osb.c, ghg.c, B.sg.c], [B.oa.c])
                    DMA("sp", oaT[h * 128:(h + 1) * 128, T0:T0 + 512], B.oa.t[:, :], [B.oa.c], [])

                load_w(0)
                front_proj(0)
                front_mid(0)
                front_end(0)
                nb_ = len(batches)
                for bi in range(nb_):
                    h, s_, j = batches[bi]
                    if s_ == 0 and j == 0 and h + 1 < 8:
                        load_w(h + 1)
                    nx = bi + 1 < nb_
                    back_u(bi)
                    if nx:
                        front_proj(bi + 1)
                    back_chain(bi)
                    if nx:
                        front_mid(bi + 1)
                    back_norm(bi)
                    if nx:
                        front_end(bi + 1)
            em.barrier()


        if stage_limit >= 2:
            with contextlib.ExitStack() as st:
                wm = sbt(st, "wm", [128, 8, 768], BF16)
                wuq = sbt(st, "wuq", [128, 3, 1536], BF16)
                wuqs = sbt(st, "wuqs", [128, 3, 512], BF16)
                wukv = sbt(st, "wukv", [128, 2, 2048], BF16)
                gq = sbt(st, "gq", [128, 3], F32)
                gkv = sbt(st, "gkv", [128, 2], F32)
                cos2 = sbt(st, "cos2", [64, T], BF16)
                sin2 = sbt(st, "sin2", [64, T], BF16)
                scl = sbt(st, "scl", [64, 1], F32)
                rope_st = contextlib.ExitStack()
                posi = sbt(rope_st, "posi", [64, 1024], I32)
                ang = sbt(rope_st, "ang", [64, 1024], F32)
                uu = sbt(rope_st, "uu", [64, 1024], F32)
                ui = sbt(rope_st, "ui", [64, 1024], I32)
                uf = sbt(rope_st, "uf", [64, 1024], F32)
                PO, PL = PS[6], PS[7]
                TWO_PI = 2.0 * math.pi
                DMA("pool", wm.t[:, :, 0:704], w_in_v[:, :, 4096:4800], [], [wm.c])
                DMA("pool", wm.t[:, :, 704:768], w_krsw.rearrange("(c p) n -> p c n", p=128), [], [wm.c])
                DMA("pool", wuq.t[:, :, :], w_uq.rearrange("(c p) n -> p c n", p=128), [], [wuq.c])
                DMA("pool", wuqs.t[:, :, :], w_uqsw.rearrange("(c p) n -> p c n", p=128), [], [wuqs.c])
                DMA("pool", wukv.t[:, :, :], w_ukv.rearrange("(c p) n -> p c n", p=128), [], [wukv.c])
                DMA("sp", gq.t[:, :], g_q[:, :], [], [gq.c])
                DMA("sp", gkv.t[:, :], g_kv[:, :], [], [gkv.c])
                TS("dve", scl.t[:, :], cf.t[0:64, C_SGN:C_SGN + 1], TWO_PI * (1.0 - 1e-6), None, ALU.mult, None, [cf.c], [scl.c])
                for blk in range(4):
                    cs = slice(blk * 1024, (blk + 1) * 1024)
                    DMA("sp", posi.t[:, :], pos[0:1, cs].to_broadcast([64, 1024]), [], [posi.c])
                    CP("dve", ang.t[:, :], posi.t[:, :], [posi.c], [ang.c])
                    TS("dve", ang.t[:, :], ang.t[:, :], cf.t[0:64, C_INVF:C_INVF + 1], 1.0 / TWO_PI, ALU.mult, ALU.mult, [ang.c, cf.c], [ang.c])
                    for kind, off in (("sin", 0.0), ("cos", 0.25)):
                        TS("dve", uu.t[:, :], ang.t[:, :], off, None, ALU.add, None, [ang.c], [uu.c])
                        CP("dve", ui.t[:, :], uu.t[:, :], [uu.c], [ui.c])
                        CP("dve", uf.t[:, :], ui.t[:, :], [ui.c], [uf.c])
                        TT("dve", uu.t[:, :], uu.t[:, :], uf.t[:, :], ALU.subtract, [uu.c, uf.c], [uu.c])
                        TS("dve", uf.t[:, :], uu.t[:, :], 0.5, None, ALU.is_gt, None, [uu.c], [uf.c])
                        TT("dve", uu.t[:, :], uu.t[:, :], uf.t[:, :], ALU.subtract, [uu.c, uf.c], [uu.c])
                        TS("dve", uf.t[:, :], uu.t[:, :], -0.5, None, ALU.is_lt, None, [uu.c], [uf.c])
                        TT("dve", uu.t[:, :], uu.t[:, :], uf.t[:, :], ALU.add, [uu.c, uf.c], [uu.c])
                        if kind == "sin":
                            ACT(sin2.t[:, cs], uu.t[:, :], AF.Sin, [uu.c, scl.c], [sin2.c], scale=scl.t[:, 0:1])
                        else:
                            ACT(cos2.t[:, cs], uu.t[:, :], AF.Sin, [uu.c], [cos2.c], scale=TWO_PI * (1.0 - 1e-6))
                em.barrier()
                rope_st.close()
                sqc = [sbt(st, f"sqc{i}", [128, 512], BF16) for i in range(3)]
                lnv = sbt(st, "lnv2", [128, 512], F32)
                rs = sbt(st, "rs2", [128, 512], F32)
                cqn = sbt(st, "cqn", [128, 3, SEQ], BF16)
                ckvn = sbt(st, "ckvn", [128, 2, SEQ], BF16)
                krT = sbt(st, "krT", [64, SEQ], BF16)
                t1 = sbt(st, "t1", [64, 512], F32)
                t2 = sbt(st, "t2", [64, 512], F32)
                KnTs = [sbt(st, f"KnT{i}", [128, SEQ], BF16) for i in range(2)]
                Vhs = [sbt(st, f"Vh{i}", [128, 16, 128], BF16) for i in range(2)]
                qn = sbt(st, "qn", [128, 512], BF16)
                qr = sbt(st, "qr", [64, 512], BF16)
                pts = [sbt(st, f"pt{i}", [128, 512], BF16) for i in range(4)]
                lnl = sbt(st, "lnl", [128, 512], F32)
                rl = sbt(st, "rl", [128, 512], F32)
                obs = [sbt(st, f"ob{i}", [128, 512], BF16) for i in range(2)]
                nbat = 0
                for s in range(2):
                    for j in range(4):
                        T0 = s * SEQ + j * 512
                        L0 = j * 512
                        xc = xnT_c[T0 // 512]
                        for (dst_t, ncc, col0, gt, dim) in ((cqn, 3, 0, gq, 384), (ckvn, 2, 384, gkv, 256)):
                            banks = [nb() for _ in range(ncc)]
                            for cc in range(ncc):
                                for c in range(8):
                                    MM(banks[cc].t[:, :], wm.t[:, c, col0 + cc * 128:col0 + (cc + 1) * 128], xnT.t[:, c, T0:T0 + 512],
                                       c == 0, c == 7, [wm.c, xc], [banks[cc].c])
                            for cc in range(ncc):
                                ACT(sqc[cc].t[:, :], banks[cc].t[:, :], AF.Square, [banks[cc].c], [sqc[cc].c])
                            bs = nb()
                            for cc in range(ncc):
                                MM(bs.t[:, :], ones_bf, sqc[cc].t[:, :], cc == 0, cc == ncc - 1, [cb.c, sqc[cc].c], [bs.c])
                            rstd_from(bs.t[:, :], 128, 512, 1.0 / dim, lnv, rs, [bs.c])
                            for cc in range(ncc):
                                STT(dst_t.t[:, cc, L0:L0 + 512], banks[cc].t[:, :], gt.t[:, cc:cc + 1], rs.t[:, :], ALU.mult, ALU.mult,
                                    [banks[cc].c, gt.c, rs.c], [dst_t.c])
                        bk1, bk2 = nb(), nb()
                        for c in range(8):
                            MM(bk1.t[0:64, :], wm.t[:, c, 640:704], xnT.t[:, c, T0:T0 + 512], c == 0, c == 7, [wm.c, xc], [bk1.c])
                        for c in range(8):
                            MM(bk2.t[0:64, :], wm.t[:, c, 704:768], xnT.t[:, c, T0:T0 + 512], c == 0, c == 7, [wm.c, xc], [bk2.c])
                        TT("dve", t1.t[:, :], bk1.t[0:64, :], cos2.t[:, T0:T0 + 512], ALU.mult, [bk1.c, cos2.c], [t1.c])
                        TT("dve", t2.t[:, :], bk2.t[0:64, :], sin2.t[:, T0:T0 + 512], ALU.mult, [bk2.c, sin2.c], [t2.c])
                        TT("pool", krT.t[:, L0:L0 + 512], t1.t[:, :], t2.t[:, :], ALU.add, [t1.c, t2.c], [krT.c])
                    for h in range(8):
                        KnT = KnTs[h % 2]
                        Vh = Vhs[h % 2]
                        for j in range(4):
                            T0 = s * SEQ + j * 512
                            L0 = j * 512
                            bkk = nb()
                            for c in range(2):
                                MM(bkk.t[:, :], wukv.t[:, c, h * 256:h * 256 + 128], ckvn.t[:, c, L0:L0 + 512], c == 0, c == 1,
                                   [wukv.c, ckvn.c], [bkk.c])
                            CP("act", KnT.t[:, L0:L0 + 512], bkk.t[:, :], [bkk.c], [KnT.c])
                            bv = nb()
                            for ti in range(4):
                                for c in range(2):
                                    MM(bv.t[:, ti * 128:(ti + 1) * 128], ckvn.t[:, c, L0 + ti * 128:L0 + (ti + 1) * 128],
                                       wukv.t[:, c, h * 256 + 128:h * 256 + 256], c == 0, c == 1, [wukv.c, ckvn.c], [bv.c])
                            CP("dve", Vh.t[:, j * 4:(j + 1) * 4, :], bv.t[:, :].rearrange("p (a b) -> p a b", a=4), [bv.c], [Vh.c])
                            bq, bp, bps = nb(), nb(), nb()
                            for c in range(3):
                                MM(bq.t[:, :], wuq.t[:, c, h * 192:h * 192 + 128], cqn.t[:, c, L0:L0 + 512], c == 0, c == 2, [wuq.c, cqn.c], [bq.c])
                            for c in range(3):
                                MM(bp.t[0:64, :], wuq.t[:, c, h * 192 + 128:h * 192 + 192], cqn.t[:, c, L0:L0 + 512], c == 0, c == 2,
                                   [wuq.c, cqn.c], [bp.c])
                            for c in range(3):
                                MM(bps.t[0:64, :], wuqs.t[:, c, h * 64:(h + 1) * 64], cqn.t[:, c, L0:L0 + 512], c == 0, c == 2,
                                   [wuqs.c, cqn.c], [bps.c])
                            CP("act", qn.t[:, :], bq.t[:, :], [bq.c], [qn.c])
                            TT("dve", t1.t[:, :], bp.t[0:64, :], cos2.t[:, T0:T0 + 512], ALU.mult, [bp.c, cos2.c], [t1.c])
                            TT("dve", t2.t[:, :], bps.t[0:64, :], sin2.t[:, T0:T0 + 512], ALU.mult, [bps.c, sin2.c], [t2.c])
                            TT("pool", qr.t[:, :], t1.t[:, :], t2.t[:, :], ALU.add, [t1.c, t2.c], [qr.c])
                            nkt = 4 * j + 4
                            def s_mm(kt):
                                bst = nb()
                                MM(bst.t[:, :], KnT.t[:, kt * 128:(kt + 1) * 128], qn.t[:, :], True, False, [KnT.c, qn.c], [bst.c])
                                MM(bst.t[:, :], krT.t[:, kt * 128:(kt + 1) * 128], qr.t[:, :], False, True, [krT.c, qr.c], [bst.c])
                                return bst
                            pend = [s_mm(0), s_mm(1)]
                            for kt in range(nkt):
                                bst = pend.pop(0)
                                if kt + 2 < nkt:
                                    pend.append(s_mm(kt + 2))
                                pt = pts[kt % 4]
                                ACT(pt.t[:, :], bst.t[:, :], AF.Exp, [bst.c], [pt.c], scale=192.0 ** -0.5)
                                if kt >= 4 * j:
                                    r = kt - 4 * j
                                    TT("dve", pt.t[:, :], pt.t[:, :], cb.t[:, C_AMASK + r * 512:C_AMASK + (r + 1) * 512], ALU.mult,
                                       [pt.c, cb.c], [pt.c])
                                MM(PO.t[:, :], Vh.t[:, kt, :], pt.t[:, :], kt == 0, kt == nkt - 1, [Vh.c, pt.c], [PO.c])
                                MM(PL.t[:, :], ones_bf, pt.t[:, :], kt == 0, kt == nkt - 1, [cb.c, pt.c], [PL.c])
                            ACT(lnl.t[:, :], PL.t[:, :], AF.Ln, [PL.c], [lnl.c])
                            ACT(rl.t[:, :], lnl.t[:, :], AF.Exp, [lnl.c], [rl.c], scale=-1.0)
                            ob = obs[nbat % 2]
                            nbat += 1
                            TT("dve", ob.t[:, :], PO.t[:, :], rl.t[:, :], ALU.mult, [PO.c, rl.c], [ob.c])
                            DMA("sp", obT[h * 128:(h + 1) * 128, T0:T0 + 512], ob.t[:, :], [ob.c], [])
            em.barrier()

        if stage_limit >= 3:
            with contextlib.ExitStack() as st:
                wg = sbt(st, "wg", [128, 8, 2048], BF16)
                wbh = sbt(st, "wbh", [128, 8, D], BF16)
                wbm = sbt(st, "wbm", [128, 8, D], BF16)
                wo = sbt(st, "wo", [128, 8, D], BF16)
                wr = sbt(st, "wr", [128, 8, 72], F32)
                br = sbt(st, "br", [128, 72], F32)
                g2 = sbt(st, "g2", [128, D], F32)
                oabs = [sbt(st, f"oab{i}", [128, 8, 128], BF16) for i in range(2)]
                obbs = [sbt(st, f"obb{i}", [128, 8, 128], BF16) for i in range(2)]
                Ta = sbt(st, "Ta", [128, D], F32)
                Tb = sbt(st, "Tb", [128, D], F32)
                ybf = sbt(st, "ybf", [128, D], BF16)
                yT = sbt(st, "yT", [128, 8, 128], BF16)
                h2bfs = [sbt(st, f"h2bf{i}", [128, D], BF16) for i in range(2)]
                h2T = sbt(st, "h2T", [128, 8, 128], F32)
                ssq = sbt(st, "ssq3", [128, 2], F32)
                lnv = sbt(st, "lnv3", [128, 2], F32)
                rstd = sbt(st, "rstd3", [128, 2], F32)
                lg = sbt(st, "lg", [128, 72], F32)
                g8 = sbt(st, "g8", [128, 8], F32)
                ohg = sbt(st, "ohg", [128, 8], F32)
                ngm = sbt(st, "ngm", [128, 1], F32)
                ex = sbt(st, "ex", [128, 8], F32)
                gs = sbt(st, "gs", [128, 1], F32)
                gw = sbt(st, "gw", [128, 1], F32)
                pen = sbt(st, "pen", [128, 8], F32)
                msk = sbt(st, "msk", [128, 64], F32)
                m8 = sbt(st, "m8", [128, 8], F32)
                i8 = sbt(st, "i8", [128, 8], U32)
                idf = sbt(st, "idf", [128, 2], F32)
                dlt = sbt(st, "dlt", [128, 1], F32)
                ed = sbt(st, "ed", [128, 1], F32)
                den = sbt(st, "den", [128, 1], F32)
                e1 = sbt(st, "e1", [128, 1], F32)
                e2 = sbt(st, "e2", [128, 1], F32)
                oh1 = sbt(st, "oh1", [128, 64], F32)
                oh2 = sbt(st, "oh2", [128, 64], F32)
                oh = sbt(st, "oh", [128, 64], F32)
                tmp64 = sbt(st, "tmp64", [128, 64], F32)
                sl = sbt(st, "sl", [128, 2], F32)
                ovf = sbt(st, "ovf", [128, 2], F32)
                dsf = sbt(st, "dsf", [128, 2], F32)
                base = sbt(st, "base", [1, 64], F32)
                xg_c = em.cell("xg")
                DMA("pool", wg.t[:, :, :], w_in_v[:, :, 4800:6848], [], [wg.c])
                DMA("pool", wbh.t[:, :, :], w_bh.rearrange("(c p) n -> p c n", p=128), [], [wbh.c])
                DMA("pool", wbm.t[:, :, :], w_bm.rearrange("(c p) n -> p c n", p=128), [], [wbm.c])
                DMA("pool", wo.t[:, :, :], w_out.rearrange("(c p) n -> p c n", p=128), [], [wo.c])
                DMA("sp", wr.t[:, :, :], w_r.rearrange("(c p) n -> p c n", p=128), [], [wr.c])
                DMA("sp", br.t[:, :], b_r[0:1, :].to_broadcast([128, 72]), [], [br.c])
                DMA("sp", g2.t[:, :], g_ffn[0:1, :].to_broadcast([128, D]), [], [g2.c])
                MSET("dve", base.t[:, :], 0.0, [base.c])
                oaT_v = oaT.rearrange("(c p) t -> p c t", p=128)
                obT_v = obT.rearrange("(c p) t -> p c t", p=128)
                ident_f = cf.t[:, C_IDENT:C_IDENT + 128]
                iota = cf.t[:, C_IOTA:C_IOTA + 64]
                Tas = [Ta, sbt(st, "Ta1", [128, D], F32)]
                Tbs = [Tb, sbt(st, "Tb1", [128, D], F32)]
                ybfs = [ybf, sbt(st, "ybf1", [128, D], BF16)]
                NT = T // 128

                def f_load(i):
                    tok = slice(i * 128, (i + 1) * 128)
                    DMA("sp", oabs[i % 2].t[:, :, :], oaT_v[:, :, tok], [], [oabs[i % 2].c])
                    DMA("sp", obbs[i % 2].t[:, :, :], obT_v[:, :, tok], [], [obbs[i % 2].c])

                def f_gate(i, which):
                    tok = slice(i * 128, (i + 1) * 128)
                    xc = xnT_c[i // 4]
                    Tt = (Tas if which == 0 else Tbs)[i % 2]
                    goff = 0 if which == 0 else 1024
                    for hf in range(2):
                        b = nb()
                        for c in range(8):
                            MM(b.t[:, :], xnT.t[:, c, tok], wg.t[:, c, goff + hf * 512:goff + (hf + 1) * 512], c == 0, c == 7,
                               [xc, wg.c], [b.c])
                        ACT(Tt.t[:, hf * 512:(hf + 1) * 512], b.t[:, :], AF.Sigmoid, [b.c], [Tt.c])

                def f_branch(i, which):
                    Tt = (Tas if which == 0 else Tbs)[i % 2]
                    src = (oabs if which == 0 else obbs)[i % 2]
                    wb = wbh if which == 0 else wbm
                    for hf in range(2):
                        b = nb()
                        for c in range(8):
                            MM(b.t[:, :], src.t[:, c, :], wb.t[:, c, hf * 512:(hf + 1) * 512], c == 0, c == 7, [src.c, wb.c], [b.c])
                        TT("dve", Tt.t[:, hf * 512:(hf + 1) * 512], b.t[:, :], Tt.t[:, hf * 512:(hf + 1) * 512], ALU.mult, [b.c, Tt.c], [Tt.c])
                    if which == 1:
                        TT("pool", ybfs[i % 2].t[:, :], Tas[i % 2].t[:, :], Tbs[i % 2].t[:, :], ALU.add,
                           [Tas[i % 2].c, Tbs[i % 2].c], [ybfs[i % 2].c])

                def b1(i):
                    tok = slice(i * 128, (i + 1) * 128)
                    yb_, Tb_ = ybfs[i % 2], Tbs[i % 2]
                    bt = nb()
                    btb = bt.t[:, :].bitcast(BF16)
                    for c in range(8):
                        TR(btb[:, c * 128:(c + 1) * 128], yb_.t[:, c * 128:(c + 1) * 128], ident_bf, [yb_.c, cb.c], [bt.c])
                    CP("act", yT.t[:, :, :], btb.rearrange("p (c t) -> p c t", c=8), [bt.c], [yT.c])
                    DMA("sp", Tb_.t[:, :], x[tok, :], [], [Tb_.c])

                def b2(i):
                    tok = slice(i * 128, (i + 1) * 128)
                    Ta_, Tb_, yb_ = Tas[i % 2], Tbs[i % 2], ybfs[i % 2]
                    h2bf = h2bfs[i % 2]
                    for hf in range(2):
                        b = nb()
                        for c in range(8):
                            MM(b.t[:, :], yT.t[:, c, :], wo.t[:, c, hf * 512:(hf + 1) * 512], c == 0, c == 7, [yT.c, wo.c], [b.c])
                        TT("dve", Ta_.t[:, hf * 512:(hf + 1) * 512], b.t[:, :], Tb_.t[:, hf * 512:(hf + 1) * 512], ALU.add, [b.c, Tb_.c], [Ta_.c])
                    DMA("sp", x1d[tok, :], Ta_.t[:, :], [Ta_.c], [])
                    ACT(yb_.t[:, :], Ta_.t[:, :], AF.Square, [Ta_.c], [yb_.c, ssq.c], accum_out=ssq.t[:, 0:1])
                    rstd_from(ssq.t[:, 0:1], 128, 1, 1.0 / D, lnv, rstd, [ssq.c])
                    STT(Tb_.t[:, :], Ta_.t[:, :], rstd.t[:, 0:1], g2.t[:, :], ALU.mult, ALU.mult, [Ta_.c, rstd.c, g2.c], [Tb_.c])
                    CP("pool", h2bf.t[:, :], Tb_.t[:, :], [Tb_.c], [h2bf.c])

                def b3(i):
                    Tb_ = Tbs[i % 2]
                    p1, p2 = nb(), nb()
                    for c in range(8):
                        bb = p1 if c < 4 else p2
                        TR(bb.t[:, (c % 4) * 128:(c % 4 + 1) * 128], Tb_.t[:, c * 128:(c + 1) * 128], ident_f, [Tb_.c, cf.c], [bb.c])
                    CP("act", h2T.t[:, 0:4, :], p1.t[:, :].rearrange("p (c t) -> p c t", c=4), [p1.c], [h2T.c])
                    CP("act", h2T.t[:, 4:8, :], p2.t[:, :].rearrange("p (c t) -> p c t", c=4), [p2.c], [h2T.c])

                def b4(i):
                    bl = nb()
                    for c in range(8):
                        MM(bl.t[:, 0:72], h2T.t[:, c, :], wr.t[:, c, :], c == 0, c == 7, [h2T.c, wr.c], [bl.c])
                    TT("dve", lg.t[:, :], bl.t[:, 0:72], br.t[:, :], ALU.add, [bl.c, br.c], [lg.c])
                    em.op("dve", lambda e: e.max(out=g8.t[:, :], in_=lg.t[:, 0:8]), [lg.c], [g8.c])
                    TS("dve", ohg.t[:, :], lg.t[:, 0:8], g8.t[:, 0:1], None, ALU.is_equal, None, [lg.c, g8.c], [ohg.c])
                    TS("dve", ngm.t[:, :], g8.t[:, 0:1], -1.0, None, ALU.mult, None, [g8.c], [ngm.c])
                    ACT(ex.t[:, :], lg.t[:, 0:8], AF.Exp, [lg.c, ngm.c], [ex.c, gs.c], bias=ngm.t[:, 0:1], accum_out=gs.t[:, 0:1])
                    em.op("dve", lambda e: e.reciprocal(out=gw.t[:, :], in_=gs.t[:, :]), [gs.c], [gw.c])
                    TS("dve", pen.t[:, :], ohg.t[:, :], -1.0, 1e30, ALU.add, ALU.mult, [ohg.c], [pen.c])
                    TT("dve", msk.t[:, :].rearrange("p (a b) -> p a b", a=8), lg.t[:, 8:72].rearrange("p (a b) -> p a b", a=8),
                       pen.t[:, :].rearrange("p (a b) -> p a b", b=1).to_broadcast([128, 8, 8]), ALU.add, [lg.c, pen.c], [msk.c])
                    em.op("dve", lambda e: e.max(out=m8.t[:, :], in_=msk.t[:, :]), [msk.c], [m8.c])
                    em.op("dve", lambda e: e.max_index(out=i8.t[:, :], in_max=m8.t[:, :], in_values=msk.t[:, :]), [m8.c, msk.c], [i8.c])
                    CP("dve", idf.t[:, :], i8.t[:, 0:2], [i8.c], [idf.c])
                    TT("dve", dlt.t[:, :], m8.t[:, 1:2], m8.t[:, 0:1], ALU.subtract, [m8.c], [dlt.c])
                    ACT(ed.t[:, :], dlt.t[:, :], AF.Exp, [dlt.c], [ed.c])
                    TS("dve", den.t[:, :], ed.t[:, :], 1.0, None, ALU.add, None, [ed.c], [den.c])
                    em.op("dve", lambda e: e.reciprocal(out=e1.t[:, :], in_=den.t[:, :]), [den.c], [e1.c])
                    TT("dve", e2.t[:, :], ed.t[:, :], e1.t[:, :], ALU.mult, [ed.c, e1.c], [e2.c])
                    TT("dve", wts.t[:, 2 * i:2 * i + 1], e1.t[:, :], gw.t[:, :], ALU.mult, [e1.c, gw.c], [wts.c])
                    TT("dve", wts.t[:, 2 * i + 1:2 * i + 2], e2.t[:, :], gw.t[:, :], ALU.mult, [e2.c, gw.c], [wts.c])
                    TS("dve", oh1.t[:, :], iota, idf.t[:, 0:1], None, ALU.is_equal, None, [cf.c, idf.c], [oh1.c])
                    TS("dve", oh2.t[:, :], iota, idf.t[:, 1:2], None, ALU.is_equal, None, [cf.c, idf.c], [oh2.c])
                    TT("dve", oh.t[:, :], oh1.t[:, :], oh2.t[:, :], ALU.add, [oh1.c, oh2.c], [oh.c])

                def b5(i):
                    h2bf = h2bfs[i % 2]
                    bp_ = nb()
                    MM(bp_.t[:, 0:64], cf.t[:, C_TRIST:C_TRIST + 128], oh.t[:, :], True, False, [cf.c, oh.c], [bp_.c])
                    MM(bp_.t[:, 0:64], cf.t[0:1, C_ONES:C_ONES + 128], base.t[0:1, :], False, True, [cf.c, base.c], [bp_.c])
                    bc_ = nb()
                    MM(bc_.t[0:1, 0:64], cf.t[:, C_ONES:C_ONES + 1], oh.t[:, :], True, True, [cf.c, oh.c], [bc_.c])
                    TT("dve", tmp64.t[:, :], bp_.t[:, 0:64], oh1.t[:, :], ALU.mult, [bp_.c, oh1.c], [tmp64.c])
                    em.op("dve", lambda e: e.reduce_sum(out=sl.t[:, 0:1], in_=tmp64.t[:, :], axis=mybir.AxisListType.X), [tmp64.c], [sl.c])
                    TT("dve", tmp64.t[:, :], bp_.t[:, 0:64], oh2.t[:, :], ALU.mult, [bp_.c, oh2.c], [tmp64.c])
                    em.op("dve", lambda e: e.reduce_sum(out=sl.t[:, 1:2], in_=tmp64.t[:, :], axis=mybir.AxisListType.X), [tmp64.c], [sl.c])
                    TT("dve", base.t[0:1, :], base.t[0:1, :], bc_.t[0:1, 0:64], ALU.add, [base.c, bc_.c], [base.c])
                    TS("dve", ovf.t[:, :], sl.t[:, :], float(CAP), 1e6, ALU.is_ge, ALU.mult, [sl.c], [ovf.c])
                    STT(dsf.t[:, :], idf.t[:, :], float(CAP), sl.t[:, :], ALU.mult, ALU.add, [idf.c, sl.c], [dsf.c])
                    TT("dve", dsf.t[:, :], dsf.t[:, :], ovf.t[:, :], ALU.add, [dsf.c, ovf.c], [dsf.c])
                    TS("dve", dsf.t[:, :], dsf.t[:, :], float(NROWS), None, ALU.min, None, [dsf.c], [dsf.c])
                    CP("dve", dst.t[:, 2 * i:2 * i + 2], dsf.t[:, :], [dsf.c], [dst.c])
                    for k in range(2):
                        em.op("pool", lambda e, k=k, i=i, h2bf=h2bf: e.indirect_dma_start(
                            out=xg[:, :], out_offset=bass.IndirectOffsetOnAxis(ap=dst.t[:, 2 * i + k:2 * i + k + 1], axis=0),
                            in_=h2bf.t[:, :], in_offset=None), [dst.c, h2bf.c], [xg_c], kind="d")

                f_load(0)
                f_gate(0, 0)
                f_branch(0, 0)
                f_gate(0, 1)
                f_branch(0, 1)
                for i in range(NT):
                    nx = i + 1 < NT
                    if nx:
                        f_load(i + 1)
                    b1(i)
                    if nx:
                        f_gate(i + 1, 0)
                    b2(i)
                    if nx:
                        f_branch(i + 1, 0)
                    b3(i)
                    if nx:
                        f_gate(i + 1, 1)
                    b4(i)
                    if nx:
                        f_branch(i + 1, 1)
                    b5(i)
            em.barrier()

        if stage_limit >= 4:
            with contextlib.ExitStack() as st:
                NR = CAP // 128
                w1s = [sbt(st, f"w1s{i}", [128, 8, 512], BF16) for i in range(3)]
                w3s = [sbt(st, f"w3s{i}", [128, 8, 512], BF16) for i in range(3)]
                w2s = [sbt(st, f"w2s{i}", [128, 4, D], BF16) for i in range(3)]
                xgs = [sbt(st, f"xgs{i}", [128, NR, D], BF16) for i in range(2)]
                xgT = [sbt(st, f"xgT{i}", [128, 8, CAP], BF16) for i in range(2)]
                s1s = [sbt(st, f"s1s{i}", [128, CAP], F32) for i in range(2)]
                hid = [sbt(st, f"hid{i}", [128, 4, CAP], BF16) for i in range(2)]
                ysb = [sbt(st, f"ysb{i}", [128, D], F32) for i in range(2)]
                nsi = [0]

                def load_e(ex_):
                    sl_ = ex_ % 3
                    DMA("pool", w1s[sl_].t[:, :, :], ew1[ex_].rearrange("(c p) n -> p c n", p=128), [], [w1s[sl_].c])
                    DMA("pool", w3s[sl_].t[:, :, :], ew3[ex_].rearrange("(c p) n -> p c n", p=128), [], [w3s[sl_].c])
                    DMA("pool", w2s[sl_].t[:, :, :], ew2[ex_].rearrange("(c p) n -> p c n", p=128), [], [w2s[sl_].c])
                    xs = xgs[ex_ % 2]
                    DMA("sp", xs.t[:, :, :], xg[ex_ * CAP:(ex_ + 1) * CAP, :].rearrange("(r p) d -> p r d", p=128), [], [xs.c])

                def front_e(ex_):
                    sl_ = ex_ % 3
                    xs = xgs[ex_ % 2]
                    xT = xgT[ex_ % 2]
                    hd = hid[ex_ % 2]
                    for r in range(NR):
                        bt = nb()
                        btb = bt.t[:, :].bitcast(BF16)
                        for c in range(8):
                            TR(btb[:, c * 128:(c + 1) * 128], xs.t[:, r, c * 128:(c + 1) * 128], ident_bf, [xs.c, cb.c], [bt.c])
                        CP("act" if r == 0 else "dve", xT.t[:, :, r * 128:(r + 1) * 128], btb.rearrange("p (c t) -> p c t", c=8), [bt.c], [xT.c])
                    for fc in range(4):
                        b1, b3 = nb(), nb()
                        for c in range(8):
                            MM(b1.t[:, 0:CAP], w1s[sl_].t[:, c, fc * 128:(fc + 1) * 128], xT.t[:, c, :], c == 0, c == 7, [w1s[sl_].c, xT.c], [b1.c])
                        for c in range(8):
                            MM(b3.t[:, 0:CAP], w3s[sl_].t[:, c, fc * 128:(fc + 1) * 128], xT.t[:, c, :], c == 0, c == 7, [w3s[sl_].c, xT.c], [b3.c])
                        s1 = s1s[nsi[0] % 2]
                        nsi[0] += 1
                        ACT(s1.t[:, :], b1.t[:, 0:CAP], AF.Silu, [b1.c], [s1.c])
                        TT("dve", hd.t[:, fc, :], b3.t[:, 0:CAP], s1.t[:, :], ALU.mult, [b3.c, s1.c], [hd.c])

                def back_e(ex_):
                    sl_ = ex_ % 3
                    hd = hid[ex_ % 2]
                    for r in range(NR):
                        ys = ysb[r % 2]
                        for hf in range(2):
                            b = PS[6 + hf]
                            for fc in range(4):
                                MM(b.t[:, :], hd.t[:, fc, r * 128:(r + 1) * 128], w2s[sl_].t[:, fc, hf * 512:(hf + 1) * 512], fc == 0, fc == 3,
                                   [hd.c, w2s[sl_].c], [b.c])
                            CP("act" if hf == 0 else "dve", ys.t[:, hf * 512:(hf + 1) * 512], b.t[:, :], [b.c], [ys.c])
                        DMA("sp", yb[ex_ * CAP + r * 128:ex_ * CAP + (r + 1) * 128, :], ys.t[:, :], [ys.c], [])

                load_e(0)
                load_e(1)
                front_e(0)
                for ex_ in range(NEXP):
                    if ex_ + 2 < NEXP:
                        load_e(ex_ + 2)
                    if ex_ + 1 < NEXP:
                        front_e(ex_ + 1)
                    back_e(ex_)
            em.barrier()

        if stage_limit >= 5:
            with contextlib.ExitStack() as st:
                y0s = [sbt(st, f"y0s{i}", [128, D], F32) for i in range(4)]
                y1s = [sbt(st, f"y1s{i}", [128, D], F32) for i in range(4)]
                x1s = [sbt(st, f"x1s{i}", [128, D], F32) for i in range(4)]
                accs = [sbt(st, f"acc{i}", [128, D], F32) for i in range(4)]
                junk = sbt(st, "junk5", [128, D], BF16)
                gF = sbt(st, "gF", [128, D], F32)
                ssq = sbt(st, "ssq5", [128, 2], F32)
                lnv = sbt(st, "lnv5", [128, 2], F32)
                rstd = sbt(st, "rstd5", [128, 2], F32)
                DMA("sp", gF.t[:, :], g_fin[0:1, :].to_broadcast([128, D]), [], [gF.c])
                ssqs = [sbt(st, f"ssq5_{i}", [128, 2], F32) for i in range(2)]
                lnvs = [sbt(st, f"lnv5_{i}", [128, 2], F32) for i in range(2)]
                rstds = [sbt(st, f"rstd5_{i}", [128, 2], F32) for i in range(2)]

                def s5_pre(i):
                    tok = slice(i * 128, (i + 1) * 128)
                    y0, y1, x1t, acc = y0s[i % 4], y1s[i % 4], x1s[i % 4], accs[i % 4]
                    for k, yt in ((0, y0), (1, y1)):
                        em.op("pool", lambda e, k=k, i=i, yt=yt: e.indirect_dma_start(
                            out=yt.t[:, :], out_offset=None, in_=yb[:, :],
                            in_offset=bass.IndirectOffsetOnAxis(ap=dst.t[:, 2 * i + k:2 * i + k + 1], axis=0)), [dst.c], [yt.c], kind="d")
                    DMA("sp", x1t.t[:, :], x1d[tok, :], [], [x1t.c])
                    STT(acc.t[:, :], y0.t[:, :], wts.t[:, 2 * i:2 * i + 1], x1t.t[:, :], ALU.mult, ALU.add, [y0.c, wts.c, x1t.c], [acc.c])
                    STT(acc.t[:, :], y1.t[:, :], wts.t[:, 2 * i + 1:2 * i + 2], acc.t[:, :], ALU.mult, ALU.add, [y1.c, wts.c, acc.c], [acc.c])
                    sq_, ln_, rs_ = ssqs[i % 2], lnvs[i % 2], rstds[i % 2]
                    ACT(junk.t[:, :], acc.t[:, :], AF.Square, [acc.c], [junk.c, sq_.c], accum_out=sq_.t[:, 0:1])
                    rstd_from(sq_.t[:, 0:1], 128, 1, 1.0 / D, ln_, rs_, [sq_.c])

                def s5_post(i):
                    tok = slice(i * 128, (i + 1) * 128)
                    acc = accs[i % 4]
                    rs_ = rstds[i % 2]
                    STT(acc.t[:, :], acc.t[:, :], rs_.t[:, 0:1], gF.t[:, :], ALU.mult, ALU.mult, [acc.c, rs_.c, gF.c], [acc.c])
                    DMA("sp", out[tok, :], acc.t[:, :], [acc.c], [])

                s5_pre(0)
                for i in range(T // 128):
                    if i + 1 < T // 128:
                        s5_pre(i + 1)
                    s5_post(i)

        stats = em.emit()
    return nc, stats


_CACHE = {}


def prep_inputs(inputs):
    f = lambda a: np.ascontiguousarray(np.asarray(a, dtype=np.float32))

    def pc(w, nch):
        e, r, n = w.shape
        return np.ascontiguousarray(w.reshape(e, nch, 128, n).transpose(0, 2, 1, 3)).reshape(e, 128, nch * n)
    x = f(inputs["x"])
    positions = np.asarray(inputs["positions"]).astype(np.int32)
    w_in = f(inputs["w_in"][0])
    kr0 = 4096 + 384 + 256
    w_krsw = np.ascontiguousarray(np.concatenate([w_in[:, kr0 + 32:kr0 + 64], w_in[:, kr0:kr0 + 32]], axis=1))
    hl = f(inputs["hg_lower_bound"])
    lbT = np.ascontiguousarray(hl.reshape(2, 8, 128).transpose(2, 0, 1).reshape(128, 16))
    w_uq = f(inputs["mla_w_uq"][0])
    uq3 = w_uq.reshape(384, 8, 192)
    w_uqsw = np.ascontiguousarray(np.concatenate([uq3[:, :, 160:192], uq3[:, :, 128:160]], axis=2).reshape(384, 512))
    shared = {
        "cst": make_consts(),
        "w_in": w_in,
        "w_krsw": w_krsw,
        "g_attn": f(inputs["attn_norm_w"]).reshape(1, D),
        "g_ffn": f(inputs["ffn_norm_w"]).reshape(1, D),
        "g_fin": f(inputs["final_norm_w"]).reshape(1, D),
        "lbT": lbT,
        "g_hg": f(inputs["hg_out_norm_w"]).reshape(128, 1),
        "g_q": np.ascontiguousarray(f(inputs["mla_q_norm_w"]).reshape(3, 128).T),
        "g_kv": np.ascontiguousarray(f(inputs["mla_kv_norm_w"]).reshape(2, 128).T),
        "w_uq": w_uq,
        "w_uqsw": w_uqsw,
        "w_ukv": f(inputs["mla_w_ukv"][0]),
        "w_bh": f(inputs["w_branch_hgrn"][0]),
        "w_bm": f(inputs["w_branch_mla"][0]),
        "w_out": f(inputs["w_out"][0]),
        "w_r": np.ascontiguousarray(np.concatenate([f(inputs["router_group_w"][0]), f(inputs["router_expert_w"][0])], axis=1)),
        "b_r": np.ascontiguousarray(np.concatenate([f(inputs["router_group_b"][0]), f(inputs["router_expert_b"][0])]).reshape(1, 72)),
        "ew1": f(inputs["expert_w1"][0]),
        "ew3": f(inputs["expert_w3"][0]),
        "ew2": f(inputs["expert_w2"][0]),
    }
    in_maps = []
    for c in range(NCORES):
        m = dict(shared)
        m["x"] = np.ascontiguousarray(x[2 * c:2 * c + 2].reshape(T, D))
        m["pos"] = np.ascontiguousarray(positions[2 * c:2 * c + 2].reshape(1, T))
        in_maps.append(m)
    return in_maps


def kernel(**inputs):
    if "nc" not in _CACHE:
        _CACHE["nc"] = build()[0]
    nc = _CACHE["nc"]
    in_maps = prep_inputs(inputs)
    res = run_bass_kernel_spmd(nc, in_maps, core_ids=list(range(NCORES)))
    outs = [np.asarray(r["out"]).reshape(2, SEQ, D) for r in res.results]
    return np.concatenate(outs, axis=0).astype(np.float32)
```

```python
import math
import contextlib
import numpy as np
import concourse.bass as bass
import concourse.mybir as mybir
from concourse.bass_utils import run_bass_kernel_spmd

F32 = mybir.dt.float32
BF16 = mybir.dt.bfloat16
I32 = mybir.dt.int32
U32 = mybir.dt.uint32
AF = mybir.ActivationFunctionType
ALU = mybir.AluOpType

NCORES = 8
T = 4096
SEQ = 2048
D = 1024
EPS = 1e-6
CAP = 256
NEXP = 64
NROWS = NEXP * CAP
SAME_ENG_SYNC = True
N_DMA_SEMS = 40
SCRATCH_INTERNAL = True

C_IDENT = 0
C_TRILE = 128
C_TRIST = 256
C_MASK256 = 384
C_RESET = 640
C_IOTA = 1152
C_INVF = 1216
C_SGN = 1217
C_ONES = 1218
C_AMASK = 1346
NCONST = C_AMASK + 2048


def make_consts():
    c = np.zeros((128, NCONST), np.float32)
    p = np.arange(128)[:, None]
    f = np.arange(128)[None, :]
    c[:, C_IDENT:C_IDENT + 128] = (p == f)
    c[:, C_TRILE:C_TRILE + 128] = (p <= f)
    c[:, C_TRIST:C_TRIST + 128] = (p < f)
    f256 = np.arange(256)[None, :]
    c[:, C_MASK256:C_MASK256 + 256] = ((p % 64) <= (f256 % 64))
    f512 = np.arange(512)[None, :]
    c[:, C_RESET:C_RESET + 512] = ((f512 % 64) != 0)
    c[:, C_IOTA:C_IOTA + 64] = np.arange(64)[None, :]
    half = 32
    inv_freq = (10000.0 ** (-np.arange(half, dtype=np.float32) / half)).astype(np.float32)
    c[:, C_INVF] = inv_freq[np.arange(128) % 32]
    c[:, C_SGN] = np.where((np.arange(128) % 64) < 32, -1.0, 1.0)
    c[:, C_ONES:C_ONES + 128] = 1.0
    for r in range(4):
        c[:, C_AMASK + r * 512:C_AMASK + (r + 1) * 512] = ((p + r * 128) <= f512)
    return c


class Cell:
    __slots__ = ("name", "w", "r")

    def __init__(self, name):
        self.name = name
        self.w = None
        self.r = []


class TB:
    def __init__(self, t, c):
        self.t = t
        self.c = c


class Emitter:
    def __init__(self, nc, es):
        self.nc = nc
        self.es = es
        self.eng = {"pe": nc.tensor, "act": nc.scalar, "dve": nc.vector, "pool": nc.gpsimd, "sp": nc.sync}
        self.ops = []
        self.last = {}
        self.dmas_since = []

    def cell(self, name="c"):
        return Cell(name)

    def op(self, eng, fn, reads=(), writes=(), kind="c"):
        oid = len(self.ops)
        deps = set()
        for c in reads:
            if c.w is not None:
                deps.add(c.w)
        for c in writes:
            if c.w is not None:
                deps.add(c.w)
            deps.update(c.r)
        self.ops.append(dict(eng=eng, fn=fn, deps=deps, kind=kind, sig=False))
        for c in reads:
            if kind == "c":
                c.r = [q for q in c.r if not (self.ops[q]["kind"] == "c" and self.ops[q]["eng"] == eng)]
            c.r.append(oid)
        for c in writes:
            c.w = oid
            c.r = []
        if kind == "d":
            self.dmas_since.append(oid)
        else:
            self.last[eng] = oid
        return oid

    def barrier(self):
        deps = set(self.last.values()) | set(self.dmas_since)
        for e in self.eng:
            self.ops.append(dict(eng=e, fn=None, deps=set(deps), kind="b", sig=False))
        self.dmas_since = []

    def emit(self):
        nc = self.nc
        ops = self.ops

        def skip(po, o):
            if po["kind"] != "c" or o["kind"] != "c":
                return False
            if po["eng"] == "pe" and o["eng"] == "pe":
                return True
            if (not SAME_ENG_SYNC) and po["eng"] == o["eng"]:
                return True
            return False

        for o in ops:
            for d in o["deps"]:
                po = ops[d]
                if po["kind"] == "c" and not skip(po, o):
                    po["sig"] = True
        sems = {e: self.es.enter_context(nc.semaphore("s_" + e)) for e in self.eng}
        dpool = {}
        for q in ("sp", "pool", "act"):
            n = N_DMA_SEMS if q != "act" else 4
            dpool[q] = dict(sems=[self.es.enter_context(nc.semaphore(f"s_d{q}{i}")) for i in range(n)],
                            cnt=[0] * n, last=[None] * n, nxt=0, n=n)
        cnt = {e: 0 for e in self.eng}
        waited = {e: {} for e in self.eng}
        tok = [None] * len(ops)
        nw = 0

        def wait(e, key, sem, val):
            nonlocal nw
            if waited[e].get(key, 0) >= val:
                return
            self.eng[e].wait_ge(sem, val)
            waited[e][key] = val
            nw += 1

        for oid, o in enumerate(ops):
            e = o["eng"]
            for d in sorted(o["deps"]):
                po = ops[d]
                if skip(po, o):
                    continue
                if tok[d] is None:
                    continue
                key, sem, val = tok[d]
                wait(e, key, sem, val)
            if o["kind"] == "b":
                continue
            if o["kind"] == "d":
                P = dpool[e]
                i = P["nxt"]
                P["nxt"] = (i + 1) % P["n"]
                if P["last"][i] is not None:
                    wait(e, ("d", e, i), P["sems"][i], P["last"][i])
                inst = o["fn"](self.eng[e])
                P["cnt"][i] += 16
                inst.then_inc(P["sems"][i], 16)
                P["last"][i] = P["cnt"][i]
                tok[oid] = (("d", e, i), P["sems"][i], P["cnt"][i])
            else:
                inst = o["fn"](self.eng[e])
                if o["sig"]:
                    cnt[e] += 1
                    inst.then_inc(sems[e], 1)
                    tok[oid] = (e, sems[e], cnt[e])
        for q, P in dpool.items():
            for i in range(P["n"]):
                if P["last"][i] is not None:
                    wait("sp", ("d", q, i), P["sems"][i], P["last"][i])
        return dict(n_ops=len(ops), n_waits=nw, cnt=cnt)


def build(stage_limit=99, dbg=False):
    nc = bass.Bass("TRN2", target_bir_lowering=False)

    def DI(name, shape, dt):
        return nc.dram_tensor(name, list(shape), dt, kind="ExternalInput").ap()

    def DS(name, shape, dt):
        kind = "ExternalOutput" if (dbg or not SCRATCH_INTERNAL) else "Internal"
        return nc.dram_tensor(name, list(shape), dt, kind=kind).ap()

    x = DI("x", [T, D], F32)
    pos = DI("pos", [1, T], I32)
    cst = DI("cst", [128, NCONST], F32)
    w_in = DI("w_in", [D, 6848], F32)
    w_krsw = DI("w_krsw", [D, 64], F32)
    g_attn = DI("g_attn", [1, D], F32)
    g_ffn = DI("g_ffn", [1, D], F32)
    g_fin = DI("g_fin", [1, D], F32)
    lbT = DI("lbT", [128, 16], F32)
    g_hg = DI("g_hg", [128, 1], F32)
    g_q = DI("g_q", [128, 3], F32)
    g_kv = DI("g_kv", [128, 2], F32)
    w_uq = DI("w_uq", [384, 1536], F32)
    w_uqsw = DI("w_uqsw", [384, 512], F32)
    w_ukv = DI("w_ukv", [256, 2048], F32)
    w_bh = DI("w_bh", [D, D], F32)
    w_bm = DI("w_bm", [D, D], F32)
    w_out = DI("w_out", [D, D], F32)
    w_r = DI("w_r", [D, 72], F32)
    b_r = DI("b_r", [1, 72], F32)
    ew1 = DI("ew1", [NEXP, D, 512], F32)
    ew3 = DI("ew3", [NEXP, D, 512], F32)
    ew2 = DI("ew2", [NEXP, 512, D], F32)
    out = nc.dram_tensor("out", [T, D], F32, kind="ExternalOutput").ap()

    oaT = DS("oaT", [D, T], BF16)
    obT = DS("obT", [D, T], BF16)
    x1d = DS("x1d", [T, D], F32)
    xg = DS("xg", [NROWS + 128, D], BF16)
    yb = DS("yb", [NROWS + 128, D], F32)

    w_in_v = w_in.rearrange("(c p) n -> p c n", p=128)

    with contextlib.ExitStack() as es:
        em = Emitter(nc, es)

        def sbt(st, name, shape, dt):
            t = st.enter_context(nc.sbuf_tensor(name, list(shape), dt))
            return TB(t, em.cell(name))

        def ACT(out_, in_, func, r, w, **kw):
            em.op("act", lambda e: e.activation(out=out_, in_=in_, func=func, **kw), r, w)

        def TT(eng, out_, in0, in1, op, r, w):
            em.op(eng, lambda e: e.tensor_tensor(out=out_, in0=in0, in1=in1, op=op), r, w)

        def TS(eng, out_, in0, s1, s2, op0, op1, r, w):
            if op1 is None:
                em.op(eng, lambda e: e.tensor_scalar(out=out_, in0=in0, scalar1=s1, scalar2=None, op0=op0), r, w)
            else:
                em.op(eng, lambda e: e.tensor_scalar(out=out_, in0=in0, scalar1=s1, scalar2=s2, op0=op0, op1=op1), r, w)

        def STT(out_, in0, scalar, in1, op0, op1, r, w):
            em.op("dve", lambda e: e.scalar_tensor_tensor(out=out_, in0=in0, scalar=scalar, in1=in1, op0=op0, op1=op1), r, w)

        def MM(out_, lhsT, rhs, start, stop, r, w):
            em.op("pe", lambda e: e.matmul(out_, lhsT, rhs, start=start, stop=stop), r, w)

        def TR(out_, in_, ident, r, w):
            em.op("pe", lambda e: e.transpose(out_, in_, ident), r, w)

        def CP(eng, out_, in_, r, w):
            if eng == "act":
                em.op("act", lambda e: e.activation(out=out_, in_=in_, func=AF.Copy), r, w)
            else:
                em.op(eng, lambda e: e.tensor_copy(out=out_, in_=in_), r, w)

        def MSET(eng, ap, val, w):
            em.op(eng, lambda e: e.memset(ap, val), [], w)

        def DMA(q, out_, in_, r, w):
            em.op(q, lambda e: e.dma_start(out=out_, in_=in_), r, w, kind="d")

        PS = []
        for i in range(8):
            t = es.enter_context(nc.psum_tensor(f"ps{i}", [128, 512], F32))
            PS.append(TB(t, em.cell(f"ps{i}")))
        rot = [0]

        def nb(n=6):
            b = PS[rot[0] % n]
            rot[0] += 1
            return b

        xnT = sbt(es, "xnT", [128, 8, T], BF16)
        xnT_c = [em.cell(f"xnT{b}") for b in range(T // 512)]
        cb = sbt(es, "cb", [128, 1346 + 2048], BF16)
        cf = sbt(es, "cf", [128, 1346], F32)
        epsT = sbt(es, "epsT", [128, 1], F32)
        dst = sbt(es, "dst", [128, 64], U32)
        wts = sbt(es, "wts", [128, 64], F32)

        DMA("pool", cb.t[:, :], cst[:, :], [], [cb.c])
        DMA("sp", cf.t[:, :], cst[:, 0:1346], [], [cf.c])
        MSET("dve", epsT.t[:, :], EPS, [epsT.c])
        ident_bf = cb.t[:, C_IDENT:C_IDENT + 128]
        ones_bf = cb.t[:, C_ONES:C_ONES + 128]

        def rstd_from(ssq_ap, npart, ncol, scale, tmp, outt, reads):
            ACT(tmp.t[0:npart, 0:ncol], ssq_ap, AF.Ln, reads + [epsT.c], [tmp.c], scale=scale, bias=epsT.t[0:npart, 0:1])
            ACT(outt.t[0:npart, 0:ncol], tmp.t[0:npart, 0:ncol], AF.Exp, [tmp.c], [outt.c], scale=-0.5)

        with contextlib.ExitStack() as st:
            xts = [sbt(st, f"xt{i}", [128, D], F32) for i in range(3)]
            xns = [sbt(st, f"xn{i}", [128, D], BF16) for i in range(2)]
            junk = sbt(st, "junk", [128, D], BF16)
            gA = sbt(st, "gA", [128, D], F32)
            ssq = sbt(st, "ssq", [128, 2], F32)
            lnv = sbt(st, "lnv", [128, 2], F32)
            rstd = sbt(st, "rstd", [128, 2], F32)
            DMA("sp", gA.t[:, :], g_attn[0:1, :].to_broadcast([128, D]), [], [gA.c])
            s0_banks = {}

            def s0_pre(i):
                xt = xts[i % 3]
                xn = xns[i % 2]
                DMA("sp", xt.t[:, :], x[i * 128:(i + 1) * 128, :], [], [xt.c])
                ACT(junk.t[:, :], xt.t[:, :], AF.Square, [xt.c], [junk.c, ssq.c], accum_out=ssq.t[:, 0:1])
                rstd_from(ssq.t[:, 0:1], 128, 1, 1.0 / D, lnv, rstd, [ssq.c])
                STT(xn.t[:, :], xt.t[:, :], rstd.t[:, 0:1], gA.t[:, :], ALU.mult, ALU.mult, [xt.c, rstd.c, gA.c], [xn.c])
                bk = nb()
                bkb = bk.t[:, :].bitcast(BF16)
                for c in range(8):
                    TR(bkb[:, c * 128:(c + 1) * 128], xn.t[:, c * 128:(c + 1) * 128], ident_bf, [xn.c, cb.c], [bk.c])
                s0_banks[i] = bk

            def s0_post(i):
                bk = s0_banks.pop(i)
                bkb = bk.t[:, :].bitcast(BF16)
                CP("act", xnT.t[:, :, i * 128:(i + 1) * 128], bkb.rearrange("p (c t) -> p c t", c=8), [bk.c], [xnT_c[i // 4]])

            s0_pre(0)
            for i in range(T // 128):
                if i + 1 < T // 128:
                    s0_pre(i + 1)
                s0_post(i)
        em.barrier()

        if stage_limit >= 1:
            with contextlib.ExitStack() as st:
                whs = [sbt(st, f"wh{i}", [128, 8, 4, 128], BF16) for i in range(2)]
                lb_sb = sbt(st, "lb_sb", [128, 16], F32)
                oml = sbt(st, "oml", [128, 8], F32)
                ghg = sbt(st, "ghg", [128, 1], F32)
                class NS:
                    pass
                bufs = []
                for bi_ in range(2):
                    B = NS()
                    for nm in ("sq", "kk", "logf", "bcum", "dd", "ek", "eq", "sg", "osb", "lnv", "rs"):
                        setattr(B, nm, sbt(st, f"{nm}{bi_}", [128, 512], F32))
                    for nm in ("kdec", "qx", "osq", "oa"):
                        setattr(B, nm, sbt(st, f"{nm}{bi_}", [128, 512], BF16))
                    B.eb = sbt(st, f"eb{bi_}", [128, 8], F32)
                    B.vtok = sbt(st, f"vtok{bi_}", [128, 4, 128], BF16)
                    B.kdT = sbt(st, f"kdT{bi_}", [128, 4, 128], BF16)
                    B.A = sbt(st, f"A{bi_}", [128, 256], BF16)
                    bufs.append(B)
                S = sbt(st, "S", [128, 128], F32)
                Sds = [sbt(st, f"Sd{i}", [128, 128], BF16) for i in range(8)]
                zt = sbt(st, "zt", [128, 8192], BF16)
                zf = sbt(st, "zf", [128, D], F32)
                MSET("pool", zt.t[:, :], 0.0, [zt.c])
                MSET("pool", zf.t[:, :], 0.0, [zf.c])
                xg_flat = xg.rearrange("(p r) d -> p (r d)", p=128)
                nper = (NROWS + 128) // 128 * D
                for k0 in range(0, nper, 8192):
                    k1 = min(nper, k0 + 8192)
                    DMA("sp", xg_flat[:, k0:k1], zt.t[:, 0:k1 - k0], [zt.c], [])
                DMA("sp", yb[NROWS:NROWS + 128, :], zf.t[:, :], [zf.c], [])
                DMA("sp", lb_sb.t[:, :], lbT[:, :], [], [lb_sb.c])
                DMA("sp", ghg.t[:, :], g_hg[:, :], [], [ghg.c])
                TT("dve", oml.t[:, :], lb_sb.t[:, 8:16], lb_sb.t[:, 0:8], ALU.subtract, [lb_sb.c], [oml.c])
                ACT(oml.t[:, :], oml.t[:, :], AF.Sigmoid, [oml.c], [oml.c])
                PQ, PF_, PG_, PV, PTA, PUe, PUo, PO = PS
                pt_c = PTA.c
                pa_c = PTA.c
                lnc = math.log(128 ** -0.5)
                lncT = sbt(st, "lncT", [128, 1], F32)
                MSET("dve", lncT.t[:, :], lnc, [lncT.c])
                batches = [(h, s_, j) for h in range(8) for s_ in range(2) for j in range(4)]

                def load_w(h):
                    wh = whs[h % 2]
                    for fam in range(4):
                        col = fam * 1024 + h * 128
                        DMA("pool", wh.t[:, :, fam, :], w_in_v[:, :, col:col + 128], [], [wh.c])

                def geo(bi):
                    h, s_, j = batches[bi]
                    return h, s_, j, bufs[bi % 2], whs[h % 2], s_ * SEQ + j * 512

                def front_proj(bi):
                    h, s_, j, B, wh, T0 = geo(bi)
                    xc = xnT_c[T0 // 512]
                    for c in range(8):
                        MM(PQ.t[:, :], wh.t[:, c, 0, :], xnT.t[:, c, T0:T0 + 512], c == 0, c == 7, [wh.c, xc], [PQ.c])
                    for c in range(8):
                        MM(PF_.t[:, :], wh.t[:, c, 1, :], xnT.t[:, c, T0:T0 + 512], c == 0, c == 7, [wh.c, xc], [PF_.c])
                    for c in range(8):
                        MM(PG_.t[:, :], wh.t[:, c, 3, :], xnT.t[:, c, T0:T0 + 512], c == 0, c == 7, [wh.c, xc], [PG_.c])
                    for ti in range(4):
                        for c in range(8):
                            MM(PV.t[:, ti * 128:(ti + 1) * 128], xnT.t[:, c, T0 + ti * 128:T0 + (ti + 1) * 128],
                               wh.t[:, c, 2, :], c == 0, c == 7, [wh.c, xc], [PV.c])
                    ACT(B.kk.t[:, :], PF_.t[:, :], AF.Sigmoid, [PF_.c], [B.kk.c], scale=-1.0)
                    ACT(B.sq.t[:, :], PQ.t[:, :], AF.Silu, [PQ.c], [B.sq.c])
                    ACT(B.sg.t[:, :], PG_.t[:, :], AF.Silu, [PG_.c], [B.sg.c])
                    CP("act", B.vtok.t[:, :, :], PV.t[:, :].rearrange("p (a b) -> p a b", a=4), [PV.c], [B.vtok.c])

                def front_mid(bi):
                    h, s_, j, B, wh, T0 = geo(bi)
                    TS("dve", B.kk.t[:, :], B.kk.t[:, :], oml.t[:, h:h + 1], None, ALU.mult, None, [B.kk.c, oml.c], [B.kk.c])
                    ACT(B.logf.t[:, :], B.kk.t[:, :], AF.Ln, [B.kk.c, cf.c], [B.logf.c], scale=-1.0, bias=cf.t[:, C_ONES:C_ONES + 1])
                    em.op("dve", lambda e: e.tensor_tensor_scan(
                        out=B.bcum.t[:, :], data0=cf.t[:, C_RESET:C_RESET + 512], data1=B.logf.t[:, :],
                        initial=0.0, op0=ALU.mult, op1=ALU.add), [cf.c, B.logf.c], [B.bcum.c])
                    b3 = B.bcum.t[:, :].rearrange("p (a b) -> p a b", a=8)
                    d3 = B.dd.t[:, :].rearrange("p (a b) -> p a b", a=8)
                    TT("dve", d3, b3[:, :, 63:64].to_broadcast([128, 8, 64]), b3, ALU.subtract, [B.bcum.c], [B.dd.c])
                    ACT(B.ek.t[:, :], B.dd.t[:, :], AF.Exp, [B.dd.c], [B.ek.c])
                    ACT(B.eq.t[:, :], B.dd.t[:, :], AF.Exp, [B.dd.c, lncT.c], [B.eq.c], scale=-1.0, bias=lncT.t[:, 0:1])
                    ACT(B.eb.t[:, :], b3[:, :, 63], AF.Exp, [B.bcum.c], [B.eb.c])
                    TT("pool", B.kdec.t[:, :], B.kk.t[:, :], B.ek.t[:, :], ALU.mult, [B.kk.c, B.ek.c], [B.kdec.c])
                    TT("pool", B.qx.t[:, :], B.sq.t[:, :], B.eq.t[:, :], ALU.mult, [B.sq.c, B.eq.c], [B.qx.c])

                def front_end(bi):
                    h, s_, j, B, wh, T0 = geo(bi)
                    ptb = PTA.t[:, :].bitcast(BF16)
                    for ti in range(4):
                        TR(ptb[:, ti * 128:(ti + 1) * 128], B.kdec.t[:, ti * 128:(ti + 1) * 128], ident_bf, [B.kdec.c, cb.c], [pt_c])
                    CP("act", B.kdT.t[:, :, :], ptb[:, 0:512].rearrange("p (a b) -> p a b", a=4), [pt_c], [B.kdT.c])
                    for c8 in range(8):
                        ti, hf = c8 // 2, c8 % 2
                        MM(PTA.t[hf * 64:(hf + 1) * 64, 256 + ti * 64:256 + (ti + 1) * 64], B.kdec.t[:, c8 * 64:(c8 + 1) * 64],
                           B.qx.t[:, c8 * 64:(c8 + 1) * 64], True, True, [B.kdec.c, B.qx.c], [pa_c])
                    TT("dve", B.A.t[:, :], PTA.t[:, 256:512], cb.t[:, C_MASK256:C_MASK256 + 256], ALU.mult, [pa_c, cb.c], [B.A.c])

                def back_u(bi):
                    h, s_, j, B, wh, T0 = geo(bi)
                    for c8 in range(8):
                        ti, hf = c8 // 2, c8 % 2
                        hs = slice(hf * 64, (hf + 1) * 64)
                        ub = PUe if hf == 0 else PUo
                        MM(ub.t[:, ti * 128:(ti + 1) * 128], B.kdT.t[hs, ti, :], B.vtok.t[hs, ti, :], True, True,
                           [B.kdT.c, B.vtok.c], [ub.c])

                def back_chain(bi):
                    h, s_, j, B, wh, T0 = geo(bi)
                    if j == 0:
                        MSET("dve", S.t[:, :], 0.0, [S.c])
                        MSET("dve", Sds[0].t[:, :], 0.0, [Sds[0].c])
                    for c8 in range(8):
                        ti, hf = c8 // 2, c8 % 2
                        first = (j == 0 and c8 == 0)
                        ub = PUe if hf == 0 else PUo
                        if not first:
                            TS("dve", Sds[c8].t[:, :], S.t[:, :], B.eb.t[:, c8:c8 + 1], None, ALU.mult, None, [S.c, B.eb.c], [Sds[c8].c])
                        STT(S.t[:, :], S.t[:, :], B.eb.t[:, c8:c8 + 1], ub.t[:, ti * 128:(ti + 1) * 128], ALU.mult, ALU.add,
                            [S.c, B.eb.c, ub.c], [S.c])
                    for c8 in range(8):
                        ti, hf = c8 // 2, c8 % 2
                        hs = slice(hf * 64, (hf + 1) * 64)
                        MM(PO.t[:, c8 * 64:(c8 + 1) * 64], B.vtok.t[hs, ti, :], B.A.t[hs, ti * 64:(ti + 1) * 64], True, False,
                           [B.vtok.c, B.A.c], [PO.c])
                        MM(PO.t[:, c8 * 64:(c8 + 1) * 64], Sds[c8].t[:, :], B.qx.t[:, c8 * 64:(c8 + 1) * 64], False, True,
                           [Sds[c8].c, B.qx.c], [PO.c])
                    CP("act", B.osb.t[:, :], PO.t[:, :], [PO.c], [B.osb.c])
                    TT("pool", B.osq.t[:, :], B.osb.t[:, :], B.osb.t[:, :], ALU.mult, [B.osb.c], [B.osq.c])
                    MM(PO.t[:, :], ones_bf, B.osq.t[:, :], True, True, [cb.c, B.osq.c], [PO.c])

                def back_norm(bi):
                    h, s_, j, B, wh, T0 = geo(bi)
                    rstd_from(PO.t[:, :], 128, 512, 1.0 / 128, B.lnv, B.rs, [PO.c])
                    TT("dve", B.osb.t[:, :], B.osb.t[:, :], B.rs.t[:, :], ALU.mult, [B.osb.c, B.rs.c], [B.osb.c])
                    STT(B.oa.t[:, :], B.osb.t[:, :], ghg.t[:, 0:1], B.sg.t[:, :], ALU.mult, ALU.mult, [B.osb.c, ghg.c, B.sg.c], [B.oa.c])
                    DMA("sp", oaT[h * 128:(h + 1) * 128, T0:T0 + 512], B.oa.t[:, :], [B.oa.c], [])

                load_w(0)
                front_proj(0)
                front_mid(0)
                front_end(0)
                nb_ = len(batches)
                for bi in range(nb_):
                    h, s_, j = batches[bi]
                    if s_ == 0 and j == 0 and h + 1 < 8:
                        load_w(h + 1)
                    nx = bi + 1 < nb_
                    back_u(bi)
                    if nx:
                        front_proj(bi + 1)
                    back_chain(bi)
                    if nx:
                        front_mid(bi + 1)
                    back_norm(bi)
                    if nx:
                        front_end(bi + 1)
            em.barrier()


        if stage_limit >= 2:
            with contextlib.ExitStack() as st:
                wm = sbt(st, "wm", [128, 8, 768], BF16)
                wuq = sbt(st, "wuq", [128, 3, 1536], BF16)
                wuqs = sbt(st, "wuqs", [128, 3, 512], BF16)
                wukv = sbt(st, "wukv", [128, 2, 2048], BF16)
                gq = sbt(st, "gq", [128, 3], F32)
                gkv = sbt(st, "gkv", [128, 2], F32)
                cos2 = sbt(st, "cos2", [64, T], BF16)
                sin2 = sbt(st, "sin2", [64, T], BF16)
                scl = sbt(st, "scl", [64, 1], F32)
                rope_st = contextlib.ExitStack()
                posi = sbt(rope_st, "posi", [64, 1024], I32)
                ang = sbt(rope_st, "ang", [64, 1024], F32)
                uu = sbt(rope_st, "uu", [64, 1024], F32)
                ui = sbt(rope_st, "ui", [64, 1024], I32)
                uf = sbt(rope_st, "uf", [64, 1024], F32)
                PO, PL = PS[6], PS[7]
                TWO_PI = 2.0 * math.pi
                DMA("pool", wm.t[:, :, 0:704], w_in_v[:, :, 4096:4800], [], [wm.c])
                DMA("pool", wm.t[:, :, 704:768], w_krsw.rearrange("(c p) n -> p c n", p=128), [], [wm.c])
                DMA("pool", wuq.t[:, :, :], w_uq.rearrange("(c p) n -> p c n", p=128), [], [wuq.c])
                DMA("pool", wuqs.t[:, :, :], w_uqsw.rearrange("(c p) n -> p c n", p=128), [], [wuqs.c])
                DMA("pool", wukv.t[:, :, :], w_ukv.rearrange("(c p) n -> p c n", p=128), [], [wukv.c])
                DMA("sp", gq.t[:, :], g_q[:, :], [], [gq.c])
                DMA("sp", gkv.t[:, :], g_kv[:, :], [], [gkv.c])
                TS("dve", scl.t[:, :], cf.t[0:64, C_SGN:C_SGN + 1], TWO_PI * (1.0 - 1e-6), None, ALU.mult, None, [cf.c], [scl.c])
                for blk in range(4):
                    cs = slice(blk * 1024, (blk + 1) * 1024)
                    DMA("sp", posi.t[:, :], pos[0:1, cs].to_broadcast([64, 1024]), [], [posi.c])
                    CP("dve", ang.t[:, :], posi.t[:, :], [posi.c], [ang.c])
                    TS("dve", ang.t[:, :], ang.t[:, :], cf.t[0:64, C_INVF:C_INVF + 1], 1.0 / TWO_PI, ALU.mult, ALU.mult, [ang.c, cf.c], [ang.c])
                    for kind, off in (("sin", 0.0), ("cos", 0.25)):
                        TS("dve", uu.t[:, :], ang.t[:, :], off, None, ALU.add, None, [ang.c], [uu.c])
                        CP("dve", ui.t[:, :], uu.t[:, :], [uu.c], [ui.c])
                        CP("dve", uf.t[:, :], ui.t[:, :], [ui.c], [uf.c])
                        TT("dve", uu.t[:, :], uu.t[:, :], uf.t[:, :], ALU.subtract, [uu.c, uf.c], [uu.c])
                        TS("dve", uf.t[:, :], uu.t[:, :], 0.5, None, ALU.is_gt, None, [uu.c], [uf.c])
                        TT("dve", uu.t[:, :], uu.t[:, :], uf.t[:, :], ALU.subtract, [uu.c, uf.c], [uu.c])
                        TS("dve", uf.t[:, :], uu.t[:, :], -0.5, None, ALU.is_lt, None, [uu.c], [uf.c])
                        TT("dve", uu.t[:, :], uu.t[:, :], uf.t[:, :], ALU.add, [uu.c, uf.c], [uu.c])
                        if kind == "sin":
                            ACT(sin2.t[:, cs], uu.t[:, :], AF.Sin, [uu.c, scl.c], [sin2.c], scale=scl.t[:, 0:1])
                        else:
                            ACT(cos2.t[:, cs], uu.t[:, :], AF.Sin, [uu.c], [cos2.c], scale=TWO_PI * (1.0 - 1e-6))
                em.barrier()
                rope_st.close()
                sqc = [sbt(st, f"sqc{i}", [128, 512], BF16) for i in range(3)]
                lnv = sbt(st, "lnv2", [128, 512], F32)
                rs = sbt(st, "rs2", [128, 512], F32)
                cqn = sbt(st, "cqn", [128, 3, SEQ], BF16)
                ckvn = sbt(st, "ckvn", [128, 2, SEQ], BF16)
                krT = sbt(st, "krT", [64, SEQ], BF16)
                t1 = sbt(st, "t1", [64, 512], F32)
                t2 = sbt(st, "t2", [64, 512], F32)
                KnTs = [sbt(st, f"KnT{i}", [128, SEQ], BF16) for i in range(2)]
                Vhs = [sbt(st, f"Vh{i}", [128, 16, 128], BF16) for i in range(2)]
                qn = sbt(st, "qn", [128, 512], BF16)
                qr = sbt(st, "qr", [64, 512], BF16)
                pts = [sbt(st, f"pt{i}", [128, 512], BF16) for i in range(4)]
                lnl = sbt(st, "lnl", [128, 512], F32)
                rl = sbt(st, "rl", [128, 512], F32)
                obs = [sbt(st, f"ob{i}", [128, 512], BF16) for i in range(2)]
                nbat = 0
                for s in range(2):
                    for j in range(4):
                        T0 = s * SEQ + j * 512
                        L0 = j * 512
                        xc = xnT_c[T0 // 512]
                        for (dst_t, ncc, col0, gt, dim) in ((cqn, 3, 0, gq, 384), (ckvn, 2, 384, gkv, 256)):
                            banks = [nb() for _ in range(ncc)]
                            for cc in range(ncc):
                                for c in range(8):
                                    MM(banks[cc].t[:, :], wm.t[:, c, col0 + cc * 128:col0 + (cc + 1) * 128], xnT.t[:, c, T0:T0 + 512],
                                       c == 0, c == 7, [wm.c, xc], [banks[cc].c])
                            for cc in range(ncc):
                                ACT(sqc[cc].t[:, :], banks[cc].t[:, :], AF.Square, [banks[cc].c], [sqc[cc].c])
                            bs = nb()
                            for cc in range(ncc):
                                MM(bs.t[:, :], ones_bf, sqc[cc].t[:, :], cc == 0, cc == ncc - 1, [cb.c, sqc[cc].c], [bs.c])
                            rstd_from(bs.t[:, :], 128, 512, 1.0 / dim, lnv, rs, [bs.c])
                            for cc in range(ncc):
                                STT(dst_t.t[:, cc, L0:L0 + 512], banks[cc].t[:, :], gt.t[:, cc:cc + 1], rs.t[:, :], ALU.mult, ALU.mult,
                                    [banks[cc].c, gt.c, rs.c], [dst_t.c])
                        bk1, bk2 = nb(), nb()
                        for c in range(8):
                            MM(bk1.t[0:64, :], wm.t[:, c, 640:704], xnT.t[:, c, T0:T0 + 512], c == 0, c == 7, [wm.c, xc], [bk1.c])
                        for c in range(8):
                            MM(bk2.t[0:64, :], wm.t[:, c, 704:768], xnT.t[:, c, T0:T0 + 512], c == 0, c == 7, [wm.c, xc], [bk2.c])
                        TT("dve", t1.t[:, :], bk1.t[0:64, :], cos2.t[:, T0:T0 + 512], ALU.mult, [bk1.c, cos2.c], [t1.c])
                        TT("dve", t2.t[:, :], bk2.t[0:64, :], sin2.t[:, T0:T0 + 512], ALU.mult, [bk2.c, sin2.c], [t2.c])
                        TT("pool", krT.t[:, L0:L0 + 512], t1.t[:, :], t2.t[:, :], ALU.add, [t1.c, t2.c], [krT.c])
                    for h in range(8):
                        KnT = KnTs[h % 2]
                        Vh = Vhs[h % 2]
                        for j in range(4):
                            T0 = s * SEQ + j * 512
                            L0 = j * 512
                            bkk = nb()
                            for c in range(2):
                                MM(bkk.t[:, :], wukv.t[:, c, h * 256:h * 256 + 128], ckvn.t[:, c, L0:L0 + 512], c == 0, c == 1,
                                   [wukv.c, ckvn.c], [bkk.c])
                            CP("act", KnT.t[:, L0:L0 + 512], bkk.t[:, :], [bkk.c], [KnT.c])
                            bv = nb()
                            for ti in range(4):
                                for c in range(2):
                                    MM(bv.t[:, ti * 128:(ti + 1) * 128], ckvn.t[:, c, L0 + ti * 128:L0 + (ti + 1) * 128],
                                       wukv.t[:, c, h * 256 + 128:h * 256 + 256], c == 0, c == 1, [wukv.c, ckvn.c], [bv.c])
                            CP("dve", Vh.t[:, j * 4:(j + 1) * 4, :], bv.t[:, :].rearrange("p (a b) -> p a b", a=4), [bv.c], [Vh.c])
                            bq, bp, bps = nb(), nb(), nb()
                            for c in range(3):
                                MM(bq.t[:, :], wuq.t[:, c, h * 192:h * 192 + 128], cqn.t[:, c, L0:L0 + 512], c == 0, c == 2, [wuq.c, cqn.c], [bq.c])
                            for c in range(3):
                                MM(bp.t[0:64, :], wuq.t[:, c, h * 192 + 128:h * 192 + 192], cqn.t[:, c, L0:L0 + 512], c == 0, c == 2,
                                   [wuq.c, cqn.c], [bp.c])
                            for c in range(3):
                                MM(bps.t[0:64, :], wuqs.t[:, c, h * 64:(h + 1) * 64], cqn.t[:, c, L0:L0 + 512], c == 0, c == 2,
                                   [wuqs.c, cqn.c], [bps.c])
                            CP("act", qn.t[:, :], bq.t[:, :], [bq.c], [qn.c])
                            TT("dve", t1.t[:, :], bp.t[0:64, :], cos2.t[:, T0:T0 + 512], ALU.mult, [bp.c, cos2.c], [t1.c])
                            TT("dve", t2.t[:, :], bps.t[0:64, :], sin2.t[:, T0:T0 + 512], ALU.mult, [bps.c, sin2.c], [t2.c])
                            TT("pool", qr.t[:, :], t1.t[:, :], t2.t[:, :], ALU.add, [t1.c, t2.c], [qr.c])
                            nkt = 4 * j + 4
                            def col0(kt):
                                return max(0, kt - 4 * j) * 128

                            def s_mm(kt):
                                bst = nb()
                                c0 = col0(kt)
                                MM(bst.t[:, c0:512], KnT.t[:, kt * 128:(kt + 1) * 128], qn.t[:, c0:512], True, False, [KnT.c, qn.c], [bst.c])
                                MM(bst.t[:, c0:512], krT.t[:, kt * 128:(kt + 1) * 128], qr.t[:, c0:512], False, True, [krT.c, qr.c], [bst.c])
                                return bst
                            pend = [s_mm(0), s_mm(1)]
                            for kt in range(nkt):
                                bst = pend.pop(0)
                                if kt + 2 < nkt:
                                    pend.append(s_mm(kt + 2))
                                pt = pts[kt % 4]
                                c0 = col0(kt)
                                ACT(pt.t[:, c0:512], bst.t[:, c0:512], AF.Exp, [bst.c], [pt.c], scale=192.0 ** -0.5)
                                if kt >= 4 * j:
                                    TT("dve", pt.t[:, c0:c0 + 128], pt.t[:, c0:c0 + 128], cb.t[:, C_TRILE:C_TRILE + 128], ALU.mult,
                                       [pt.c, cb.c], [pt.c])
                                MM(PO.t[:, c0:512], Vh.t[:, kt, :], pt.t[:, c0:512], kt == 0, kt == nkt - 1, [Vh.c, pt.c], [PO.c])
                                MM(PL.t[:, c0:512], ones_bf, pt.t[:, c0:512], kt == 0, kt == nkt - 1, [cb.c, pt.c], [PL.c])
                            ACT(lnl.t[:, :], PL.t[:, :], AF.Ln, [PL.c], [lnl.c])
                            ACT(rl.t[:, :], lnl.t[:, :], AF.Exp, [lnl.c], [rl.c], scale=-1.0)
                            ob = obs[nbat % 2]
                            nbat += 1
                            TT("dve", ob.t[:, :], PO.t[:, :], rl.t[:, :], ALU.mult, [PO.c, rl.c], [ob.c])
                            DMA("sp", obT[h * 128:(h + 1) * 128, T0:T0 + 512], ob.t[:, :], [ob.c], [])
            em.barrier()

        if stage_limit >= 3:
            with contextlib.ExitStack() as st:
                wg = sbt(st, "wg", [128, 8, 2048], BF16)
                wbh = sbt(st, "wbh", [128, 8, D], BF16)
                wbm = sbt(st, "wbm", [128, 8, D], BF16)
                wo = sbt(st, "wo", [128, 8, D], BF16)
                wr = sbt(st, "wr", [128, 8, 72], F32)
                br = sbt(st, "br", [128, 72], F32)
                g2 = sbt(st, "g2", [128, D], F32)
                oabs = [sbt(st, f"oab{i}", [128, 8, 128], BF16) for i in range(2)]
                obbs = [sbt(st, f"obb{i}", [128, 8, 128], BF16) for i in range(2)]
                Ta = sbt(st, "Ta", [128, D], F32)
                Tb = sbt(st, "Tb", [128, D], F32)
                ybf = sbt(st, "ybf", [128, D], BF16)
                yT = sbt(st, "yT", [128, 8, 128], BF16)
                h2bfs = [sbt(st, f"h2bf{i}", [128, D], BF16) for i in range(2)]
                h2T = sbt(st, "h2T", [128, 8, 128], F32)
                ssq = sbt(st, "ssq3", [128, 2], F32)
                lnv = sbt(st, "lnv3", [128, 2], F32)
                rstd = sbt(st, "rstd3", [128, 2], F32)
                lg = sbt(st, "lg", [128, 72], F32)
                g8 = sbt(st, "g8", [128, 8], F32)
                ohg = sbt(st, "ohg", [128, 8], F32)
                ngm = sbt(st, "ngm", [128, 1], F32)
                ex = sbt(st, "ex", [128, 8], F32)
                gs = sbt(st, "gs", [128, 1], F32)
                gw = sbt(st, "gw", [128, 1], F32)
                pen = sbt(st, "pen", [128, 8], F32)
                msk = sbt(st, "msk", [128, 64], F32)
                m8 = sbt(st, "m8", [128, 8], F32)
                i8 = sbt(st, "i8", [128, 8], U32)
                idf = sbt(st, "idf", [128, 2], F32)
                dlt = sbt(st, "dlt", [128, 1], F32)
                ed = sbt(st, "ed", [128, 1], F32)
                den = sbt(st, "den", [128, 1], F32)
                e1 = sbt(st, "e1", [128, 1], F32)
                e2 = sbt(st, "e2", [128, 1], F32)
                oh1 = sbt(st, "oh1", [128, 64], F32)
                oh2 = sbt(st, "oh2", [128, 64], F32)
                oh = sbt(st, "oh", [128, 64], F32)
                tmp64 = sbt(st, "tmp64", [128, 64], F32)
                sl = sbt(st, "sl", [128, 2], F32)
                ovf = sbt(st, "ovf", [128, 2], F32)
                dsf = sbt(st, "dsf", [128, 2], F32)
                base = sbt(st, "base", [1, 64], F32)
                xg_c = em.cell("xg")
                DMA("pool", wg.t[:, :, :], w_in_v[:, :, 4800:6848], [], [wg.c])
                DMA("pool", wbh.t[:, :, :], w_bh.rearrange("(c p) n -> p c n", p=128), [], [wbh.c])
                DMA("pool", wbm.t[:, :, :], w_bm.rearrange("(c p) n -> p c n", p=128), [], [wbm.c])
                DMA("pool", wo.t[:, :, :], w_out.rearrange("(c p) n -> p c n", p=128), [], [wo.c])
                DMA("sp", wr.t[:, :, :], w_r.rearrange("(c p) n -> p c n", p=128), [], [wr.c])
                DMA("sp", br.t[:, :], b_r[0:1, :].to_broadcast([128, 72]), [], [br.c])
                DMA("sp", g2.t[:, :], g_ffn[0:1, :].to_broadcast([128, D]), [], [g2.c])
                MSET("dve", base.t[:, :], 0.0, [base.c])
                oaT_v = oaT.rearrange("(c p) t -> p c t", p=128)
                obT_v = obT.rearrange("(c p) t -> p c t", p=128)
                ident_f = cf.t[:, C_IDENT:C_IDENT + 128]
                iota = cf.t[:, C_IOTA:C_IOTA + 64]
                Tas = [Ta, sbt(st, "Ta1", [128, D], F32)]
                Tbs = [Tb, sbt(st, "Tb1", [128, D], F32)]
                ybfs = [ybf, sbt(st, "ybf1", [128, D], BF16)]
                NT = T // 128

                def f_load(i):
                    tok = slice(i * 128, (i + 1) * 128)
                    DMA("sp", oabs[i % 2].t[:, :, :], oaT_v[:, :, tok], [], [oabs[i % 2].c])
                    DMA("sp", obbs[i % 2].t[:, :, :], obT_v[:, :, tok], [], [obbs[i % 2].c])

                def f_gate(i, which):
                    tok = slice(i * 128, (i + 1) * 128)
                    xc = xnT_c[i // 4]
                    Tt = (Tas if which == 0 else Tbs)[i % 2]
                    goff = 0 if which == 0 else 1024
                    for hf in range(2):
                        b = nb()
                        for c in range(8):
                            MM(b.t[:, :], xnT.t[:, c, tok], wg.t[:, c, goff + hf * 512:goff + (hf + 1) * 512], c == 0, c == 7,
                               [xc, wg.c], [b.c])
                        ACT(Tt.t[:, hf * 512:(hf + 1) * 512], b.t[:, :], AF.Sigmoid, [b.c], [Tt.c])

                def f_branch(i, which):
                    Tt = (Tas if which == 0 else Tbs)[i % 2]
                    src = (oabs if which == 0 else obbs)[i % 2]
                    wb = wbh if which == 0 else wbm
                    for hf in range(2):
                        b = nb()
                        for c in range(8):
                            MM(b.t[:, :], src.t[:, c, :], wb.t[:, c, hf * 512:(hf + 1) * 512], c == 0, c == 7, [src.c, wb.c], [b.c])
                        TT("dve", Tt.t[:, hf * 512:(hf + 1) * 512], b.t[:, :], Tt.t[:, hf * 512:(hf + 1) * 512], ALU.mult, [b.c, Tt.c], [Tt.c])
                    if which == 1:
                        TT("pool", ybfs[i % 2].t[:, :], Tas[i % 2].t[:, :], Tbs[i % 2].t[:, :], ALU.add,
                           [Tas[i % 2].c, Tbs[i % 2].c], [ybfs[i % 2].c])

                def b1(i):
                    tok = slice(i * 128, (i + 1) * 128)
                    yb_, Tb_ = ybfs[i % 2], Tbs[i % 2]
                    bt = nb()
                    btb = bt.t[:, :].bitcast(BF16)
                    for c in range(8):
                        TR(btb[:, c * 128:(c + 1) * 128], yb_.t[:, c * 128:(c + 1) * 128], ident_bf, [yb_.c, cb.c], [bt.c])
                    CP("act", yT.t[:, :, :], btb.rearrange("p (c t) -> p c t", c=8), [bt.c], [yT.c])
                    DMA("sp", Tb_.t[:, :], x[tok, :], [], [Tb_.c])

                def b2(i):
                    tok = slice(i * 128, (i + 1) * 128)
                    Ta_, Tb_, yb_ = Tas[i % 2], Tbs[i % 2], ybfs[i % 2]
                    h2bf = h2bfs[i % 2]
                    for hf in range(2):
                        b = nb()
                        for c in range(8):
                            MM(b.t[:, :], yT.t[:, c, :], wo.t[:, c, hf * 512:(hf + 1) * 512], c == 0, c == 7, [yT.c, wo.c], [b.c])
                        TT("dve", Ta_.t[:, hf * 512:(hf + 1) * 512], b.t[:, :], Tb_.t[:, hf * 512:(hf + 1) * 512], ALU.add, [b.c, Tb_.c], [Ta_.c])
                    DMA("sp", x1d[tok, :], Ta_.t[:, :], [Ta_.c], [])
                    ACT(yb_.t[:, :], Ta_.t[:, :], AF.Square, [Ta_.c], [yb_.c, ssq.c], accum_out=ssq.t[:, 0:1])
                    rstd_from(ssq.t[:, 0:1], 128, 1, 1.0 / D, lnv, rstd, [ssq.c])
                    STT(Tb_.t[:, :], Ta_.t[:, :], rstd.t[:, 0:1], g2.t[:, :], ALU.mult, ALU.mult, [Ta_.c, rstd.c, g2.c], [Tb_.c])
                    CP("pool", h2bf.t[:, :], Tb_.t[:, :], [Tb_.c], [h2bf.c])

                def b3(i):
                    Tb_ = Tbs[i % 2]
                    p1, p2 = nb(), nb()
                    for c in range(8):
                        bb = p1 if c < 4 else p2
                        TR(bb.t[:, (c % 4) * 128:(c % 4 + 1) * 128], Tb_.t[:, c * 128:(c + 1) * 128], ident_f, [Tb_.c, cf.c], [bb.c])
                    CP("act", h2T.t[:, 0:4, :], p1.t[:, :].rearrange("p (c t) -> p c t", c=4), [p1.c], [h2T.c])
                    CP("act", h2T.t[:, 4:8, :], p2.t[:, :].rearrange("p (c t) -> p c t", c=4), [p2.c], [h2T.c])

                def b4(i):
                    bl = nb()
                    for c in range(8):
                        MM(bl.t[:, 0:72], h2T.t[:, c, :], wr.t[:, c, :], c == 0, c == 7, [h2T.c, wr.c], [bl.c])
                    TT("dve", lg.t[:, :], bl.t[:, 0:72], br.t[:, :], ALU.add, [bl.c, br.c], [lg.c])
                    em.op("dve", lambda e: e.max(out=g8.t[:, :], in_=lg.t[:, 0:8]), [lg.c], [g8.c])
                    TS("dve", ohg.t[:, :], lg.t[:, 0:8], g8.t[:, 0:1], None, ALU.is_equal, None, [lg.c, g8.c], [ohg.c])
                    TS("dve", ngm.t[:, :], g8.t[:, 0:1], -1.0, None, ALU.mult, None, [g8.c], [ngm.c])
                    ACT(ex.t[:, :], lg.t[:, 0:8], AF.Exp, [lg.c, ngm.c], [ex.c, gs.c], bias=ngm.t[:, 0:1], accum_out=gs.t[:, 0:1])
                    em.op("dve", lambda e: e.reciprocal(out=gw.t[:, :], in_=gs.t[:, :]), [gs.c], [gw.c])
                    TS("dve", pen.t[:, :], ohg.t[:, :], -1.0, 1e30, ALU.add, ALU.mult, [ohg.c], [pen.c])
                    TT("dve", msk.t[:, :].rearrange("p (a b) -> p a b", a=8), lg.t[:, 8:72].rearrange("p (a b) -> p a b", a=8),
                       pen.t[:, :].rearrange("p (a b) -> p a b", b=1).to_broadcast([128, 8, 8]), ALU.add, [lg.c, pen.c], [msk.c])
                    em.op("dve", lambda e: e.max(out=m8.t[:, :], in_=msk.t[:, :]), [msk.c], [m8.c])
                    em.op("dve", lambda e: e.max_index(out=i8.t[:, :], in_max=m8.t[:, :], in_values=msk.t[:, :]), [m8.c, msk.c], [i8.c])
                    CP("dve", idf.t[:, :], i8.t[:, 0:2], [i8.c], [idf.c])
                    TT("dve", dlt.t[:, :], m8.t[:, 1:2], m8.t[:, 0:1], ALU.subtract, [m8.c], [dlt.c])
                    ACT(ed.t[:, :], dlt.t[:, :], AF.Exp, [dlt.c], [ed.c])
                    TS("dve", den.t[:, :], ed.t[:, :], 1.0, None, ALU.add, None, [ed.c], [den.c])
                    em.op("dve", lambda e: e.reciprocal(out=e1.t[:, :], in_=den.t[:, :]), [den.c], [e1.c])
                    TT("dve", e2.t[:, :], ed.t[:, :], e1.t[:, :], ALU.mult, [ed.c, e1.c], [e2.c])
                    TT("dve", wts.t[:, 2 * i:2 * i + 1], e1.t[:, :], gw.t[:, :], ALU.mult, [e1.c, gw.c], [wts.c])
                    TT("dve", wts.t[:, 2 * i + 1:2 * i + 2], e2.t[:, :], gw.t[:, :], ALU.mult, [e2.c, gw.c], [wts.c])
                    TS("dve", oh1.t[:, :], iota, idf.t[:, 0:1], None, ALU.is_equal, None, [cf.c, idf.c], [oh1.c])
                    TS("dve", oh2.t[:, :], iota, idf.t[:, 1:2], None, ALU.is_equal, None, [cf.c, idf.c], [oh2.c])
                    TT("dve", oh.t[:, :], oh1.t[:, :], oh2.t[:, :], ALU.add, [oh1.c, oh2.c], [oh.c])

                def b5(i):
                    h2bf = h2bfs[i % 2]
                    bp_ = nb()
                    MM(bp_.t[:, 0:64], cf.t[:, C_TRIST:C_TRIST + 128], oh.t[:, :], True, False, [cf.c, oh.c], [bp_.c])
                    MM(bp_.t[:, 0:64], cf.t[0:1, C_ONES:C_ONES + 128], base.t[0:1, :], False, True, [cf.c, base.c], [bp_.c])
                    bc_ = nb()
                    MM(bc_.t[0:1, 0:64], cf.t[:, C_ONES:C_ONES + 1], oh.t[:, :], True, True, [cf.c, oh.c], [bc_.c])
                    TT("dve", tmp64.t[:, :], bp_.t[:, 0:64], oh1.t[:, :], ALU.mult, [bp_.c, oh1.c], [tmp64.c])
                    em.op("dve", lambda e: e.reduce_sum(out=sl.t[:, 0:1], in_=tmp64.t[:, :], axis=mybir.AxisListType.X), [tmp64.c], [sl.c])
                    TT("dve", tmp64.t[:, :], bp_.t[:, 0:64], oh2.t[:, :], ALU.mult, [bp_.c, oh2.c], [tmp64.c])
                    em.op("dve", lambda e: e.reduce_sum(out=sl.t[:, 1:2], in_=tmp64.t[:, :], axis=mybir.AxisListType.X), [tmp64.c], [sl.c])
                    TT("dve", base.t[0:1, :], base.t[0:1, :], bc_.t[0:1, 0:64], ALU.add, [base.c, bc_.c], [base.c])
                    TS("dve", ovf.t[:, :], sl.t[:, :], float(CAP), 1e6, ALU.is_ge, ALU.mult, [sl.c], [ovf.c])
                    STT(dsf.t[:, :], idf.t[:, :], float(CAP), sl.t[:, :], ALU.mult, ALU.add, [idf.c, sl.c], [dsf.c])
                    TT("dve", dsf.t[:, :], dsf.t[:, :], ovf.t[:, :], ALU.add, [dsf.c, ovf.c], [dsf.c])
                    TS("dve", dsf.t[:, :], dsf.t[:, :], float(NROWS), None, ALU.min, None, [dsf.c], [dsf.c])
                    CP("dve", dst.t[:, 2 * i:2 * i + 2], dsf.t[:, :], [dsf.c], [dst.c])
                    for k in range(2):
                        em.op("pool", lambda e, k=k, i=i, h2bf=h2bf: e.indirect_dma_start(
                            out=xg[:, :], out_offset=bass.IndirectOffsetOnAxis(ap=dst.t[:, 2 * i + k:2 * i + k + 1], axis=0),
                            in_=h2bf.t[:, :], in_offset=None), [dst.c, h2bf.c], [xg_c], kind="d")

                f_load(0)
                f_gate(0, 0)
                f_branch(0, 0)
                f_gate(0, 1)
                f_branch(0, 1)
                for i in range(NT):
                    nx = i + 1 < NT
                    if nx:
                        f_load(i + 1)
                    b1(i)
                    if nx:
                        f_gate(i + 1, 0)
                    b2(i)
                    if nx:
                        f_branch(i + 1, 0)
                    b3(i)
                    if nx:
                        f_gate(i + 1, 1)
                    b4(i)
                    if nx:
                        f_branch(i + 1, 1)
                    b5(i)
            em.barrier()

        if stage_limit >= 4:
            with contextlib.ExitStack() as st:
                NR = CAP // 128
                w1s = [sbt(st, f"w1s{i}", [128, 8, 512], BF16) for i in range(3)]
                w3s = [sbt(st, f"w3s{i}", [128, 8, 512], BF16) for i in range(3)]
                w2s = [sbt(st, f"w2s{i}", [128, 4, D], BF16) for i in range(3)]
                xgs = [sbt(st, f"xgs{i}", [128, NR, D], BF16) for i in range(2)]
                xgT = [sbt(st, f"xgT{i}", [128, 8, CAP], BF16) for i in range(2)]
                s1s = [sbt(st, f"s1s{i}", [128, CAP], F32) for i in range(2)]
                hid = [sbt(st, f"hid{i}", [128, 4, CAP], BF16) for i in range(2)]
                ysb = [sbt(st, f"ysb{i}", [128, D], F32) for i in range(2)]
                nsi = [0]

                def load_e(ex_):
                    sl_ = ex_ % 3
                    DMA("pool", w1s[sl_].t[:, :, :], ew1[ex_].rearrange("(c p) n -> p c n", p=128), [], [w1s[sl_].c])
                    DMA("pool", w3s[sl_].t[:, :, :], ew3[ex_].rearrange("(c p) n -> p c n", p=128), [], [w3s[sl_].c])
                    DMA("pool", w2s[sl_].t[:, :, :], ew2[ex_].rearrange("(c p) n -> p c n", p=128), [], [w2s[sl_].c])
                    xs = xgs[ex_ % 2]
                    DMA("sp", xs.t[:, :, :], xg[ex_ * CAP:(ex_ + 1) * CAP, :].rearrange("(r p) d -> p r d", p=128), [], [xs.c])

                def front_e(ex_):
                    sl_ = ex_ % 3
                    xs = xgs[ex_ % 2]
                    xT = xgT[ex_ % 2]
                    hd = hid[ex_ % 2]
                    for r in range(NR):
                        bt = nb()
                        btb = bt.t[:, :].bitcast(BF16)
                        for c in range(8):
                            TR(btb[:, c * 128:(c + 1) * 128], xs.t[:, r, c * 128:(c + 1) * 128], ident_bf, [xs.c, cb.c], [bt.c])
                        CP("act" if r == 0 else "dve", xT.t[:, :, r * 128:(r + 1) * 128], btb.rearrange("p (c t) -> p c t", c=8), [bt.c], [xT.c])
                    for fc in range(4):
                        b1, b3 = nb(), nb()
                        for c in range(8):
                            MM(b1.t[:, 0:CAP], w1s[sl_].t[:, c, fc * 128:(fc + 1) * 128], xT.t[:, c, :], c == 0, c == 7, [w1s[sl_].c, xT.c], [b1.c])
                        for c in range(8):
                            MM(b3.t[:, 0:CAP], w3s[sl_].t[:, c, fc * 128:(fc + 1) * 128], xT.t[:, c, :], c == 0, c == 7, [w3s[sl_].c, xT.c], [b3.c])
                        s1 = s1s[nsi[0] % 2]
                        nsi[0] += 1
                        ACT(s1.t[:, :], b1.t[:, 0:CAP], AF.Silu, [b1.c], [s1.c])
                        TT("dve", hd.t[:, fc, :], b3.t[:, 0:CAP], s1.t[:, :], ALU.mult, [b3.c, s1.c], [hd.c])

                def back_e(ex_):
                    sl_ = ex_ % 3
                    hd = hid[ex_ % 2]
                    for r in range(NR):
                        ys = ysb[r % 2]
                        for hf in range(2):
                            b = PS[6 + hf]
                            for fc in range(4):
                                MM(b.t[:, :], hd.t[:, fc, r * 128:(r + 1) * 128], w2s[sl_].t[:, fc, hf * 512:(hf + 1) * 512], fc == 0, fc == 3,
                                   [hd.c, w2s[sl_].c], [b.c])
                            CP("act" if hf == 0 else "dve", ys.t[:, hf * 512:(hf + 1) * 512], b.t[:, :], [b.c], [ys.c])
                        DMA("sp", yb[ex_ * CAP + r * 128:ex_ * CAP + (r + 1) * 128, :], ys.t[:, :], [ys.c], [])

                load_e(0)
                load_e(1)
                front_e(0)
                for ex_ in range(NEXP):
                    if ex_ + 2 < NEXP:
                        load_e(ex_ + 2)
                    if ex_ + 1 < NEXP:
                        front_e(ex_ + 1)
                    back_e(ex_)
            em.barrier()

        if stage_limit >= 5:
            with contextlib.ExitStack() as st:
                y0s = [sbt(st, f"y0s{i}", [128, D], F32) for i in range(4)]
                y1s = [sbt(st, f"y1s{i}", [128, D], F32) for i in range(4)]
                x1s = [sbt(st, f"x1s{i}", [128, D], F32) for i in range(4)]
                accs = [sbt(st, f"acc{i}", [128, D], F32) for i in range(4)]
                junk = sbt(st, "junk5", [128, D], BF16)
                gF = sbt(st, "gF", [128, D], F32)
                ssq = sbt(st, "ssq5", [128, 2], F32)
                lnv = sbt(st, "lnv5", [128, 2], F32)
                rstd = sbt(st, "rstd5", [128, 2], F32)
                DMA("sp", gF.t[:, :], g_fin[0:1, :].to_broadcast([128, D]), [], [gF.c])
                ssqs = [sbt(st, f"ssq5_{i}", [128, 2], F32) for i in range(2)]
                lnvs = [sbt(st, f"lnv5_{i}", [128, 2], F32) for i in range(2)]
                rstds = [sbt(st, f"rstd5_{i}", [128, 2], F32) for i in range(2)]

                def s5_pre(i):
                    tok = slice(i * 128, (i + 1) * 128)
                    y0, y1, x1t, acc = y0s[i % 4], y1s[i % 4], x1s[i % 4], accs[i % 4]
                    for k, yt in ((0, y0), (1, y1)):
                        em.op("pool", lambda e, k=k, i=i, yt=yt: e.indirect_dma_start(
                            out=yt.t[:, :], out_offset=None, in_=yb[:, :],
                            in_offset=bass.IndirectOffsetOnAxis(ap=dst.t[:, 2 * i + k:2 * i + k + 1], axis=0)), [dst.c], [yt.c], kind="d")
                    DMA("sp", x1t.t[:, :], x1d[tok, :], [], [x1t.c])
                    STT(acc.t[:, :], y0.t[:, :], wts.t[:, 2 * i:2 * i + 1], x1t.t[:, :], ALU.mult, ALU.add, [y0.c, wts.c, x1t.c], [acc.c])
                    STT(acc.t[:, :], y1.t[:, :], wts.t[:, 2 * i + 1:2 * i + 2], acc.t[:, :], ALU.mult, ALU.add, [y1.c, wts.c, acc.c], [acc.c])
                    sq_, ln_, rs_ = ssqs[i % 2], lnvs[i % 2], rstds[i % 2]
                    ACT(junk.t[:, :], acc.t[:, :], AF.Square, [acc.c], [junk.c, sq_.c], accum_out=sq_.t[:, 0:1])
                    rstd_from(sq_.t[:, 0:1], 128, 1, 1.0 / D, ln_, rs_, [sq_.c])

                def s5_post(i):
                    tok = slice(i * 128, (i + 1) * 128)
                    acc = accs[i % 4]
                    rs_ = rstds[i % 2]
                    STT(acc.t[:, :], acc.t[:, :], rs_.t[:, 0:1], gF.t[:, :], ALU.mult, ALU.mult, [acc.c, rs_.c, gF.c], [acc.c])
                    DMA("sp", out[tok, :], acc.t[:, :], [acc.c], [])

                s5_pre(0)
                for i in range(T // 128):
                    if i + 1 < T // 128:
                        s5_pre(i + 1)
                    s5_post(i)

        stats = em.emit()
    return nc, stats


_CACHE = {}


def prep_inputs(inputs):
    f = lambda a: np.ascontiguousarray(np.asarray(a, dtype=np.float32))

    def pc(w, nch):
        e, r, n = w.shape
        return np.ascontiguousarray(w.reshape(e, nch, 128, n).transpose(0, 2, 1, 3)).reshape(e, 128, nch * n)
    x = f(inputs["x"])
    positions = np.asarray(inputs["positions"]).astype(np.int32)
    w_in = f(inputs["w_in"][0])
    kr0 = 4096 + 384 + 256
    w_krsw = np.ascontiguousarray(np.concatenate([w_in[:, kr0 + 32:kr0 + 64], w_in[:, kr0:kr0 + 32]], axis=1))
    hl = f(inputs["hg_lower_bound"])
    lbT = np.ascontiguousarray(hl.reshape(2, 8, 128).transpose(2, 0, 1).reshape(128, 16))
    w_uq = f(inputs["mla_w_uq"][0])
    uq3 = w_uq.reshape(384, 8, 192)
    w_uqsw = np.ascontiguousarray(np.concatenate([uq3[:, :, 160:192], uq3[:, :, 128:160]], axis=2).reshape(384, 512))
    shared = {
        "cst": make_consts(),
        "w_in": w_in,
        "w_krsw": w_krsw,
        "g_attn": f(inputs["attn_norm_w"]).reshape(1, D),
        "g_ffn": f(inputs["ffn_norm_w"]).reshape(1, D),
        "g_fin": f(inputs["final_norm_w"]).reshape(1, D),
        "lbT": lbT,
        "g_hg": f(inputs["hg_out_norm_w"]).reshape(128, 1),
        "g_q": np.ascontiguousarray(f(inputs["mla_q_norm_w"]).reshape(3, 128).T),
        "g_kv": np.ascontiguousarray(f(inputs["mla_kv_norm_w"]).reshape(2, 128).T),
        "w_uq": w_uq,
        "w_uqsw": w_uqsw,
        "w_ukv": f(inputs["mla_w_ukv"][0]),
        "w_bh": f(inputs["w_branch_hgrn"][0]),
        "w_bm": f(inputs["w_branch_mla"][0]),
        "w_out": f(inputs["w_out"][0]),
        "w_r": np.ascontiguousarray(np.concatenate([f(inputs["router_group_w"][0]), f(inputs["router_expert_w"][0])], axis=1)),
        "b_r": np.ascontiguousarray(np.concatenate([f(inputs["router_group_b"][0]), f(inputs["router_expert_b"][0])]).reshape(1, 72)),
        "ew1": f(inputs["expert_w1"][0]),
        "ew3": f(inputs["expert_w3"][0]),
        "ew2": f(inputs["expert_w2"][0]),
    }
    in_maps = []
    for c in range(NCORES):
        m = dict(shared)
        m["x"] = np.ascontiguousarray(x[2 * c:2 * c + 2].reshape(T, D))
        m["pos"] = np.ascontiguousarray(positions[2 * c:2 * c + 2].reshape(1, T))
        in_maps.append(m)
    return in_maps


def kernel(**inputs):
    if "nc" not in _CACHE:
        _CACHE["nc"] = build()[0]
    nc = _CACHE["nc"]
    in_maps = prep_inputs(inputs)
    res = run_bass_kernel_spmd(nc, in_maps, core_ids=list(range(NCORES)))
    outs = [np.asarray(r["out"]).reshape(2, SEQ, D) for r in res.results]
    return np.concatenate(outs, axis=0).astype(np.float32)
```

```python
import math
import contextlib
import numpy as np
import concourse.bass as bass
import concourse.mybir as mybir
from concourse.bass_utils import run_bass_kernel_spmd

F32 = mybir.dt.float32
BF16 = mybir.dt.bfloat16
I32 = mybir.dt.int32
U32 = mybir.dt.uint32
AF = mybir.ActivationFunctionType
ALU = mybir.AluOpType

NCORES = 8
T = 4096
SEQ = 2048
D = 1024
EPS = 1e-6
CAP = 256
NEXP = 64
NROWS = NEXP * CAP
SAME_ENG_SYNC = True
N_DMA_SEMS = 40
SCRATCH_INTERNAL = True

C_IDENT = 0
C_TRILE = 128
C_TRIST = 256
C_MASK256 = 384
C_RESET = 640
C_IOTA = 1152
C_INVF = 1216
C_SGN = 1217
C_ONES = 1218
C_AMASK = 1346
NCONST = C_AMASK + 2048


def make_consts():
    c = np.zeros((128, NCONST), np.float32)
    p = np.arange(128)[:, None]
    f = np.arange(128)[None, :]
    c[:, C_IDENT:C_IDENT + 128] = (p == f)
    c[:, C_TRILE:C_TRILE + 128] = (p <= f)
    c[:, C_TRIST:C_TRIST + 128] = (p < f)
    f256 = np.arange(256)[None, :]
    c[:, C_MASK256:C_MASK256 + 256] = ((p % 64) <= (f256 % 64))
    f512 = np.arange(512)[None, :]
    c[:, C_RESET:C_RESET + 512] = ((f512 % 64) != 0)
    c[:, C_IOTA:C_IOTA + 64] = np.arange(64)[None, :]
    half = 32
    inv_freq = (10000.0 ** (-np.arange(half, dtype=np.float32) / half)).astype(np.float32)
    c[:, C_INVF] = inv_freq[np.arange(128) % 32]
    c[:, C_SGN] = np.where((np.arange(128) % 64) < 32, -1.0, 1.0)
    c[:, C_ONES:C_ONES + 128] = 1.0
    for r in range(4):
        c[:, C_AMASK + r * 512:C_AMASK + (r + 1) * 512] = ((p + r * 128) <= f512)
    return c


class Cell:
    __slots__ = ("name", "w", "r")

    def __init__(self, name):
        self.name = name
        self.w = None
        self.r = []


class TB:
    def __init__(self, t, c):
        self.t = t
        self.c = c


class Emitter:
    def __init__(self, nc, es):
        self.nc = nc
        self.es = es
        self.eng = {"pe": nc.tensor, "act": nc.scalar, "dve": nc.vector, "pool": nc.gpsimd, "sp": nc.sync}
        self.ops = []
        self.last = {}
        self.dmas_since = []

    def cell(self, name="c"):
        return Cell(name)

    def op(self, eng, fn, reads=(), writes=(), kind="c"):
        oid = len(self.ops)
        deps = set()
        for c in reads:
            if c.w is not None:
                deps.add(c.w)
        for c in writes:
            if c.w is not None:
                deps.add(c.w)
            deps.update(c.r)
        self.ops.append(dict(eng=eng, fn=fn, deps=deps, kind=kind, sig=False))
        for c in reads:
            if kind == "c":
                c.r = [q for q in c.r if not (self.ops[q]["kind"] == "c" and self.ops[q]["eng"] == eng)]
            c.r.append(oid)
        for c in writes:
            c.w = oid
            c.r = []
        if kind == "d":
            self.dmas_since.append(oid)
        else:
            self.last[eng] = oid
        return oid

    def barrier(self):
        deps = set(self.last.values()) | set(self.dmas_since)
        for e in self.eng:
            self.ops.append(dict(eng=e, fn=None, deps=set(deps), kind="b", sig=False))
        self.dmas_since = []

    def emit(self):
        nc = self.nc
        ops = self.ops

        def skip(po, o):
            if po["kind"] != "c" or o["kind"] != "c":
                return False
            if po["eng"] == "pe" and o["eng"] == "pe":
                return True
            if (not SAME_ENG_SYNC) and po["eng"] == o["eng"]:
                return True
            return False

        for o in ops:
            for d in o["deps"]:
                po = ops[d]
                if po["kind"] == "c" and not skip(po, o):
                    po["sig"] = True
        sems = {e: self.es.enter_context(nc.semaphore("s_" + e)) for e in self.eng}
        dpool = {}
        for q in ("sp", "pool", "act"):
            n = N_DMA_SEMS if q != "act" else 4
            dpool[q] = dict(sems=[self.es.enter_context(nc.semaphore(f"s_d{q}{i}")) for i in range(n)],
                            cnt=[0] * n, last=[None] * n, nxt=0, n=n)
        cnt = {e: 0 for e in self.eng}
        waited = {e: {} for e in self.eng}
        tok = [None] * len(ops)
        nw = 0

        def wait(e, key, sem, val):
            nonlocal nw
            if waited[e].get(key, 0) >= val:
                return
            self.eng[e].wait_ge(sem, val)
            waited[e][key] = val
            nw += 1

        for oid, o in enumerate(ops):
            e = o["eng"]
            for d in sorted(o["deps"]):
                po = ops[d]
                if skip(po, o):
                    continue
                if tok[d] is None:
                    continue
                key, sem, val = tok[d]
                wait(e, key, sem, val)
            if o["kind"] == "b":
                continue
            if o["kind"] == "d":
                P = dpool[e]
                i = P["nxt"]
                P["nxt"] = (i + 1) % P["n"]
                if P["last"][i] is not None:
                    wait(e, ("d", e, i), P["sems"][i], P["last"][i])
                inst = o["fn"](self.eng[e])
                P["cnt"][i] += 16
                inst.then_inc(P["sems"][i], 16)
                P["last"][i] = P["cnt"][i]
                tok[oid] = (("d", e, i), P["sems"][i], P["cnt"][i])
            else:
                inst = o["fn"](self.eng[e])
                if o["sig"]:
                    cnt[e] += 1
                    inst.then_inc(sems[e], 1)
                    tok[oid] = (e, sems[e], cnt[e])
        for q, P in dpool.items():
            for i in range(P["n"]):
                if P["last"][i] is not None:
                    wait("sp", ("d", q, i), P["sems"][i], P["last"][i])
        return dict(n_ops=len(ops), n_waits=nw, cnt=cnt)


def build(stage_limit=99, dbg=False):
    nc = bass.Bass("TRN2", target_bir_lowering=False)

    def DI(name, shape, dt):
        return nc.dram_tensor(name, list(shape), dt, kind="ExternalInput").ap()

    def DS(name, shape, dt):
        kind = "ExternalOutput" if (dbg or not SCRATCH_INTERNAL) else "Internal"
        return nc.dram_tensor(name, list(shape), dt, kind=kind).ap()

    x = DI("x", [T, D], F32)
    pos = DI("pos", [1, T], I32)
    cst = DI("cst", [128, NCONST], F32)
    w_in = DI("w_in", [D, 6848], F32)
    w_krsw = DI("w_krsw", [D, 64], F32)
    g_attn = DI("g_attn", [1, D], F32)
    g_ffn = DI("g_ffn", [1, D], F32)
    g_fin = DI("g_fin", [1, D], F32)
    lbT = DI("lbT", [128, 16], F32)
    g_hg = DI("g_hg", [128, 1], F32)
    g_q = DI("g_q", [128, 3], F32)
    g_kv = DI("g_kv", [128, 2], F32)
    w_uq = DI("w_uq", [384, 1536], F32)
    w_uqsw = DI("w_uqsw", [384, 512], F32)
    w_ukv = DI("w_ukv", [256, 2048], F32)
    w_bh = DI("w_bh", [D, D], F32)
    w_bm = DI("w_bm", [D, D], F32)
    w_out = DI("w_out", [D, D], F32)
    w_r = DI("w_r", [D, 72], F32)
    b_r = DI("b_r", [1, 72], F32)
    ew1 = DI("ew1", [NEXP, D, 512], F32)
    ew3 = DI("ew3", [NEXP, D, 512], F32)
    ew2 = DI("ew2", [NEXP, 512, D], F32)
    out = nc.dram_tensor("out", [T, D], F32, kind="ExternalOutput").ap()

    oaT = DS("oaT", [D, T], BF16)
    obT = DS("obT", [D, T], BF16)
    x1d = DS("x1d", [T, D], F32)
    xg = DS("xg", [NROWS + 128, D], BF16)
    yb = DS("yb", [NROWS + 128, D], F32)

    w_in_v = w_in.rearrange("(c p) n -> p c n", p=128)

    with contextlib.ExitStack() as es:
        em = Emitter(nc, es)

        def sbt(st, name, shape, dt):
            t = st.enter_context(nc.sbuf_tensor(name, list(shape), dt))
            return TB(t, em.cell(name))

        def ACT(out_, in_, func, r, w, **kw):
            em.op("act", lambda e: e.activation(out=out_, in_=in_, func=func, **kw), r, w)

        def TT(eng, out_, in0, in1, op, r, w):
            em.op(eng, lambda e: e.tensor_tensor(out=out_, in0=in0, in1=in1, op=op), r, w)

        def TS(eng, out_, in0, s1, s2, op0, op1, r, w):
            if op1 is None:
                em.op(eng, lambda e: e.tensor_scalar(out=out_, in0=in0, scalar1=s1, scalar2=None, op0=op0), r, w)
            else:
                em.op(eng, lambda e: e.tensor_scalar(out=out_, in0=in0, scalar1=s1, scalar2=s2, op0=op0, op1=op1), r, w)

        def STT(out_, in0, scalar, in1, op0, op1, r, w):
            em.op("dve", lambda e: e.scalar_tensor_tensor(out=out_, in0=in0, scalar=scalar, in1=in1, op0=op0, op1=op1), r, w)

        def MM(out_, lhsT, rhs, start, stop, r, w):
            em.op("pe", lambda e: e.matmul(out_, lhsT, rhs, start=start, stop=stop), r, w)

        def TR(out_, in_, ident, r, w):
            em.op("pe", lambda e: e.transpose(out_, in_, ident), r, w)

        def CP(eng, out_, in_, r, w):
            if eng == "act":
                em.op("act", lambda e: e.activation(out=out_, in_=in_, func=AF.Copy), r, w)
            else:
                em.op(eng, lambda e: e.tensor_copy(out=out_, in_=in_), r, w)

        def MSET(eng, ap, val, w):
            em.op(eng, lambda e: e.memset(ap, val), [], w)

        def DMA(q, out_, in_, r, w):
            em.op(q, lambda e: e.dma_start(out=out_, in_=in_), r, w, kind="d")

        PS = []
        for i in range(8):
            t = es.enter_context(nc.psum_tensor(f"ps{i}", [128, 512], F32))
            PS.append(TB(t, em.cell(f"ps{i}")))
        rot = [0]

        def nb(n=6):
            b = PS[rot[0] % n]
            rot[0] += 1
            return b

        xnT = sbt(es, "xnT", [128, 8, T], BF16)
        xnT_c = [em.cell(f"xnT{b}") for b in range(T // 512)]
        cb = sbt(es, "cb", [128, 1346 + 2048], BF16)
        cf = sbt(es, "cf", [128, 1346], F32)
        epsT = sbt(es, "epsT", [128, 1], F32)
        dst = sbt(es, "dst", [128, 64], U32)
        wts = sbt(es, "wts", [128, 64], F32)

        DMA("pool", cb.t[:, :], cst[:, :], [], [cb.c])
        DMA("sp", cf.t[:, :], cst[:, 0:1346], [], [cf.c])
        MSET("dve", epsT.t[:, :], EPS, [epsT.c])
        ident_bf = cb.t[:, C_IDENT:C_IDENT + 128]
        ones_bf = cb.t[:, C_ONES:C_ONES + 128]

        def rstd_from(ssq_ap, npart, ncol, scale, tmp, outt, reads):
            ACT(tmp.t[0:npart, 0:ncol], ssq_ap, AF.Ln, reads + [epsT.c], [tmp.c], scale=scale, bias=epsT.t[0:npart, 0:1])
            ACT(outt.t[0:npart, 0:ncol], tmp.t[0:npart, 0:ncol], AF.Exp, [tmp.c], [outt.c], scale=-0.5)

        with contextlib.ExitStack() as st:
            xts = [sbt(st, f"xt{i}", [128, D], F32) for i in range(3)]
            xns = [sbt(st, f"xn{i}", [128, D], BF16) for i in range(2)]
            junk = sbt(st, "junk", [128, D], BF16)
            gA = sbt(st, "gA", [128, D], F32)
            ssq = sbt(st, "ssq", [128, 2], F32)
            lnv = sbt(st, "lnv", [128, 2], F32)
            rstd = sbt(st, "rstd", [128, 2], F32)
            DMA("sp", gA.t[:, :], g_attn[0:1, :].to_broadcast([128, D]), [], [gA.c])
            s0_banks = {}

            def s0_pre(i):
                xt = xts[i % 3]
                xn = xns[i % 2]
                DMA("sp", xt.t[:, :], x[i * 128:(i + 1) * 128, :], [], [xt.c])
                ACT(junk.t[:, :], xt.t[:, :], AF.Square, [xt.c], [junk.c, ssq.c], accum_out=ssq.t[:, 0:1])
                rstd_from(ssq.t[:, 0:1], 128, 1, 1.0 / D, lnv, rstd, [ssq.c])
                STT(xn.t[:, :], xt.t[:, :], rstd.t[:, 0:1], gA.t[:, :], ALU.mult, ALU.mult, [xt.c, rstd.c, gA.c], [xn.c])
                bk = nb()
                bkb = bk.t[:, :].bitcast(BF16)
                for c in range(8):
                    TR(bkb[:, c * 128:(c + 1) * 128], xn.t[:, c * 128:(c + 1) * 128], ident_bf, [xn.c, cb.c], [bk.c])
                s0_banks[i] = bk

            def s0_post(i):
                bk = s0_banks.pop(i)
                bkb = bk.t[:, :].bitcast(BF16)
                CP("act", xnT.t[:, :, i * 128:(i + 1) * 128], bkb.rearrange("p (c t) -> p c t", c=8), [bk.c], [xnT_c[i // 4]])

            s0_pre(0)
            for i in range(T // 128):
                if i + 1 < T // 128:
                    s0_pre(i + 1)
                s0_post(i)
        em.barrier()

        if stage_limit >= 1:
            with contextlib.ExitStack() as st:
                whs = [sbt(st, f"wh{i}", [128, 8, 4, 128], BF16) for i in range(2)]
                lb_sb = sbt(st, "lb_sb", [128, 16], F32)
                oml = sbt(st, "oml", [128, 8], F32)
                ghg = sbt(st, "ghg", [128, 1], F32)
                class NS:
                    pass
                bufs = []
                for bi_ in range(2):
                    B = NS()
                    for nm in ("sq", "kk", "logf", "bcum", "dd", "ek", "eq", "sg", "osb", "lnv", "rs"):
                        setattr(B, nm, sbt(st, f"{nm}{bi_}", [128, 512], F32))
                    for nm in ("kdec", "qx", "osq", "oa"):
                        setattr(B, nm, sbt(st, f"{nm}{bi_}", [128, 512], BF16))
                    B.eb = sbt(st, f"eb{bi_}", [128, 8], F32)
                    B.vtok = sbt(st, f"vtok{bi_}", [128, 4, 128], BF16)
                    B.kdT = sbt(st, f"kdT{bi_}", [128, 4, 128], BF16)
                    B.A = sbt(st, f"A{bi_}", [128, 256], BF16)
                    bufs.append(B)
                S = sbt(st, "S", [128, 128], F32)
                Sds = [sbt(st, f"Sd{i}", [128, 128], BF16) for i in range(8)]
                zt = sbt(st, "zt", [128, 8192], BF16)
                zf = sbt(st, "zf", [128, D], F32)
                MSET("pool", zt.t[:, :], 0.0, [zt.c])
                MSET("pool", zf.t[:, :], 0.0, [zf.c])
                xg_flat = xg.rearrange("(p r) d -> p (r d)", p=128)
                nper = (NROWS + 128) // 128 * D
                for k0 in range(0, nper, 8192):
                    k1 = min(nper, k0 + 8192)
                    DMA("sp", xg_flat[:, k0:k1], zt.t[:, 0:k1 - k0], [zt.c], [])
                DMA("sp", yb[NROWS:NROWS + 128, :], zf.t[:, :], [zf.c], [])
                DMA("sp", lb_sb.t[:, :], lbT[:, :], [], [lb_sb.c])
                DMA("sp", ghg.t[:, :], g_hg[:, :], [], [ghg.c])
                TT("dve", oml.t[:, :], lb_sb.t[:, 8:16], lb_sb.t[:, 0:8], ALU.subtract, [lb_sb.c], [oml.c])
                ACT(oml.t[:, :], oml.t[:, :], AF.Sigmoid, [oml.c], [oml.c])
                PQ, PF_, PG_, PV, PTA, PUe, PUo, PO = PS
                pt_c = PTA.c
                pa_c = PTA.c
                lnc = math.log(128 ** -0.5)
                lncT = sbt(st, "lncT", [128, 1], F32)
                MSET("dve", lncT.t[:, :], lnc, [lncT.c])
                batches = [(h, s_, j) for h in range(8) for s_ in range(2) for j in range(4)]

                def load_w(h):
                    wh = whs[h % 2]
                    for fam in range(4):
                        col = fam * 1024 + h * 128
                        DMA("pool", wh.t[:, :, fam, :], w_in_v[:, :, col:col + 128], [], [wh.c])

                def geo(bi):
                    h, s_, j = batches[bi]
                    return h, s_, j, bufs[bi % 2], whs[h % 2], s_ * SEQ + j * 512

                def front_proj(bi):
                    h, s_, j, B, wh, T0 = geo(bi)
                    xc = xnT_c[T0 // 512]
                    for c in range(8):
                        MM(PQ.t[:, :], wh.t[:, c, 0, :], xnT.t[:, c, T0:T0 + 512], c == 0, c == 7, [wh.c, xc], [PQ.c])
                    for c in range(8):
                        MM(PF_.t[:, :], wh.t[:, c, 1, :], xnT.t[:, c, T0:T0 + 512], c == 0, c == 7, [wh.c, xc], [PF_.c])
                    for c in range(8):
                        MM(PG_.t[:, :], wh.t[:, c, 3, :], xnT.t[:, c, T0:T0 + 512], c == 0, c == 7, [wh.c, xc], [PG_.c])
                    for ti in range(4):
                        for c in range(8):
                            MM(PV.t[:, ti * 128:(ti + 1) * 128], xnT.t[:, c, T0 + ti * 128:T0 + (ti + 1) * 128],
                               wh.t[:, c, 2, :], c == 0, c == 7, [wh.c, xc], [PV.c])
                    ACT(B.kk.t[:, :], PF_.t[:, :], AF.Sigmoid, [PF_.c], [B.kk.c], scale=-1.0)
                    ACT(B.sq.t[:, :], PQ.t[:, :], AF.Silu, [PQ.c], [B.sq.c])
                    ACT(B.sg.t[:, :], PG_.t[:, :], AF.Silu, [PG_.c], [B.sg.c])
                    CP("act", B.vtok.t[:, :, :], PV.t[:, :].rearrange("p (a b) -> p a b", a=4), [PV.c], [B.vtok.c])

                def front_mid(bi):
                    h, s_, j, B, wh, T0 = geo(bi)
                    TS("dve", B.kk.t[:, :], B.kk.t[:, :], oml.t[:, h:h + 1], None, ALU.mult, None, [B.kk.c, oml.c], [B.kk.c])
                    ACT(B.logf.t[:, :], B.kk.t[:, :], AF.Ln, [B.kk.c, cf.c], [B.logf.c], scale=-1.0, bias=cf.t[:, C_ONES:C_ONES + 1])
                    em.op("dve", lambda e: e.tensor_tensor_scan(
                        out=B.bcum.t[:, :], data0=cf.t[:, C_RESET:C_RESET + 512], data1=B.logf.t[:, :],
                        initial=0.0, op0=ALU.mult, op1=ALU.add), [cf.c, B.logf.c], [B.bcum.c])
                    b3 = B.bcum.t[:, :].rearrange("p (a b) -> p a b", a=8)
                    d3 = B.dd.t[:, :].rearrange("p (a b) -> p a b", a=8)
                    TT("dve", d3, b3[:, :, 63:64].to_broadcast([128, 8, 64]), b3, ALU.subtract, [B.bcum.c], [B.dd.c])
                    ACT(B.ek.t[:, :], B.dd.t[:, :], AF.Exp, [B.dd.c], [B.ek.c])
                    ACT(B.eq.t[:, :], B.dd.t[:, :], AF.Exp, [B.dd.c, lncT.c], [B.eq.c], scale=-1.0, bias=lncT.t[:, 0:1])
                    ACT(B.eb.t[:, :], b3[:, :, 63], AF.Exp, [B.bcum.c], [B.eb.c])
                    TT("pool", B.kdec.t[:, :], B.kk.t[:, :], B.ek.t[:, :], ALU.mult, [B.kk.c, B.ek.c], [B.kdec.c])
                    TT("pool", B.qx.t[:, :], B.sq.t[:, :], B.eq.t[:, :], ALU.mult, [B.sq.c, B.eq.c], [B.qx.c])

                def front_end(bi):
                    h, s_, j, B, wh, T0 = geo(bi)
                    ptb = PTA.t[:, :].bitcast(BF16)
                    for ti in range(4):
                        TR(ptb[:, ti * 128:(ti + 1) * 128], B.kdec.t[:, ti * 128:(ti + 1) * 128], ident_bf, [B.kdec.c, cb.c], [pt_c])
                    CP("act", B.kdT.t[:, :, :], ptb[:, 0:512].rearrange("p (a b) -> p a b", a=4), [pt_c], [B.kdT.c])
                    for c8 in range(8):
                        ti, hf = c8 // 2, c8 % 2
                        MM(PTA.t[hf * 64:(hf + 1) * 64, 256 + ti * 64:256 + (ti + 1) * 64], B.kdec.t[:, c8 * 64:(c8 + 1) * 64],
                           B.qx.t[:, c8 * 64:(c8 + 1) * 64], True, True, [B.kdec.c, B.qx.c], [pa_c])
                    TT("dve", B.A.t[:, :], PTA.t[:, 256:512], cb.t[:, C_MASK256:C_MASK256 + 256], ALU.mult, [pa_c, cb.c], [B.A.c])

                def back_u(bi):
                    h, s_, j, B, wh, T0 = geo(bi)
                    for c8 in range(8):
                        ti, hf = c8 // 2, c8 % 2
                        hs = slice(hf * 64, (hf + 1) * 64)
                        ub = PUe if hf == 0 else PUo
                        MM(ub.t[:, ti * 128:(ti + 1) * 128], B.kdT.t[hs, ti, :], B.vtok.t[hs, ti, :], True, True,
                           [B.kdT.c, B.vtok.c], [ub.c])

                def back_chain(bi):
                    h, s_, j, B, wh, T0 = geo(bi)
                    if j == 0:
                        MSET("dve", S.t[:, :], 0.0, [S.c])
                        MSET("dve", Sds[0].t[:, :], 0.0, [Sds[0].c])
                    for c8 in range(8):
                        ti, hf = c8 // 2, c8 % 2
                        first = (j == 0 and c8 == 0)
                        ub = PUe if hf == 0 else PUo
                        if not first:
                            TS("dve", Sds[c8].t[:, :], S.t[:, :], B.eb.t[:, c8:c8 + 1], None, ALU.mult, None, [S.c, B.eb.c], [Sds[c8].c])
                        STT(S.t[:, :], S.t[:, :], B.eb.t[:, c8:c8 + 1], ub.t[:, ti * 128:(ti + 1) * 128], ALU.mult, ALU.add,
                            [S.c, B.eb.c, ub.c], [S.c])
                    for c8 in range(8):
                        ti, hf = c8 // 2, c8 % 2
                        hs = slice(hf * 64, (hf + 1) * 64)
                        MM(PO.t[:, c8 * 64:(c8 + 1) * 64], B.vtok.t[hs, ti, :], B.A.t[hs, ti * 64:(ti + 1) * 64], True, False,
                           [B.vtok.c, B.A.c], [PO.c])
                        MM(PO.t[:, c8 * 64:(c8 + 1) * 64], Sds[c8].t[:, :], B.qx.t[:, c8 * 64:(c8 + 1) * 64], False, True,
                           [Sds[c8].c, B.qx.c], [PO.c])
                    CP("act", B.osb.t[:, :], PO.t[:, :], [PO.c], [B.osb.c])
                    TT("pool", B.osq.t[:, :], B.osb.t[:, :], B.osb.t[:, :], ALU.mult, [B.osb.c], [B.osq.c])
                    MM(PO.t[:, :], ones_bf, B.osq.t[:, :], True, True, [cb.c, B.osq.c], [PO.c])

                def back_norm(bi):
                    h, s_, j, B, wh, T0 = geo(bi)
                    rstd_from(PO.t[:, :], 128, 512, 1.0 / 128, B.lnv, B.rs, [PO.c])
                    TT("dve", B.osb.t[:, :], B.osb.t[:, :], B.rs.t[:, :], ALU.mult, [B.osb.c, B.rs.c], [B.osb.c])
                    STT(B.oa.t[:, :], B.osb.t[:, :], ghg.t[:, 0:1], B.sg.t[:, :], ALU.mult, ALU.mult, [B.osb.c, ghg.c, B.sg.c], [B.oa.c])
                    DMA("sp", oaT[h * 128:(h + 1) * 128, T0:T0 + 512], B.oa.t[:, :], [B.oa.c], [])

                load_w(0)
                front_proj(0)
                front_mid(0)
                front_end(0)
                nb_ = len(batches)
                for bi in range(nb_):
                    h, s_, j = batches[bi]
                    if s_ == 0 and j == 0 and h + 1 < 8:
                        load_w(h + 1)
                    nx = bi + 1 < nb_
                    back_u(bi)
                    if nx:
                        front_proj(bi + 1)
                    back_chain(bi)
                    if nx:
                        front_mid(bi + 1)
                    back_norm(bi)
                    if nx:
                        front_end(bi + 1)
            em.barrier()


        if stage_limit >= 2:
            with contextlib.ExitStack() as st:
                wm = sbt(st, "wm", [128, 8, 768], BF16)
                wuq = sbt(st, "wuq", [128, 3, 1536], BF16)
                wuqs = sbt(st, "wuqs", [128, 3, 512], BF16)
                wukv = sbt(st, "wukv", [128, 2, 2048], BF16)
                gq = sbt(st, "gq", [128, 3], F32)
                gkv = sbt(st, "gkv", [128, 2], F32)
                cos2 = sbt(st, "cos2", [64, T], BF16)
                sin2 = sbt(st, "sin2", [64, T], BF16)
                scl = sbt(st, "scl", [64, 1], F32)
                rope_st = contextlib.ExitStack()
                posi = sbt(rope_st, "posi", [64, 1024], I32)
                ang = sbt(rope_st, "ang", [64, 1024], F32)
                uu = sbt(rope_st, "uu", [64, 1024], F32)
                ui = sbt(rope_st, "ui", [64, 1024], I32)
                uf = sbt(rope_st, "uf", [64, 1024], F32)
                PO, PL = PS[6], PS[7]
                TWO_PI = 2.0 * math.pi
                DMA("pool", wm.t[:, :, 0:704], w_in_v[:, :, 4096:4800], [], [wm.c])
                DMA("pool", wm.t[:, :, 704:768], w_krsw.rearrange("(c p) n -> p c n", p=128), [], [wm.c])
                DMA("pool", wuq.t[:, :, :], w_uq.rearrange("(c p) n -> p c n", p=128), [], [wuq.c])
                DMA("pool", wuqs.t[:, :, :], w_uqsw.rearrange("(c p) n -> p c n", p=128), [], [wuqs.c])
                DMA("pool", wukv.t[:, :, :], w_ukv.rearrange("(c p) n -> p c n", p=128), [], [wukv.c])
                DMA("sp", gq.t[:, :], g_q[:, :], [], [gq.c])
                DMA("sp", gkv.t[:, :], g_kv[:, :], [], [gkv.c])
                TS("dve", scl.t[:, :], cf.t[0:64, C_SGN:C_SGN + 1], TWO_PI * (1.0 - 1e-6), None, ALU.mult, None, [cf.c], [scl.c])
                for blk in range(4):
                    cs = slice(blk * 1024, (blk + 1) * 1024)
                    DMA("sp", posi.t[:, :], pos[0:1, cs].to_broadcast([64, 1024]), [], [posi.c])
                    CP("dve", ang.t[:, :], posi.t[:, :], [posi.c], [ang.c])
                    TS("dve", ang.t[:, :], ang.t[:, :], cf.t[0:64, C_INVF:C_INVF + 1], 1.0 / TWO_PI, ALU.mult, ALU.mult, [ang.c, cf.c], [ang.c])
                    for kind, off in (("sin", 0.0), ("cos", 0.25)):
                        TS("dve", uu.t[:, :], ang.t[:, :], off, None, ALU.add, None, [ang.c], [uu.c])
                        CP("dve", ui.t[:, :], uu.t[:, :], [uu.c], [ui.c])
                        CP("dve", uf.t[:, :], ui.t[:, :], [ui.c], [uf.c])
                        TT("dve", uu.t[:, :], uu.t[:, :], uf.t[:, :], ALU.subtract, [uu.c, uf.c], [uu.c])
                        TS("dve", uf.t[:, :], uu.t[:, :], 0.5, None, ALU.is_gt, None, [uu.c], [uf.c])
                        TT("dve", uu.t[:, :], uu.t[:, :], uf.t[:, :], ALU.subtract, [uu.c, uf.c], [uu.c])
                        TS("dve", uf.t[:, :], uu.t[:, :], -0.5, None, ALU.is_lt, None, [uu.c], [uf.c])
                        TT("dve", uu.t[:, :], uu.t[:, :], uf.t[:, :], ALU.add, [uu.c, uf.c], [uu.c])
                        if kind == "sin":
                            ACT(sin2.t[:, cs], uu.t[:, :], AF.Sin, [uu.c, scl.c], [sin2.c], scale=scl.t[:, 0:1])
                        else:
                            ACT(cos2.t[:, cs], uu.t[:, :], AF.Sin, [uu.c], [cos2.c], scale=TWO_PI * (1.0 - 1e-6))
                em.barrier()
                rope_st.close()
                sqc = [sbt(st, f"sqc{i}", [128, 512], BF16) for i in range(3)]
                lnv = sbt(st, "lnv2", [128, 512], F32)
                rs = sbt(st, "rs2", [128, 512], F32)
                cqn = sbt(st, "cqn", [128, 3, SEQ], BF16)
                ckvn = sbt(st, "ckvn", [128, 2, SEQ], BF16)
                krT = sbt(st, "krT", [64, SEQ], BF16)
                t1 = sbt(st, "t1", [64, 512], F32)
                t2 = sbt(st, "t2", [64, 512], F32)
                KnTs = [sbt(st, f"KnT{i}", [128, SEQ], BF16) for i in range(2)]
                Vhs = [sbt(st, f"Vh{i}", [128, 16, 128], BF16) for i in range(2)]
                qns = [sbt(st, f"qn{i}", [128, 512], BF16) for i in range(2)]
                qrs = [sbt(st, f"qr{i}", [64, 512], BF16) for i in range(2)]
                pts = [sbt(st, f"pt{i}", [128, 512], BF16) for i in range(4)]
                lnl = sbt(st, "lnl", [128, 512], F32)
                rl = sbt(st, "rl", [128, 512], F32)
                obs = [sbt(st, f"ob{i}", [128, 512], BF16) for i in range(2)]
                nbat = 0
                for s in range(2):
                    for j in range(4):
                        T0 = s * SEQ + j * 512
                        L0 = j * 512
                        xc = xnT_c[T0 // 512]
                        for (dst_t, ncc, col0, gt, dim) in ((cqn, 3, 0, gq, 384), (ckvn, 2, 384, gkv, 256)):
                            banks = [nb() for _ in range(ncc)]
                            for cc in range(ncc):
                                for c in range(8):
                                    MM(banks[cc].t[:, :], wm.t[:, c, col0 + cc * 128:col0 + (cc + 1) * 128], xnT.t[:, c, T0:T0 + 512],
                                       c == 0, c == 7, [wm.c, xc], [banks[cc].c])
                            for cc in range(ncc):
                                ACT(sqc[cc].t[:, :], banks[cc].t[:, :], AF.Square, [banks[cc].c], [sqc[cc].c])
                            bs = nb()
                            for cc in range(ncc):
                                MM(bs.t[:, :], ones_bf, sqc[cc].t[:, :], cc == 0, cc == ncc - 1, [cb.c, sqc[cc].c], [bs.c])
                            rstd_from(bs.t[:, :], 128, 512, 1.0 / dim, lnv, rs, [bs.c])
                            for cc in range(ncc):
                                STT(dst_t.t[:, cc, L0:L0 + 512], banks[cc].t[:, :], gt.t[:, cc:cc + 1], rs.t[:, :], ALU.mult, ALU.mult,
                                    [banks[cc].c, gt.c, rs.c], [dst_t.c])
                        bk1, bk2 = nb(), nb()
                        for c in range(8):
                            MM(bk1.t[0:64, :], wm.t[:, c, 640:704], xnT.t[:, c, T0:T0 + 512], c == 0, c == 7, [wm.c, xc], [bk1.c])
                        for c in range(8):
                            MM(bk2.t[0:64, :], wm.t[:, c, 704:768], xnT.t[:, c, T0:T0 + 512], c == 0, c == 7, [wm.c, xc], [bk2.c])
                        TT("dve", t1.t[:, :], bk1.t[0:64, :], cos2.t[:, T0:T0 + 512], ALU.mult, [bk1.c, cos2.c], [t1.c])
                        TT("dve", t2.t[:, :], bk2.t[0:64, :], sin2.t[:, T0:T0 + 512], ALU.mult, [bk2.c, sin2.c], [t2.c])
                        TT("pool", krT.t[:, L0:L0 + 512], t1.t[:, :], t2.t[:, :], ALU.add, [t1.c, t2.c], [krT.c])
                    items = [(h, j) for h in range(8) for j in range(4)]

                    def prologue(k, s=s):
                        h, j = items[k]
                        KnT, Vh = KnTs[h % 2], Vhs[h % 2]
                        qn, qr = qns[k % 2], qrs[k % 2]
                        T0 = s * SEQ + j * 512
                        L0 = j * 512
                        bkk = nb()
                        for c in range(2):
                            MM(bkk.t[:, :], wukv.t[:, c, h * 256:h * 256 + 128], ckvn.t[:, c, L0:L0 + 512], c == 0, c == 1,
                               [wukv.c, ckvn.c], [bkk.c])
                        CP("act", KnT.t[:, L0:L0 + 512], bkk.t[:, :], [bkk.c], [KnT.c])
                        bv = nb()
                        for ti in range(4):
                            for c in range(2):
                                MM(bv.t[:, ti * 128:(ti + 1) * 128], ckvn.t[:, c, L0 + ti * 128:L0 + (ti + 1) * 128],
                                   wukv.t[:, c, h * 256 + 128:h * 256 + 256], c == 0, c == 1, [wukv.c, ckvn.c], [bv.c])
                        CP("dve", Vh.t[:, j * 4:(j + 1) * 4, :], bv.t[:, :].rearrange("p (a b) -> p a b", a=4), [bv.c], [Vh.c])
                        bq, bp, bps = nb(), nb(), nb()
                        for c in range(3):
                            MM(bq.t[:, :], wuq.t[:, c, h * 192:h * 192 + 128], cqn.t[:, c, L0:L0 + 512], c == 0, c == 2, [wuq.c, cqn.c], [bq.c])
                        for c in range(3):
                            MM(bp.t[0:64, :], wuq.t[:, c, h * 192 + 128:h * 192 + 192], cqn.t[:, c, L0:L0 + 512], c == 0, c == 2,
                               [wuq.c, cqn.c], [bp.c])
                        for c in range(3):
                            MM(bps.t[0:64, :], wuqs.t[:, c, h * 64:(h + 1) * 64], cqn.t[:, c, L0:L0 + 512], c == 0, c == 2,
                               [wuqs.c, cqn.c], [bps.c])
                        CP("act", qn.t[:, :], bq.t[:, :], [bq.c], [qn.c])
                        TT("dve", t1.t[:, :], bp.t[0:64, :], cos2.t[:, T0:T0 + 512], ALU.mult, [bp.c, cos2.c], [t1.c])
                        TT("dve", t2.t[:, :], bps.t[0:64, :], sin2.t[:, T0:T0 + 512], ALU.mult, [bps.c, sin2.c], [t2.c])
                        TT("pool", qr.t[:, :], t1.t[:, :], t2.t[:, :], ALU.add, [t1.c, t2.c], [qr.c])

                    def attention(k, s=s):
                        h, j = items[k]
                        KnT, Vh = KnTs[h % 2], Vhs[h % 2]
                        qn, qr = qns[k % 2], qrs[k % 2]
                        T0 = s * SEQ + j * 512
                        nkt = 4 * j + 4

                        def col0(kt):
                            return max(0, kt - 4 * j) * 128

                        def s_mm(kt):
                            bst = nb()
                            c0 = col0(kt)
                            MM(bst.t[:, c0:512], KnT.t[:, kt * 128:(kt + 1) * 128], qn.t[:, c0:512], True, False, [KnT.c, qn.c], [bst.c])
                            MM(bst.t[:, c0:512], krT.t[:, kt * 128:(kt + 1) * 128], qr.t[:, c0:512], False, True, [krT.c, qr.c], [bst.c])
                            return bst
                        pend = [s_mm(0), s_mm(1)]
                        for kt in range(nkt):
                            bst = pend.pop(0)
                            if kt + 2 < nkt:
                                pend.append(s_mm(kt + 2))
                            pt = pts[kt % 4]
                            c0 = col0(kt)
                            ACT(pt.t[:, c0:512], bst.t[:, c0:512], AF.Exp, [bst.c], [pt.c], scale=192.0 ** -0.5)
                            if kt >= 4 * j:
                                TT("dve", pt.t[:, c0:c0 + 128], pt.t[:, c0:c0 + 128], cb.t[:, C_TRILE:C_TRILE + 128], ALU.mult,
                                   [pt.c, cb.c], [pt.c])
                            MM(PO.t[:, c0:512], Vh.t[:, kt, :], pt.t[:, c0:512], kt == 0, kt == nkt - 1, [Vh.c, pt.c], [PO.c])
                            MM(PL.t[:, c0:512], ones_bf, pt.t[:, c0:512], kt == 0, kt == nkt - 1, [cb.c, pt.c], [PL.c])
                        ACT(lnl.t[:, :], PL.t[:, :], AF.Ln, [PL.c], [lnl.c])
                        ACT(rl.t[:, :], lnl.t[:, :], AF.Exp, [lnl.c], [rl.c], scale=-1.0)
                        ob = obs[k % 2]
                        TT("dve", ob.t[:, :], PO.t[:, :], rl.t[:, :], ALU.mult, [PO.c, rl.c], [ob.c])
                        DMA("sp", obT[h * 128:(h + 1) * 128, T0:T0 + 512], ob.t[:, :], [ob.c], [])

                    prologue(0)
                    for k in range(len(items)):
                        if k + 1 < len(items):
                            prologue(k + 1)
                        attention(k)
            em.barrier()

        if stage_limit >= 3:
            with contextlib.ExitStack() as st:
                wg = sbt(st, "wg", [128, 8, 2048], BF16)
                wbh = sbt(st, "wbh", [128, 8, D], BF16)
                wbm = sbt(st, "wbm", [128, 8, D], BF16)
                wo = sbt(st, "wo", [128, 8, D], BF16)
                wr = sbt(st, "wr", [128, 8, 72], F32)
                br = sbt(st, "br", [128, 72], F32)
                g2 = sbt(st, "g2", [128, D], F32)
                oabs = [sbt(st, f"oab{i}", [128, 8, 128], BF16) for i in range(2)]
                obbs = [sbt(st, f"obb{i}", [128, 8, 128], BF16) for i in range(2)]
                Ta = sbt(st, "Ta", [128, D], F32)
                Tb = sbt(st, "Tb", [128, D], F32)
                ybf = sbt(st, "ybf", [128, D], BF16)
                yT = sbt(st, "yT", [128, 8, 128], BF16)
                h2bfs = [sbt(st, f"h2bf{i}", [128, D], BF16) for i in range(2)]
                h2T = sbt(st, "h2T", [128, 8, 128], F32)
                ssq = sbt(st, "ssq3", [128, 2], F32)
                lnv = sbt(st, "lnv3", [128, 2], F32)
                rstd = sbt(st, "rstd3", [128, 2], F32)
                lg = sbt(st, "lg", [128, 72], F32)
                g8 = sbt(st, "g8", [128, 8], F32)
                ohg = sbt(st, "ohg", [128, 8], F32)
                ngm = sbt(st, "ngm", [128, 1], F32)
                ex = sbt(st, "ex", [128, 8], F32)
                gs = sbt(st, "gs", [128, 1], F32)
                gw = sbt(st, "gw", [128, 1], F32)
                pen = sbt(st, "pen", [128, 8], F32)
                msk = sbt(st, "msk", [128, 64], F32)
                m8 = sbt(st, "m8", [128, 8], F32)
                i8 = sbt(st, "i8", [128, 8], U32)
                idf = sbt(st, "idf", [128, 2], F32)
                dlt = sbt(st, "dlt", [128, 1], F32)
                ed = sbt(st, "ed", [128, 1], F32)
                den = sbt(st, "den", [128, 1], F32)
                e1 = sbt(st, "e1", [128, 1], F32)
                e2 = sbt(st, "e2", [128, 1], F32)
                oh1 = sbt(st, "oh1", [128, 64], F32)
                oh2 = sbt(st, "oh2", [128, 64], F32)
                oh = sbt(st, "oh", [128, 64], F32)
                tmp64 = sbt(st, "tmp64", [128, 64], F32)
                sl = sbt(st, "sl", [128, 2], F32)
                ovf = sbt(st, "ovf", [128, 2], F32)
                dsf = sbt(st, "dsf", [128, 2], F32)
                base = sbt(st, "base", [1, 64], F32)
                xg_c = em.cell("xg")
                DMA("pool", wg.t[:, :, :], w_in_v[:, :, 4800:6848], [], [wg.c])
                DMA("pool", wbh.t[:, :, :], w_bh.rearrange("(c p) n -> p c n", p=128), [], [wbh.c])
                DMA("pool", wbm.t[:, :, :], w_bm.rearrange("(c p) n -> p c n", p=128), [], [wbm.c])
                DMA("pool", wo.t[:, :, :], w_out.rearrange("(c p) n -> p c n", p=128), [], [wo.c])
                DMA("sp", wr.t[:, :, :], w_r.rearrange("(c p) n -> p c n", p=128), [], [wr.c])
                DMA("sp", br.t[:, :], b_r[0:1, :].to_broadcast([128, 72]), [], [br.c])
                DMA("sp", g2.t[:, :], g_ffn[0:1, :].to_broadcast([128, D]), [], [g2.c])
                MSET("dve", base.t[:, :], 0.0, [base.c])
                oaT_v = oaT.rearrange("(c p) t -> p c t", p=128)
                obT_v = obT.rearrange("(c p) t -> p c t", p=128)
                ident_f = cf.t[:, C_IDENT:C_IDENT + 128]
                iota = cf.t[:, C_IOTA:C_IOTA + 64]
                Tas = [Ta, sbt(st, "Ta1", [128, D], F32)]
                Tbs = [Tb, sbt(st, "Tb1", [128, D], F32)]
                ybfs = [ybf, sbt(st, "ybf1", [128, D], BF16)]
                NT = T // 128

                def f_load(i):
                    tok = slice(i * 128, (i + 1) * 128)
                    DMA("sp", oabs[i % 2].t[:, :, :], oaT_v[:, :, tok], [], [oabs[i % 2].c])
                    DMA("sp", obbs[i % 2].t[:, :, :], obT_v[:, :, tok], [], [obbs[i % 2].c])

                def f_gate(i, which):
                    tok = slice(i * 128, (i + 1) * 128)
                    xc = xnT_c[i // 4]
                    Tt = (Tas if which == 0 else Tbs)[i % 2]
                    goff = 0 if which == 0 else 1024
                    for hf in range(2):
                        b = nb()
                        for c in range(8):
                            MM(b.t[:, :], xnT.t[:, c, tok], wg.t[:, c, goff + hf * 512:goff + (hf + 1) * 512], c == 0, c == 7,
                               [xc, wg.c], [b.c])
                        ACT(Tt.t[:, hf * 512:(hf + 1) * 512], b.t[:, :], AF.Sigmoid, [b.c], [Tt.c])

                def f_branch(i, which):
                    Tt = (Tas if which == 0 else Tbs)[i % 2]
                    src = (oabs if which == 0 else obbs)[i % 2]
                    wb = wbh if which == 0 else wbm
                    for hf in range(2):
                        b = nb()
                        for c in range(8):
                            MM(b.t[:, :], src.t[:, c, :], wb.t[:, c, hf * 512:(hf + 1) * 512], c == 0, c == 7, [src.c, wb.c], [b.c])
                        TT("dve", Tt.t[:, hf * 512:(hf + 1) * 512], b.t[:, :], Tt.t[:, hf * 512:(hf + 1) * 512], ALU.mult, [b.c, Tt.c], [Tt.c])
                    if which == 1:
                        TT("pool", ybfs[i % 2].t[:, :], Tas[i % 2].t[:, :], Tbs[i % 2].t[:, :], ALU.add,
                           [Tas[i % 2].c, Tbs[i % 2].c], [ybfs[i % 2].c])

                def b1(i):
                    tok = slice(i * 128, (i + 1) * 128)
                    yb_, Tb_ = ybfs[i % 2], Tbs[i % 2]
                    bt = nb()
                    btb = bt.t[:, :].bitcast(BF16)
                    for c in range(8):
                        TR(btb[:, c * 128:(c + 1) * 128], yb_.t[:, c * 128:(c + 1) * 128], ident_bf, [yb_.c, cb.c], [bt.c])
                    CP("act", yT.t[:, :, :], btb.rearrange("p (c t) -> p c t", c=8), [bt.c], [yT.c])
                    DMA("sp", Tb_.t[:, :], x[tok, :], [], [Tb_.c])

                def b2(i):
                    tok = slice(i * 128, (i + 1) * 128)
                    Ta_, Tb_, yb_ = Tas[i % 2], Tbs[i % 2], ybfs[i % 2]
                    h2bf = h2bfs[i % 2]
                    for hf in range(2):
                        b = nb()
                        for c in range(8):
                            MM(b.t[:, :], yT.t[:, c, :], wo.t[:, c, hf * 512:(hf + 1) * 512], c == 0, c == 7, [yT.c, wo.c], [b.c])
                        TT("dve", Ta_.t[:, hf * 512:(hf + 1) * 512], b.t[:, :], Tb_.t[:, hf * 512:(hf + 1) * 512], ALU.add, [b.c, Tb_.c], [Ta_.c])
                    DMA("sp", x1d[tok, :], Ta_.t[:, :], [Ta_.c], [])
                    ACT(yb_.t[:, :], Ta_.t[:, :], AF.Square, [Ta_.c], [yb_.c, ssq.c], accum_out=ssq.t[:, 0:1])
                    rstd_from(ssq.t[:, 0:1], 128, 1, 1.0 / D, lnv, rstd, [ssq.c])
                    STT(Tb_.t[:, :], Ta_.t[:, :], rstd.t[:, 0:1], g2.t[:, :], ALU.mult, ALU.mult, [Ta_.c, rstd.c, g2.c], [Tb_.c])
                    CP("pool", h2bf.t[:, :], Tb_.t[:, :], [Tb_.c], [h2bf.c])

                def b3(i):
                    Tb_ = Tbs[i % 2]
                    p1, p2 = nb(), nb()
                    for c in range(8):
                        bb = p1 if c < 4 else p2
                        TR(bb.t[:, (c % 4) * 128:(c % 4 + 1) * 128], Tb_.t[:, c * 128:(c + 1) * 128], ident_f, [Tb_.c, cf.c], [bb.c])
                    CP("act", h2T.t[:, 0:4, :], p1.t[:, :].rearrange("p (c t) -> p c t", c=4), [p1.c], [h2T.c])
                    CP("act", h2T.t[:, 4:8, :], p2.t[:, :].rearrange("p (c t) -> p c t", c=4), [p2.c], [h2T.c])

                def b4(i):
                    bl = nb()
                    for c in range(8):
                        MM(bl.t[:, 0:72], h2T.t[:, c, :], wr.t[:, c, :], c == 0, c == 7, [h2T.c, wr.c], [bl.c])
                    TT("dve", lg.t[:, :], bl.t[:, 0:72], br.t[:, :], ALU.add, [bl.c, br.c], [lg.c])
                    em.op("dve", lambda e: e.max(out=g8.t[:, :], in_=lg.t[:, 0:8]), [lg.c], [g8.c])
                    TS("dve", ohg.t[:, :], lg.t[:, 0:8], g8.t[:, 0:1], None, ALU.is_equal, None, [lg.c, g8.c], [ohg.c])
                    TS("dve", ngm.t[:, :], g8.t[:, 0:1], -1.0, None, ALU.mult, None, [g8.c], [ngm.c])
                    ACT(ex.t[:, :], lg.t[:, 0:8], AF.Exp, [lg.c, ngm.c], [ex.c, gs.c], bias=ngm.t[:, 0:1], accum_out=gs.t[:, 0:1])
                    em.op("dve", lambda e: e.reciprocal(out=gw.t[:, :], in_=gs.t[:, :]), [gs.c], [gw.c])
                    TS("dve", pen.t[:, :], ohg.t[:, :], -1.0, 1e30, ALU.add, ALU.mult, [ohg.c], [pen.c])
                    TT("dve", msk.t[:, :].rearrange("p (a b) -> p a b", a=8), lg.t[:, 8:72].rearrange("p (a b) -> p a b", a=8),
                       pen.t[:, :].rearrange("p (a b) -> p a b", b=1).to_broadcast([128, 8, 8]), ALU.add, [lg.c, pen.c], [msk.c])
                    em.op("dve", lambda e: e.max(out=m8.t[:, :], in_=msk.t[:, :]), [msk.c], [m8.c])
                    em.op("dve", lambda e: e.max_index(out=i8.t[:, :], in_max=m8.t[:, :], in_values=msk.t[:, :]), [m8.c, msk.c], [i8.c])
                    CP("dve", idf.t[:, :], i8.t[:, 0:2], [i8.c], [idf.c])
                    TT("dve", dlt.t[:, :], m8.t[:, 1:2], m8.t[:, 0:1], ALU.subtract, [m8.c], [dlt.c])
                    ACT(ed.t[:, :], dlt.t[:, :], AF.Exp, [dlt.c], [ed.c])
                    TS("dve", den.t[:, :], ed.t[:, :], 1.0, None, ALU.add, None, [ed.c], [den.c])
                    em.op("dve", lambda e: e.reciprocal(out=e1.t[:, :], in_=den.t[:, :]), [den.c], [e1.c])
                    TT("dve", e2.t[:, :], ed.t[:, :], e1.t[:, :], ALU.mult, [ed.c, e1.c], [e2.c])
                    TT("dve", wts.t[:, 2 * i:2 * i + 1], e1.t[:, :], gw.t[:, :], ALU.mult, [e1.c, gw.c], [wts.c])
                    TT("dve", wts.t[:, 2 * i + 1:2 * i + 2], e2.t[:, :], gw.t[:, :], ALU.mult, [e2.c, gw.c], [wts.c])
                    TS("dve", oh1.t[:, :], iota, idf.t[:, 0:1], None, ALU.is_equal, None, [cf.c, idf.c], [oh1.c])
                    TS("dve", oh2.t[:, :], iota, idf.t[:, 1:2], None, ALU.is_equal, None, [cf.c, idf.c], [oh2.c])
                    TT("dve", oh.t[:, :], oh1.t[:, :], oh2.t[:, :], ALU.add, [oh1.c, oh2.c], [oh.c])

                def b5(i):
                    h2bf = h2bfs[i % 2]
                    bp_ = nb()
                    MM(bp_.t[:, 0:64], cf.t[:, C_TRIST:C_TRIST + 128], oh.t[:, :], True, False, [cf.c, oh.c], [bp_.c])
                    MM(bp_.t[:, 0:64], cf.t[0:1, C_ONES:C_ONES + 128], base.t[0:1, :], False, True, [cf.c, base.c], [bp_.c])
                    bc_ = nb()
                    MM(bc_.t[0:1, 0:64], cf.t[:, C_ONES:C_ONES + 1], oh.t[:, :], True, True, [cf.c, oh.c], [bc_.c])
                    TT("dve", tmp64.t[:, :], bp_.t[:, 0:64], oh1.t[:, :], ALU.mult, [bp_.c, oh1.c], [tmp64.c])
                    em.op("dve", lambda e: e.reduce_sum(out=sl.t[:, 0:1], in_=tmp64.t[:, :], axis=mybir.AxisListType.X), [tmp64.c], [sl.c])
                    TT("dve", tmp64.t[:, :], bp_.t[:, 0:64], oh2.t[:, :], ALU.mult, [bp_.c, oh2.c], [tmp64.c])
                    em.op("dve", lambda e: e.reduce_sum(out=sl.t[:, 1:2], in_=tmp64.t[:, :], axis=mybir.AxisListType.X), [tmp64.c], [sl.c])
                    TT("dve", base.t[0:1, :], base.t[0:1, :], bc_.t[0:1, 0:64], ALU.add, [base.c, bc_.c], [base.c])
                    TS("dve", ovf.t[:, :], sl.t[:, :], float(CAP), 1e6, ALU.is_ge, ALU.mult, [sl.c], [ovf.c])
                    STT(dsf.t[:, :], idf.t[:, :], float(CAP), sl.t[:, :], ALU.mult, ALU.add, [idf.c, sl.c], [dsf.c])
                    TT("dve", dsf.t[:, :], dsf.t[:, :], ovf.t[:, :], ALU.add, [dsf.c, ovf.c], [dsf.c])
                    TS("dve", dsf.t[:, :], dsf.t[:, :], float(NROWS), None, ALU.min, None, [dsf.c], [dsf.c])
                    CP("dve", dst.t[:, 2 * i:2 * i + 2], dsf.t[:, :], [dsf.c], [dst.c])
                    for k in range(2):
                        em.op("pool", lambda e, k=k, i=i, h2bf=h2bf: e.indirect_dma_start(
                            out=xg[:, :], out_offset=bass.IndirectOffsetOnAxis(ap=dst.t[:, 2 * i + k:2 * i + k + 1], axis=0),
                            in_=h2bf.t[:, :], in_offset=None), [dst.c, h2bf.c], [xg_c], kind="d")

                f_load(0)
                f_gate(0, 0)
                f_branch(0, 0)
                f_gate(0, 1)
                f_branch(0, 1)
                for i in range(NT):
                    nx = i + 1 < NT
                    if nx:
                        f_load(i + 1)
                    b1(i)
                    if nx:
                        f_gate(i + 1, 0)
                    b2(i)
                    if nx:
                        f_branch(i + 1, 0)
                    b3(i)
                    if nx:
                        f_gate(i + 1, 1)
                    b4(i)
                    if nx:
                        f_branch(i + 1, 1)
                    b5(i)
            em.barrier()

        if stage_limit >= 4:
            with contextlib.ExitStack() as st:
                NR = CAP // 128
                w1s = [sbt(st, f"w1s{i}", [128, 8, 512], BF16) for i in range(3)]
                w3s = [sbt(st, f"w3s{i}", [128, 8, 512], BF16) for i in range(3)]
                w2s = [sbt(st, f"w2s{i}", [128, 4, D], BF16) for i in range(3)]
                xgs = [sbt(st, f"xgs{i}", [128, NR, D], BF16) for i in range(2)]
                xgT = [sbt(st, f"xgT{i}", [128, 8, CAP], BF16) for i in range(2)]
                s1s = [sbt(st, f"s1s{i}", [128, CAP], F32) for i in range(2)]
                hid = [sbt(st, f"hid{i}", [128, 4, CAP], BF16) for i in range(2)]
                ysb = [sbt(st, f"ysb{i}", [128, D], F32) for i in range(2)]
                nsi = [0]

                def load_e(ex_):
                    sl_ = ex_ % 3
                    DMA("pool", w1s[sl_].t[:, :, :], ew1[ex_].rearrange("(c p) n -> p c n", p=128), [], [w1s[sl_].c])
                    DMA("pool", w3s[sl_].t[:, :, :], ew3[ex_].rearrange("(c p) n -> p c n", p=128), [], [w3s[sl_].c])
                    DMA("pool", w2s[sl_].t[:, :, :], ew2[ex_].rearrange("(c p) n -> p c n", p=128), [], [w2s[sl_].c])
                    xs = xgs[ex_ % 2]
                    DMA("sp", xs.t[:, :, :], xg[ex_ * CAP:(ex_ + 1) * CAP, :].rearrange("(r p) d -> p r d", p=128), [], [xs.c])

                def front_e(ex_):
                    sl_ = ex_ % 3
                    xs = xgs[ex_ % 2]
                    xT = xgT[ex_ % 2]
                    hd = hid[ex_ % 2]
                    for r in range(NR):
                        bt = nb()
                        btb = bt.t[:, :].bitcast(BF16)
                        for c in range(8):
                            TR(btb[:, c * 128:(c + 1) * 128], xs.t[:, r, c * 128:(c + 1) * 128], ident_bf, [xs.c, cb.c], [bt.c])
                        CP("act" if r == 0 else "dve", xT.t[:, :, r * 128:(r + 1) * 128], btb.rearrange("p (c t) -> p c t", c=8), [bt.c], [xT.c])
                    for fc in range(4):
                        b1, b3 = nb(), nb()
                        for c in range(8):
                            MM(b1.t[:, 0:CAP], w1s[sl_].t[:, c, fc * 128:(fc + 1) * 128], xT.t[:, c, :], c == 0, c == 7, [w1s[sl_].c, xT.c], [b1.c])
                        for c in range(8):
                            MM(b3.t[:, 0:CAP], w3s[sl_].t[:, c, fc * 128:(fc + 1) * 128], xT.t[:, c, :], c == 0, c == 7, [w3s[sl_].c, xT.c], [b3.c])
                        s1 = s1s[nsi[0] % 2]
                        nsi[0] += 1
                        ACT(s1.t[:, :], b1.t[:, 0:CAP], AF.Silu, [b1.c], [s1.c])
                        TT("dve", hd.t[:, fc, :], b3.t[:, 0:CAP], s1.t[:, :], ALU.mult, [b3.c, s1.c], [hd.c])

                def back_e(ex_):
                    sl_ = ex_ % 3
                    hd = hid[ex_ % 2]
                    for r in range(NR):
                        ys = ysb[r % 2]
                        for hf in range(2):
                            b = PS[6 + hf]
                            for fc in range(4):
                                MM(b.t[:, :], hd.t[:, fc, r * 128:(r + 1) * 128], w2s[sl_].t[:, fc, hf * 512:(hf + 1) * 512], fc == 0, fc == 3,
                                   [hd.c, w2s[sl_].c], [b.c])
                            CP("act" if hf == 0 else "dve", ys.t[:, hf * 512:(hf + 1) * 512], b.t[:, :], [b.c], [ys.c])
                        DMA("sp", yb[ex_ * CAP + r * 128:ex_ * CAP + (r + 1) * 128, :], ys.t[:, :], [ys.c], [])

                load_e(0)
                load_e(1)
                front_e(0)
                for ex_ in range(NEXP):
                    if ex_ + 2 < NEXP:
                        load_e(ex_ + 2)
                    if ex_ + 1 < NEXP:
                        front_e(ex_ + 1)
                    back_e(ex_)
            em.barrier()

        if stage_limit >= 5:
            with contextlib.ExitStack() as st:
                y0s = [sbt(st, f"y0s{i}", [128, D], F32) for i in range(4)]
                y1s = [sbt(st, f"y1s{i}", [128, D], F32) for i in range(4)]
                x1s = [sbt(st, f"x1s{i}", [128, D], F32) for i in range(4)]
                accs = [sbt(st, f"acc{i}", [128, D], F32) for i in range(4)]
                junk = sbt(st, "junk5", [128, D], BF16)
                gF = sbt(st, "gF", [128, D], F32)
                ssq = sbt(st, "ssq5", [128, 2], F32)
                lnv = sbt(st, "lnv5", [128, 2], F32)
                rstd = sbt(st, "rstd5", [128, 2], F32)
                DMA("sp", gF.t[:, :], g_fin[0:1, :].to_broadcast([128, D]), [], [gF.c])
                ssqs = [sbt(st, f"ssq5_{i}", [128, 2], F32) for i in range(2)]
                lnvs = [sbt(st, f"lnv5_{i}", [128, 2], F32) for i in range(2)]
                rstds = [sbt(st, f"rstd5_{i}", [128, 2], F32) for i in range(2)]

                def s5_pre(i):
                    tok = slice(i * 128, (i + 1) * 128)
                    y0, y1, x1t, acc = y0s[i % 4], y1s[i % 4], x1s[i % 4], accs[i % 4]
                    for k, yt in ((0, y0), (1, y1)):
                        em.op("pool", lambda e, k=k, i=i, yt=yt: e.indirect_dma_start(
                            out=yt.t[:, :], out_offset=None, in_=yb[:, :],
                            in_offset=bass.IndirectOffsetOnAxis(ap=dst.t[:, 2 * i + k:2 * i + k + 1], axis=0)), [dst.c], [yt.c], kind="d")
                    DMA("sp", x1t.t[:, :], x1d[tok, :], [], [x1t.c])
                    STT(acc.t[:, :], y0.t[:, :], wts.t[:, 2 * i:2 * i + 1], x1t.t[:, :], ALU.mult, ALU.add, [y0.c, wts.c, x1t.c], [acc.c])
                    STT(acc.t[:, :], y1.t[:, :], wts.t[:, 2 * i + 1:2 * i + 2], acc.t[:, :], ALU.mult, ALU.add, [y1.c, wts.c, acc.c], [acc.c])
                    sq_, ln_, rs_ = ssqs[i % 2], lnvs[i % 2], rstds[i % 2]
                    ACT(junk.t[:, :], acc.t[:, :], AF.Square, [acc.c], [junk.c, sq_.c], accum_out=sq_.t[:, 0:1])
                    rstd_from(sq_.t[:, 0:1], 128, 1, 1.0 / D, ln_, rs_, [sq_.c])

                def s5_post(i):
                    tok = slice(i * 128, (i + 1) * 128)
                    acc = accs[i % 4]
                    rs_ = rstds[i % 2]
                    STT(acc.t[:, :], acc.t[:, :], rs_.t[:, 0:1], gF.t[:, :], ALU.mult, ALU.mult, [acc.c, rs_.c, gF.c], [acc.c])
                    DMA("sp", out[tok, :], acc.t[:, :], [acc.c], [])

                s5_pre(0)
                for i in range(T // 128):
                    if i + 1 < T // 128:
                        s5_pre(i + 1)
                    s5_post(i)

        stats = em.emit()
    return nc, stats


_CACHE = {}


def prep_inputs(inputs):
    f = lambda a: np.ascontiguousarray(np.asarray(a, dtype=np.float32))

    def pc(w, nch):
        e, r, n = w.shape
        return np.ascontiguousarray(w.reshape(e, nch, 128, n).transpose(0, 2, 1, 3)).reshape(e, 128, nch * n)
    x = f(inputs["x"])
    positions = np.asarray(inputs["positions"]).astype(np.int32)
    w_in = f(inputs["w_in"][0])
    kr0 = 4096 + 384 + 256
    w_krsw = np.ascontiguousarray(np.concatenate([w_in[:, kr0 + 32:kr0 + 64], w_in[:, kr0:kr0 + 32]], axis=1))
    hl = f(inputs["hg_lower_bound"])
    lbT = np.ascontiguousarray(hl.reshape(2, 8, 128).transpose(2, 0, 1).reshape(128, 16))
    w_uq = f(inputs["mla_w_uq"][0])
    uq3 = w_uq.reshape(384, 8, 192)
    w_uqsw = np.ascontiguousarray(np.concatenate([uq3[:, :, 160:192], uq3[:, :, 128:160]], axis=2).reshape(384, 512))
    shared = {
        "cst": make_consts(),
        "w_in": w_in,
        "w_krsw": w_krsw,
        "g_attn": f(inputs["attn_norm_w"]).reshape(1, D),
        "g_ffn": f(inputs["ffn_norm_w"]).reshape(1, D),
        "g_fin": f(inputs["final_norm_w"]).reshape(1, D),
        "lbT": lbT,
        "g_hg": f(inputs["hg_out_norm_w"]).reshape(128, 1),
        "g_q": np.ascontiguousarray(f(inputs["mla_q_norm_w"]).reshape(3, 128).T),
        "g_kv": np.ascontiguousarray(f(inputs["mla_kv_norm_w"]).reshape(2, 128).T),
        "w_uq": w_uq,
        "w_uqsw": w_uqsw,
        "w_ukv": f(inputs["mla_w_ukv"][0]),
        "w_bh": f(inputs["w_branch_hgrn"][0]),
        "w_bm": f(inputs["w_branch_mla"][0]),
        "w_out": f(inputs["w_out"][0]),
        "w_r": np.ascontiguousarray(np.concatenate([f(inputs["router_group_w"][0]), f(inputs["router_expert_w"][0])], axis=1)),
        "b_r": np.ascontiguousarray(np.concatenate([f(inputs["router_group_b"][0]), f(inputs["router_expert_b"][0])]).reshape(1, 72)),
        "ew1": f(inputs["expert_w1"][0]),
        "ew3": f(inputs["expert_w3"][0]),
        "ew2": f(inputs["expert_w2"][0]),
    }
    in_maps = []
    for c in range(NCORES):
        m = dict(shared)
        m["x"] = np.ascontiguousarray(x[2 * c:2 * c + 2].reshape(T, D))
        m["pos"] = np.ascontiguousarray(positions[2 * c:2 * c + 2].reshape(1, T))
        in_maps.append(m)
    return in_maps


def kernel(**inputs):
    if "nc" not in _CACHE:
        _CACHE["nc"] = build()[0]
    nc = _CACHE["nc"]
    in_maps = prep_inputs(inputs)
    res = run_bass_kernel_spmd(nc, in_maps, core_ids=list(range(NCORES)))
    outs = [np.asarray(r["out"]).reshape(2, SEQ, D) for r in res.results]
    return np.concatenate(outs, axis=0).astype(np.float32)
```

```python
import math
import contextlib
import numpy as np
import concourse.bass as bass
import concourse.mybir as mybir
from concourse.bass_utils import run_bass_kernel_spmd

F32 = mybir.dt.float32
BF16 = mybir.dt.bfloat16
I32 = mybir.dt.int32
U32 = mybir.dt.uint32
AF = mybir.ActivationFunctionType
ALU = mybir.AluOpType

NCORES = 8
T = 4096
SEQ = 2048
D = 1024
EPS = 1e-6
CAP = 256
NEXP = 64
NROWS = NEXP * CAP
SAME_ENG_SYNC = True
N_DMA_SEMS = 40
SCRATCH_INTERNAL = True

C_IDENT = 0
C_TRILE = 128
C_TRIST = 256
C_MASK256 = 384
C_RESET = 640
C_IOTA = 1152
C_INVF = 1216
C_SGN = 1217
C_ONES = 1218
C_AMASK = 1346
NCONST = C_AMASK + 2048


def make_consts():
    c = np.zeros((128, NCONST), np.float32)
    p = np.arange(128)[:, None]
    f = np.arange(128)[None, :]
    c[:, C_IDENT:C_IDENT + 128] = (p == f)
    c[:, C_TRILE:C_TRILE + 128] = (p <= f)
    c[:, C_TRIST:C_TRIST + 128] = (p < f)
    f256 = np.arange(256)[None, :]
    c[:, C_MASK256:C_MASK256 + 256] = ((p % 64) <= (f256 % 64))
    f512 = np.arange(512)[None, :]
    c[:, C_RESET:C_RESET + 512] = ((f512 % 64) != 0)
    c[:, C_IOTA:C_IOTA + 64] = np.arange(64)[None, :]
    half = 32
    inv_freq = (10000.0 ** (-np.arange(half, dtype=np.float32) / half)).astype(np.float32)
    c[:, C_INVF] = inv_freq[np.arange(128) % 32]
    c[:, C_SGN] = np.where((np.arange(128) % 64) < 32, -1.0, 1.0)
    c[:, C_ONES:C_ONES + 128] = 1.0
    for r in range(4):
        c[:, C_AMASK + r * 512:C_AMASK + (r + 1) * 512] = ((p + r * 128) <= f512)
    return c


class Cell:
    __slots__ = ("name", "w", "r")

    def __init__(self, name):
        self.name = name
        self.w = None
        self.r = []


class TB:
    def __init__(self, t, c):
        self.t = t
        self.c = c


class Emitter:
    def __init__(self, nc, es):
        self.nc = nc
        self.es = es
        self.eng = {"pe": nc.tensor, "act": nc.scalar, "dve": nc.vector, "pool": nc.gpsimd, "sp": nc.sync}
        self.ops = []
        self.last = {}
        self.dmas_since = []

    def cell(self, name="c"):
        return Cell(name)

    def op(self, eng, fn, reads=(), writes=(), kind="c"):
        oid = len(self.ops)
        deps = set()
        for c in reads:
            if c.w is not None:
                deps.add(c.w)
        for c in writes:
            if c.w is not None:
                deps.add(c.w)
            deps.update(c.r)
        self.ops.append(dict(eng=eng, fn=fn, deps=deps, kind=kind, sig=False))
        for c in reads:
            if kind == "c":
                c.r = [q for q in c.r if not (self.ops[q]["kind"] == "c" and self.ops[q]["eng"] == eng)]
            c.r.append(oid)
        for c in writes:
            c.w = oid
            c.r = []
        if kind == "d":
            self.dmas_since.append(oid)
        else:
            self.last[eng] = oid
        return oid

    def barrier(self):
        deps = set(self.last.values()) | set(self.dmas_since)
        for e in self.eng:
            self.ops.append(dict(eng=e, fn=None, deps=set(deps), kind="b", sig=False))
        self.dmas_since = []

    def emit(self):
        nc = self.nc
        ops = self.ops

        def skip(po, o):
            if po["kind"] != "c" or o["kind"] != "c":
                return False
            if po["eng"] == "pe" and o["eng"] == "pe":
                return True
            if (not SAME_ENG_SYNC) and po["eng"] == o["eng"]:
                return True
            return False

        for o in ops:
            for d in o["deps"]:
                po = ops[d]
                if po["kind"] == "c" and not skip(po, o):
                    po["sig"] = True
        sems = {e: self.es.enter_context(nc.semaphore("s_" + e)) for e in self.eng}
        dpool = {}
        for q in ("sp", "pool", "act"):
            n = N_DMA_SEMS if q != "act" else 4
            dpool[q] = dict(sems=[self.es.enter_context(nc.semaphore(f"s_d{q}{i}")) for i in range(n)],
                            cnt=[0] * n, last=[None] * n, nxt=0, n=n)
        cnt = {e: 0 for e in self.eng}
        waited = {e: {} for e in self.eng}
        tok = [None] * len(ops)
        nw = 0

        def wait(e, key, sem, val):
            nonlocal nw
            if waited[e].get(key, 0) >= val:
                return
            self.eng[e].wait_ge(sem, val)
            waited[e][key] = val
            nw += 1

        for oid, o in enumerate(ops):
            e = o["eng"]
            for d in sorted(o["deps"]):
                po = ops[d]
                if skip(po, o):
                    continue
                if tok[d] is None:
                    continue
                key, sem, val = tok[d]
                wait(e, key, sem, val)
            if o["kind"] == "b":
                continue
            if o["kind"] == "d":
                P = dpool[e]
                i = P["nxt"]
                P["nxt"] = (i + 1) % P["n"]
                if P["last"][i] is not None:
                    wait(e, ("d", e, i), P["sems"][i], P["last"][i])
                inst = o["fn"](self.eng[e])
                P["cnt"][i] += 16
                inst.then_inc(P["sems"][i], 16)
                P["last"][i] = P["cnt"][i]
                tok[oid] = (("d", e, i), P["sems"][i], P["cnt"][i])
            else:
                inst = o["fn"](self.eng[e])
                if o["sig"]:
                    cnt[e] += 1
                    inst.then_inc(sems[e], 1)
                    tok[oid] = (e, sems[e], cnt[e])
        for q, P in dpool.items():
            for i in range(P["n"]):
                if P["last"][i] is not None:
                    wait("sp", ("d", q, i), P["sems"][i], P["last"][i])
        return dict(n_ops=len(ops), n_waits=nw, cnt=cnt)


def build(stage_limit=99, dbg=False):
    nc = bass.Bass("TRN2", target_bir_lowering=False)

    def DI(name, shape, dt):
        return nc.dram_tensor(name, list(shape), dt, kind="ExternalInput").ap()

    def DS(name, shape, dt):
        kind = "ExternalOutput" if (dbg or not SCRATCH_INTERNAL) else "Internal"
        return nc.dram_tensor(name, list(shape), dt, kind=kind).ap()

    x = DI("x", [T, D], F32)
    pos = DI("pos", [1, T], I32)
    cst = DI("cst", [128, NCONST], F32)
    w_in = DI("w_in", [D, 6848], F32)
    w_krsw = DI("w_krsw", [D, 64], F32)
    g_attn = DI("g_attn", [1, D], F32)
    g_ffn = DI("g_ffn", [1, D], F32)
    g_fin = DI("g_fin", [1, D], F32)
    lbT = DI("lbT", [128, 16], F32)
    g_hg = DI("g_hg", [128, 1], F32)
    g_q = DI("g_q", [128, 3], F32)
    g_kv = DI("g_kv", [128, 2], F32)
    w_uq = DI("w_uq", [384, 1536], F32)
    w_uqsw = DI("w_uqsw", [384, 512], F32)
    w_ukv = DI("w_ukv", [256, 2048], F32)
    w_bh = DI("w_bh", [D, D], F32)
    w_bm = DI("w_bm", [D, D], F32)
    w_out = DI("w_out", [D, D], F32)
    w_r = DI("w_r", [D, 72], F32)
    b_r = DI("b_r", [1, 72], F32)
    ew1 = DI("ew1", [NEXP, D, 512], F32)
    ew3 = DI("ew3", [NEXP, D, 512], F32)
    ew2 = DI("ew2", [NEXP, 512, D], F32)
    out = nc.dram_tensor("out", [T, D], F32, kind="ExternalOutput").ap()

    oaT = DS("oaT", [D, T], BF16)
    obT = DS("obT", [D, T], BF16)
    x1d = DS("x1d", [T, D], F32)
    xg = DS("xg", [NROWS + 128, D], BF16)
    yb = DS("yb", [NROWS + 128, D], F32)

    w_in_v = w_in.rearrange("(c p) n -> p c n", p=128)

    with contextlib.ExitStack() as es:
        em = Emitter(nc, es)

        def sbt(st, name, shape, dt):
            t = st.enter_context(nc.sbuf_tensor(name, list(shape), dt))
            return TB(t, em.cell(name))

        def ACT(out_, in_, func, r, w, **kw):
            em.op("act", lambda e: e.activation(out=out_, in_=in_, func=func, **kw), r, w)

        def TT(eng, out_, in0, in1, op, r, w):
            em.op(eng, lambda e: e.tensor_tensor(out=out_, in0=in0, in1=in1, op=op), r, w)

        def TS(eng, out_, in0, s1, s2, op0, op1, r, w):
            if op1 is None:
                em.op(eng, lambda e: e.tensor_scalar(out=out_, in0=in0, scalar1=s1, scalar2=None, op0=op0), r, w)
            else:
                em.op(eng, lambda e: e.tensor_scalar(out=out_, in0=in0, scalar1=s1, scalar2=s2, op0=op0, op1=op1), r, w)

        def STT(out_, in0, scalar, in1, op0, op1, r, w):
            em.op("dve", lambda e: e.scalar_tensor_tensor(out=out_, in0=in0, scalar=scalar, in1=in1, op0=op0, op1=op1), r, w)

        def MM(out_, lhsT, rhs, start, stop, r, w):
            em.op("pe", lambda e: e.matmul(out_, lhsT, rhs, start=start, stop=stop), r, w)

        def TR(out_, in_, ident, r, w):
            em.op("pe", lambda e: e.transpose(out_, in_, ident), r, w)

        def CP(eng, out_, in_, r, w):
            if eng == "act":
                em.op("act", lambda e: e.activation(out=out_, in_=in_, func=AF.Copy), r, w)
            else:
                em.op(eng, lambda e: e.tensor_copy(out=out_, in_=in_), r, w)

        def MSET(eng, ap, val, w):
            em.op(eng, lambda e: e.memset(ap, val), [], w)

        def DMA(q, out_, in_, r, w):
            em.op(q, lambda e: e.dma_start(out=out_, in_=in_), r, w, kind="d")

        PS = []
        for i in range(8):
            t = es.enter_context(nc.psum_tensor(f"ps{i}", [128, 512], F32))
            PS.append(TB(t, em.cell(f"ps{i}")))
        rot = [0]

        def nb(n=6):
            b = PS[rot[0] % n]
            rot[0] += 1
            return b

        xnT = sbt(es, "xnT", [128, 8, T], BF16)
        xnT_c = [em.cell(f"xnT{b}") for b in range(T // 512)]
        cb = sbt(es, "cb", [128, 1346 + 2048], BF16)
        cf = sbt(es, "cf", [128, 1346], F32)
        epsT = sbt(es, "epsT", [128, 1], F32)
        dst = sbt(es, "dst", [128, 64], U32)
        wts = sbt(es, "wts", [128, 64], F32)

        DMA("pool", cb.t[:, :], cst[:, :], [], [cb.c])
        DMA("sp", cf.t[:, :], cst[:, 0:1346], [], [cf.c])
        MSET("dve", epsT.t[:, :], EPS, [epsT.c])
        ident_bf = cb.t[:, C_IDENT:C_IDENT + 128]
        ones_bf = cb.t[:, C_ONES:C_ONES + 128]

        def rstd_from(ssq_ap, npart, ncol, scale, tmp, outt, reads):
            ACT(tmp.t[0:npart, 0:ncol], ssq_ap, AF.Ln, reads + [epsT.c], [tmp.c], scale=scale, bias=epsT.t[0:npart, 0:1])
            ACT(outt.t[0:npart, 0:ncol], tmp.t[0:npart, 0:ncol], AF.Exp, [tmp.c], [outt.c], scale=-0.5)

        with contextlib.ExitStack() as st:
            xts = [sbt(st, f"xt{i}", [128, D], F32) for i in range(3)]
            xns = [sbt(st, f"xn{i}", [128, D], BF16) for i in range(2)]
            junk = sbt(st, "junk", [128, D], BF16)
            gA = sbt(st, "gA", [128, D], F32)
            ssq = sbt(st, "ssq", [128, 2], F32)
            lnv = sbt(st, "lnv", [128, 2], F32)
            rstd = sbt(st, "rstd", [128, 2], F32)
            DMA("sp", gA.t[:, :], g_attn[0:1, :].to_broadcast([128, D]), [], [gA.c])
            s0_banks = {}

            def s0_pre(i):
                xt = xts[i % 3]
                xn = xns[i % 2]
                DMA("sp", xt.t[:, :], x[i * 128:(i + 1) * 128, :], [], [xt.c])
                ACT(junk.t[:, :], xt.t[:, :], AF.Square, [xt.c], [junk.c, ssq.c], accum_out=ssq.t[:, 0:1])
                rstd_from(ssq.t[:, 0:1], 128, 1, 1.0 / D, lnv, rstd, [ssq.c])
                STT(xn.t[:, :], xt.t[:, :], rstd.t[:, 0:1], gA.t[:, :], ALU.mult, ALU.mult, [xt.c, rstd.c, gA.c], [xn.c])
                bk = nb()
                bkb = bk.t[:, :].bitcast(BF16)
                for c in range(8):
                    TR(bkb[:, c * 128:(c + 1) * 128], xn.t[:, c * 128:(c + 1) * 128], ident_bf, [xn.c, cb.c], [bk.c])
                s0_banks[i] = bk

            def s0_post(i):
                bk = s0_banks.pop(i)
                bkb = bk.t[:, :].bitcast(BF16)
                CP("act", xnT.t[:, :, i * 128:(i + 1) * 128], bkb.rearrange("p (c t) -> p c t", c=8), [bk.c], [xnT_c[i // 4]])

            s0_pre(0)
            for i in range(T // 128):
                if i + 1 < T // 128:
                    s0_pre(i + 1)
                s0_post(i)
        em.barrier()

        if stage_limit >= 1:
            with contextlib.ExitStack() as st:
                whs = [sbt(st, f"wh{i}", [128, 8, 4, 128], BF16) for i in range(2)]
                lb_sb = sbt(st, "lb_sb", [128, 16], F32)
                oml = sbt(st, "oml", [128, 8], F32)
                ghg = sbt(st, "ghg", [128, 1], F32)
                class NS:
                    pass
                bufs = []
                for bi_ in range(2):
                    B = NS()
                    for nm in ("sq", "kk", "logf", "bcum", "dd", "ek", "eq", "sg", "osb", "lnv", "rs"):
                        setattr(B, nm, sbt(st, f"{nm}{bi_}", [128, 512], F32))
                    for nm in ("kdec", "qx", "osq", "oa"):
                        setattr(B, nm, sbt(st, f"{nm}{bi_}", [128, 512], BF16))
                    B.eb = sbt(st, f"eb{bi_}", [128, 8], F32)
                    B.vtok = sbt(st, f"vtok{bi_}", [128, 4, 128], BF16)
                    B.kdT = sbt(st, f"kdT{bi_}", [128, 4, 128], BF16)
                    B.A = sbt(st, f"A{bi_}", [128, 256], BF16)
                    bufs.append(B)
                S = sbt(st, "S", [128, 128], F32)
                Sds = [sbt(st, f"Sd{i}", [128, 128], BF16) for i in range(8)]
                zt = sbt(st, "zt", [128, 8192], BF16)
                zf = sbt(st, "zf", [128, D], F32)
                MSET("pool", zt.t[:, :], 0.0, [zt.c])
                MSET("pool", zf.t[:, :], 0.0, [zf.c])
                xg_flat = xg.rearrange("(p r) d -> p (r d)", p=128)
                nper = (NROWS + 128) // 128 * D
                for k0 in range(0, nper, 8192):
                    k1 = min(nper, k0 + 8192)
                    DMA("sp", xg_flat[:, k0:k1], zt.t[:, 0:k1 - k0], [zt.c], [])
                DMA("sp", yb[NROWS:NROWS + 128, :], zf.t[:, :], [zf.c], [])
                DMA("sp", lb_sb.t[:, :], lbT[:, :], [], [lb_sb.c])
                DMA("sp", ghg.t[:, :], g_hg[:, :], [], [ghg.c])
                TT("dve", oml.t[:, :], lb_sb.t[:, 8:16], lb_sb.t[:, 0:8], ALU.subtract, [lb_sb.c], [oml.c])
                ACT(oml.t[:, :], oml.t[:, :], AF.Sigmoid, [oml.c], [oml.c])
                PQ, PF_, PG_, PV, PTA, PUe, PUo, PO = PS
                pt_c = PTA.c
                pa_c = PTA.c
                lnc = math.log(128 ** -0.5)
                lncT = sbt(st, "lncT", [128, 1], F32)
                MSET("dve", lncT.t[:, :], lnc, [lncT.c])
                batches = [(h, s_, j) for h in range(8) for s_ in range(2) for j in range(4)]

                def load_w(h):
                    wh = whs[h % 2]
                    for fam in range(4):
                        col = fam * 1024 + h * 128
                        DMA("pool", wh.t[:, :, fam, :], w_in_v[:, :, col:col + 128], [], [wh.c])

                def geo(bi):
                    h, s_, j = batches[bi]
                    return h, s_, j, bufs[bi % 2], whs[h % 2], s_ * SEQ + j * 512

                def front_proj(bi):
                    h, s_, j, B, wh, T0 = geo(bi)
                    xc = xnT_c[T0 // 512]
                    for c in range(8):
                        MM(PQ.t[:, :], wh.t[:, c, 0, :], xnT.t[:, c, T0:T0 + 512], c == 0, c == 7, [wh.c, xc], [PQ.c])
                    for c in range(8):
                        MM(PF_.t[:, :], wh.t[:, c, 1, :], xnT.t[:, c, T0:T0 + 512], c == 0, c == 7, [wh.c, xc], [PF_.c])
                    for c in range(8):
                        MM(PG_.t[:, :], wh.t[:, c, 3, :], xnT.t[:, c, T0:T0 + 512], c == 0, c == 7, [wh.c, xc], [PG_.c])
                    for ti in range(4):
                        for c in range(8):
                            MM(PV.t[:, ti * 128:(ti + 1) * 128], xnT.t[:, c, T0 + ti * 128:T0 + (ti + 1) * 128],
                               wh.t[:, c, 2, :], c == 0, c == 7, [wh.c, xc], [PV.c])
                    ACT(B.kk.t[:, :], PF_.t[:, :], AF.Sigmoid, [PF_.c], [B.kk.c], scale=-1.0)
                    ACT(B.sq.t[:, :], PQ.t[:, :], AF.Silu, [PQ.c], [B.sq.c])
                    ACT(B.sg.t[:, :], PG_.t[:, :], AF.Silu, [PG_.c], [B.sg.c])
                    CP("act", B.vtok.t[:, :, :], PV.t[:, :].rearrange("p (a b) -> p a b", a=4), [PV.c], [B.vtok.c])

                def front_mid(bi):
                    h, s_, j, B, wh, T0 = geo(bi)
                    TS("dve", B.kk.t[:, :], B.kk.t[:, :], oml.t[:, h:h + 1], None, ALU.mult, None, [B.kk.c, oml.c], [B.kk.c])
                    ACT(B.logf.t[:, :], B.kk.t[:, :], AF.Ln, [B.kk.c, cf.c], [B.logf.c], scale=-1.0, bias=cf.t[:, C_ONES:C_ONES + 1])
                    em.op("dve", lambda e: e.tensor_tensor_scan(
                        out=B.bcum.t[:, :], data0=cf.t[:, C_RESET:C_RESET + 512], data1=B.logf.t[:, :],
                        initial=0.0, op0=ALU.mult, op1=ALU.add), [cf.c, B.logf.c], [B.bcum.c])
                    b3 = B.bcum.t[:, :].rearrange("p (a b) -> p a b", a=8)
                    d3 = B.dd.t[:, :].rearrange("p (a b) -> p a b", a=8)
                    TT("dve", d3, b3[:, :, 63:64].to_broadcast([128, 8, 64]), b3, ALU.subtract, [B.bcum.c], [B.dd.c])
                    ACT(B.ek.t[:, :], B.dd.t[:, :], AF.Exp, [B.dd.c], [B.ek.c])
                    ACT(B.eq.t[:, :], B.dd.t[:, :], AF.Exp, [B.dd.c, lncT.c], [B.eq.c], scale=-1.0, bias=lncT.t[:, 0:1])
                    ACT(B.eb.t[:, :], b3[:, :, 63], AF.Exp, [B.bcum.c], [B.eb.c])
                    TT("pool", B.kdec.t[:, :], B.kk.t[:, :], B.ek.t[:, :], ALU.mult, [B.kk.c, B.ek.c], [B.kdec.c])
                    TT("pool", B.qx.t[:, :], B.sq.t[:, :], B.eq.t[:, :], ALU.mult, [B.sq.c, B.eq.c], [B.qx.c])

                def front_end(bi):
                    h, s_, j, B, wh, T0 = geo(bi)
                    ptb = PTA.t[:, :].bitcast(BF16)
                    for ti in range(4):
                        TR(ptb[:, ti * 128:(ti + 1) * 128], B.kdec.t[:, ti * 128:(ti + 1) * 128], ident_bf, [B.kdec.c, cb.c], [pt_c])
                    CP("act", B.kdT.t[:, :, :], ptb[:, 0:512].rearrange("p (a b) -> p a b", a=4), [pt_c], [B.kdT.c])
                    for c8 in range(8):
                        ti, hf = c8 // 2, c8 % 2
                        MM(PTA.t[hf * 64:(hf + 1) * 64, 256 + ti * 64:256 + (ti + 1) * 64], B.kdec.t[:, c8 * 64:(c8 + 1) * 64],
                           B.qx.t[:, c8 * 64:(c8 + 1) * 64], True, True, [B.kdec.c, B.qx.c], [pa_c])
                    TT("dve", B.A.t[:, :], PTA.t[:, 256:512], cb.t[:, C_MASK256:C_MASK256 + 256], ALU.mult, [pa_c, cb.c], [B.A.c])

                def back_u(bi):
                    h, s_, j, B, wh, T0 = geo(bi)
                    for c8 in range(8):
                        ti, hf = c8 // 2, c8 % 2
                        hs = slice(hf * 64, (hf + 1) * 64)
                        ub = PUe if hf == 0 else PUo
                        MM(ub.t[:, ti * 128:(ti + 1) * 128], B.kdT.t[hs, ti, :], B.vtok.t[hs, ti, :], True, True,
                           [B.kdT.c, B.vtok.c], [ub.c])

                def back_chain(bi):
                    h, s_, j, B, wh, T0 = geo(bi)
                    if j == 0:
                        MSET("dve", S.t[:, :], 0.0, [S.c])
                        MSET("dve", Sds[0].t[:, :], 0.0, [Sds[0].c])
                    for c8 in range(8):
                        ti, hf = c8 // 2, c8 % 2
                        first = (j == 0 and c8 == 0)
                        ub = PUe if hf == 0 else PUo
                        if not first:
                            TS("dve", Sds[c8].t[:, :], S.t[:, :], B.eb.t[:, c8:c8 + 1], None, ALU.mult, None, [S.c, B.eb.c], [Sds[c8].c])
                        STT(S.t[:, :], S.t[:, :], B.eb.t[:, c8:c8 + 1], ub.t[:, ti * 128:(ti + 1) * 128], ALU.mult, ALU.add,
                            [S.c, B.eb.c, ub.c], [S.c])
                    for c8 in range(8):
                        ti, hf = c8 // 2, c8 % 2
                        hs = slice(hf * 64, (hf + 1) * 64)
                        MM(PO.t[:, c8 * 64:(c8 + 1) * 64], B.vtok.t[hs, ti, :], B.A.t[hs, ti * 64:(ti + 1) * 64], True, False,
                           [B.vtok.c, B.A.c], [PO.c])
                        MM(PO.t[:, c8 * 64:(c8 + 1) * 64], Sds[c8].t[:, :], B.qx.t[:, c8 * 64:(c8 + 1) * 64], False, True,
                           [Sds[c8].c, B.qx.c], [PO.c])
                    CP("act", B.osb.t[:, :], PO.t[:, :], [PO.c], [B.osb.c])
                    TT("pool", B.osq.t[:, :], B.osb.t[:, :], B.osb.t[:, :], ALU.mult, [B.osb.c], [B.osq.c])
                    MM(PO.t[:, :], ones_bf, B.osq.t[:, :], True, True, [cb.c, B.osq.c], [PO.c])

                def back_norm(bi):
                    h, s_, j, B, wh, T0 = geo(bi)
                    rstd_from(PO.t[:, :], 128, 512, 1.0 / 128, B.lnv, B.rs, [PO.c])
                    TT("dve", B.osb.t[:, :], B.osb.t[:, :], B.rs.t[:, :], ALU.mult, [B.osb.c, B.rs.c], [B.osb.c])
                    STT(B.oa.t[:, :], B.osb.t[:, :], ghg.t[:, 0:1], B.sg.t[:, :], ALU.mult, ALU.mult, [B.osb.c, ghg.c, B.sg.c], [B.oa.c])
                    DMA("sp", oaT[h * 128:(h + 1) * 128, T0:T0 + 512], B.oa.t[:, :], [B.oa.c], [])

                load_w(0)
                front_proj(0)
                front_mid(0)
                front_end(0)
                nb_ = len(batches)
                for bi in range(nb_):
                    h, s_, j = batches[bi]
                    if s_ == 0 and j == 0 and h + 1 < 8:
                        load_w(h + 1)
                    nx = bi + 1 < nb_
                    back_u(bi)
                    if nx:
                        front_proj(bi + 1)
                    back_chain(bi)
                    if nx:
                        front_mid(bi + 1)
                    back_norm(bi)
                    if nx:
                        front_end(bi + 1)
            em.barrier()


        if stage_limit >= 2:
            with contextlib.ExitStack() as st:
                wm = sbt(st, "wm", [128, 8, 768], BF16)
                wuq = sbt(st, "wuq", [128, 3, 1536], BF16)
                wuqs = sbt(st, "wuqs", [128, 3, 512], BF16)
                wukv = sbt(st, "wukv", [128, 2, 2048], BF16)
                gq = sbt(st, "gq", [128, 3], F32)
                gkv = sbt(st, "gkv", [128, 2], F32)
                cos2 = sbt(st, "cos2", [64, T], BF16)
                sin2 = sbt(st, "sin2", [64, T], BF16)
                scl = sbt(st, "scl", [64, 1], F32)
                rope_st = contextlib.ExitStack()
                posi = sbt(rope_st, "posi", [64, 1024], I32)
                ang = sbt(rope_st, "ang", [64, 1024], F32)
                uu = sbt(rope_st, "uu", [64, 1024], F32)
                ui = sbt(rope_st, "ui", [64, 1024], I32)
                uf = sbt(rope_st, "uf", [64, 1024], F32)
                PO, PL = PS[6], PS[7]
                TWO_PI = 2.0 * math.pi
                DMA("pool", wm.t[:, :, 0:704], w_in_v[:, :, 4096:4800], [], [wm.c])
                DMA("pool", wm.t[:, :, 704:768], w_krsw.rearrange("(c p) n -> p c n", p=128), [], [wm.c])
                DMA("pool", wuq.t[:, :, :], w_uq.rearrange("(c p) n -> p c n", p=128), [], [wuq.c])
                DMA("pool", wuqs.t[:, :, :], w_uqsw.rearrange("(c p) n -> p c n", p=128), [], [wuqs.c])
                DMA("pool", wukv.t[:, :, :], w_ukv.rearrange("(c p) n -> p c n", p=128), [], [wukv.c])
                DMA("sp", gq.t[:, :], g_q[:, :], [], [gq.c])
                DMA("sp", gkv.t[:, :], g_kv[:, :], [], [gkv.c])
                TS("dve", scl.t[:, :], cf.t[0:64, C_SGN:C_SGN + 1], TWO_PI * (1.0 - 1e-6), None, ALU.mult, None, [cf.c], [scl.c])
                for blk in range(4):
                    cs = slice(blk * 1024, (blk + 1) * 1024)
                    DMA("sp", posi.t[:, :], pos[0:1, cs].to_broadcast([64, 1024]), [], [posi.c])
                    CP("dve", ang.t[:, :], posi.t[:, :], [posi.c], [ang.c])
                    TS("dve", ang.t[:, :], ang.t[:, :], cf.t[0:64, C_INVF:C_INVF + 1], 1.0 / TWO_PI, ALU.mult, ALU.mult, [ang.c, cf.c], [ang.c])
                    for kind, off in (("sin", 0.0), ("cos", 0.25)):
                        TS("dve", uu.t[:, :], ang.t[:, :], off, None, ALU.add, None, [ang.c], [uu.c])
                        CP("dve", ui.t[:, :], uu.t[:, :], [uu.c], [ui.c])
                        CP("dve", uf.t[:, :], ui.t[:, :], [ui.c], [uf.c])
                        TT("dve", uu.t[:, :], uu.t[:, :], uf.t[:, :], ALU.subtract, [uu.c, uf.c], [uu.c])
                        TS("dve", uf.t[:, :], uu.t[:, :], 0.5, None, ALU.is_gt, None, [uu.c], [uf.c])
                        TT("dve", uu.t[:, :], uu.t[:, :], uf.t[:, :], ALU.subtract, [uu.c, uf.c], [uu.c])
                        TS("dve", uf.t[:, :], uu.t[:, :], -0.5, None, ALU.is_lt, None, [uu.c], [uf.c])
                        TT("dve", uu.t[:, :], uu.t[:, :], uf.t[:, :], ALU.add, [uu.c, uf.c], [uu.c])
                        if kind == "sin":
                            ACT(sin2.t[:, cs], uu.t[:, :], AF.Sin, [uu.c, scl.c], [sin2.c], scale=scl.t[:, 0:1])
                        else:
                            ACT(cos2.t[:, cs], uu.t[:, :], AF.Sin, [uu.c], [cos2.c], scale=TWO_PI * (1.0 - 1e-6))
                em.barrier()
                rope_st.close()
                sqc = [sbt(st, f"sqc{i}", [128, 512], BF16) for i in range(3)]
                lnv = sbt(st, "lnv2", [128, 512], F32)
                rs = sbt(st, "rs2", [128, 512], F32)
                cqn = sbt(st, "cqn", [128, 3, SEQ], BF16)
                ckvn = sbt(st, "ckvn", [128, 2, SEQ], BF16)
                krT = sbt(st, "krT", [64, SEQ], BF16)
                t1 = sbt(st, "t1", [64, 512], F32)
                t2 = sbt(st, "t2", [64, 512], F32)
                KnTs = [sbt(st, f"KnT{i}", [128, SEQ], BF16) for i in range(2)]
                Vhs = [sbt(st, f"Vh{i}", [128, 16, 128], BF16) for i in range(2)]
                qns = [sbt(st, f"qn{i}", [128, 512], BF16) for i in range(2)]
                qrs = [sbt(st, f"qr{i}", [64, 512], BF16) for i in range(2)]
                pts = [sbt(st, f"pt{i}", [128, 512], BF16) for i in range(4)]
                lnl = sbt(st, "lnl", [128, 512], F32)
                rl = sbt(st, "rl", [128, 512], F32)
                obs = [sbt(st, f"ob{i}", [128, 512], BF16) for i in range(2)]
                nbat = 0
                for s in range(2):
                    for j in range(4):
                        T0 = s * SEQ + j * 512
                        L0 = j * 512
                        xc = xnT_c[T0 // 512]
                        for (dst_t, ncc, col0, gt, dim) in ((cqn, 3, 0, gq, 384), (ckvn, 2, 384, gkv, 256)):
                            banks = [nb() for _ in range(ncc)]
                            for cc in range(ncc):
                                for c in range(8):
                                    MM(banks[cc].t[:, :], wm.t[:, c, col0 + cc * 128:col0 + (cc + 1) * 128], xnT.t[:, c, T0:T0 + 512],
                                       c == 0, c == 7, [wm.c, xc], [banks[cc].c])
                            for cc in range(ncc):
                                ACT(sqc[cc].t[:, :], banks[cc].t[:, :], AF.Square, [banks[cc].c], [sqc[cc].c])
                            bs = nb()
                            for cc in range(ncc):
                                MM(bs.t[:, :], ones_bf, sqc[cc].t[:, :], cc == 0, cc == ncc - 1, [cb.c, sqc[cc].c], [bs.c])
                            rstd_from(bs.t[:, :], 128, 512, 1.0 / dim, lnv, rs, [bs.c])
                            for cc in range(ncc):
                                STT(dst_t.t[:, cc, L0:L0 + 512], banks[cc].t[:, :], gt.t[:, cc:cc + 1], rs.t[:, :], ALU.mult, ALU.mult,
                                    [banks[cc].c, gt.c, rs.c], [dst_t.c])
                        bk1, bk2 = nb(), nb()
                        for c in range(8):
                            MM(bk1.t[0:64, :], wm.t[:, c, 640:704], xnT.t[:, c, T0:T0 + 512], c == 0, c == 7, [wm.c, xc], [bk1.c])
                        for c in range(8):
                            MM(bk2.t[0:64, :], wm.t[:, c, 704:768], xnT.t[:, c, T0:T0 + 512], c == 0, c == 7, [wm.c, xc], [bk2.c])
                        TT("dve", t1.t[:, :], bk1.t[0:64, :], cos2.t[:, T0:T0 + 512], ALU.mult, [bk1.c, cos2.c], [t1.c])
                        TT("dve", t2.t[:, :], bk2.t[0:64, :], sin2.t[:, T0:T0 + 512], ALU.mult, [bk2.c, sin2.c], [t2.c])
                        TT("pool", krT.t[:, L0:L0 + 512], t1.t[:, :], t2.t[:, :], ALU.add, [t1.c, t2.c], [krT.c])
                    items = [(h, j) for h in range(8) for j in range(4)]

                    def prologue(k, s=s):
                        h, j = items[k]
                        KnT, Vh = KnTs[h % 2], Vhs[h % 2]
                        qn, qr = qns[k % 2], qrs[k % 2]
                        T0 = s * SEQ + j * 512
                        L0 = j * 512
                        bkk = nb()
                        for c in range(2):
                            MM(bkk.t[:, :], wukv.t[:, c, h * 256:h * 256 + 128], ckvn.t[:, c, L0:L0 + 512], c == 0, c == 1,
                               [wukv.c, ckvn.c], [bkk.c])
                        CP("act", KnT.t[:, L0:L0 + 512], bkk.t[:, :], [bkk.c], [KnT.c])
                        bv = nb()
                        for ti in range(4):
                            for c in range(2):
                                MM(bv.t[:, ti * 128:(ti + 1) * 128], ckvn.t[:, c, L0 + ti * 128:L0 + (ti + 1) * 128],
                                   wukv.t[:, c, h * 256 + 128:h * 256 + 256], c == 0, c == 1, [wukv.c, ckvn.c], [bv.c])
                        CP("dve", Vh.t[:, j * 4:(j + 1) * 4, :], bv.t[:, :].rearrange("p (a b) -> p a b", a=4), [bv.c], [Vh.c])
                        bq, bp, bps = nb(), nb(), nb()
                        for c in range(3):
                            MM(bq.t[:, :], wuq.t[:, c, h * 192:h * 192 + 128], cqn.t[:, c, L0:L0 + 512], c == 0, c == 2, [wuq.c, cqn.c], [bq.c])
                        for c in range(3):
                            MM(bp.t[0:64, :], wuq.t[:, c, h * 192 + 128:h * 192 + 192], cqn.t[:, c, L0:L0 + 512], c == 0, c == 2,
                               [wuq.c, cqn.c], [bp.c])
                        for c in range(3):
                            MM(bps.t[0:64, :], wuqs.t[:, c, h * 64:(h + 1) * 64], cqn.t[:, c, L0:L0 + 512], c == 0, c == 2,
                               [wuqs.c, cqn.c], [bps.c])
                        CP("act", qn.t[:, :], bq.t[:, :], [bq.c], [qn.c])
                        TT("dve", t1.t[:, :], bp.t[0:64, :], cos2.t[:, T0:T0 + 512], ALU.mult, [bp.c, cos2.c], [t1.c])
                        TT("dve", t2.t[:, :], bps.t[0:64, :], sin2.t[:, T0:T0 + 512], ALU.mult, [bps.c, sin2.c], [t2.c])
                        TT("pool", qr.t[:, :], t1.t[:, :], t2.t[:, :], ALU.add, [t1.c, t2.c], [qr.c])

                    def attention(k, s=s):
                        h, j = items[k]
                        KnT, Vh = KnTs[h % 2], Vhs[h % 2]
                        qn, qr = qns[k % 2], qrs[k % 2]
                        T0 = s * SEQ + j * 512
                        nkt = 4 * j + 4

                        def col0(kt):
                            return max(0, kt - 4 * j) * 128

                        def s_mm(kt):
                            bst = nb()
                            c0 = col0(kt)
                            MM(bst.t[:, c0:512], KnT.t[:, kt * 128:(kt + 1) * 128], qn.t[:, c0:512], True, False, [KnT.c, qn.c], [bst.c])
                            MM(bst.t[:, c0:512], krT.t[:, kt * 128:(kt + 1) * 128], qr.t[:, c0:512], False, True, [krT.c, qr.c], [bst.c])
                            return bst
                        pend = [s_mm(0), s_mm(1), s_mm(2)]
                        for kt in range(nkt):
                            bst = pend.pop(0)
                            if kt + 3 < nkt:
                                pend.append(s_mm(kt + 3))
                            pt = pts[kt % 4]
                            c0 = col0(kt)
                            ACT(pt.t[:, c0:512], bst.t[:, c0:512], AF.Exp, [bst.c], [pt.c], scale=192.0 ** -0.5)
                            if kt >= 4 * j:
                                TT("dve", pt.t[:, c0:c0 + 128], pt.t[:, c0:c0 + 128], cb.t[:, C_TRILE:C_TRILE + 128], ALU.mult,
                                   [pt.c, cb.c], [pt.c])
                            MM(PO.t[:, c0:512], Vh.t[:, kt, :], pt.t[:, c0:512], kt == 0, kt == nkt - 1, [Vh.c, pt.c], [PO.c])
                            MM(PL.t[:, c0:512], ones_bf, pt.t[:, c0:512], kt == 0, kt == nkt - 1, [cb.c, pt.c], [PL.c])
                        ACT(lnl.t[:, :], PL.t[:, :], AF.Ln, [PL.c], [lnl.c])
                        ACT(rl.t[:, :], lnl.t[:, :], AF.Exp, [lnl.c], [rl.c], scale=-1.0)
                        ob = obs[k % 2]
                        TT("dve", ob.t[:, :], PO.t[:, :], rl.t[:, :], ALU.mult, [PO.c, rl.c], [ob.c])
                        DMA("sp", obT[h * 128:(h + 1) * 128, T0:T0 + 512], ob.t[:, :], [ob.c], [])

                    prologue(0)
                    for k in range(len(items)):
                        if k + 1 < len(items):
                            prologue(k + 1)
                        attention(k)
            em.barrier()

        if stage_limit >= 3:
            with contextlib.ExitStack() as st:
                wg = sbt(st, "wg", [128, 8, 2048], BF16)
                wbh = sbt(st, "wbh", [128, 8, D], BF16)
                wbm = sbt(st, "wbm", [128, 8, D], BF16)
                wo = sbt(st, "wo", [128, 8, D], BF16)
                wr = sbt(st, "wr", [128, 8, 72], F32)
                br = sbt(st, "br", [128, 72], F32)
                g2 = sbt(st, "g2", [128, D], F32)
                oabs = [sbt(st, f"oab{i}", [128, 8, 128], BF16) for i in range(2)]
                obbs = [sbt(st, f"obb{i}", [128, 8, 128], BF16) for i in range(2)]
                Ta = sbt(st, "Ta", [128, D], F32)
                Tb = sbt(st, "Tb", [128, D], F32)
                ybf = sbt(st, "ybf", [128, D], BF16)
                yT = sbt(st, "yT", [128, 8, 128], BF16)
                h2bfs = [sbt(st, f"h2bf{i}", [128, D], BF16) for i in range(2)]
                h2T = sbt(st, "h2T", [128, 8, 128], F32)
                ssq = sbt(st, "ssq3", [128, 2], F32)
                lnv = sbt(st, "lnv3", [128, 2], F32)
                rstd = sbt(st, "rstd3", [128, 2], F32)
                lg = sbt(st, "lg", [128, 72], F32)
                g8 = sbt(st, "g8", [128, 8], F32)
                ohg = sbt(st, "ohg", [128, 8], F32)
                ngm = sbt(st, "ngm", [128, 1], F32)
                ex = sbt(st, "ex", [128, 8], F32)
                gs = sbt(st, "gs", [128, 1], F32)
                gw = sbt(st, "gw", [128, 1], F32)
                pen = sbt(st, "pen", [128, 8], F32)
                msk = sbt(st, "msk", [128, 64], F32)
                m8 = sbt(st, "m8", [128, 8], F32)
                i8 = sbt(st, "i8", [128, 8], U32)
                idf = sbt(st, "idf", [128, 2], F32)
                dlt = sbt(st, "dlt", [128, 1], F32)
                ed = sbt(st, "ed", [128, 1], F32)
                den = sbt(st, "den", [128, 1], F32)
                e1 = sbt(st, "e1", [128, 1], F32)
                e2 = sbt(st, "e2", [128, 1], F32)
                oh1 = sbt(st, "oh1", [128, 64], F32)
                oh2 = sbt(st, "oh2", [128, 64], F32)
                oh = sbt(st, "oh", [128, 64], F32)
                tmp64 = sbt(st, "tmp64", [128, 64], F32)
                sl = sbt(st, "sl", [128, 2], F32)
                ovf = sbt(st, "ovf", [128, 2], F32)
                dsf = sbt(st, "dsf", [128, 2], F32)
                base = sbt(st, "base", [1, 64], F32)
                xg_c = em.cell("xg")
                DMA("pool", wg.t[:, :, :], w_in_v[:, :, 4800:6848], [], [wg.c])
                DMA("pool", wbh.t[:, :, :], w_bh.rearrange("(c p) n -> p c n", p=128), [], [wbh.c])
                DMA("pool", wbm.t[:, :, :], w_bm.rearrange("(c p) n -> p c n", p=128), [], [wbm.c])
                DMA("pool", wo.t[:, :, :], w_out.rearrange("(c p) n -> p c n", p=128), [], [wo.c])
                DMA("sp", wr.t[:, :, :], w_r.rearrange("(c p) n -> p c n", p=128), [], [wr.c])
                DMA("sp", br.t[:, :], b_r[0:1, :].to_broadcast([128, 72]), [], [br.c])
                DMA("sp", g2.t[:, :], g_ffn[0:1, :].to_broadcast([128, D]), [], [g2.c])
                MSET("dve", base.t[:, :], 0.0, [base.c])
                oaT_v = oaT.rearrange("(c p) t -> p c t", p=128)
                obT_v = obT.rearrange("(c p) t -> p c t", p=128)
                ident_f = cf.t[:, C_IDENT:C_IDENT + 128]
                iota = cf.t[:, C_IOTA:C_IOTA + 64]
                Tas = [Ta, sbt(st, "Ta1", [128, D], F32)]
                Tbs = [Tb, sbt(st, "Tb1", [128, D], F32)]
                ybfs = [ybf, sbt(st, "ybf1", [128, D], BF16)]
                NT = T // 128

                def f_load(i):
                    tok = slice(i * 128, (i + 1) * 128)
                    DMA("sp", oabs[i % 2].t[:, :, :], oaT_v[:, :, tok], [], [oabs[i % 2].c])
                    DMA("sp", obbs[i % 2].t[:, :, :], obT_v[:, :, tok], [], [obbs[i % 2].c])

                def f_gate(i, which):
                    tok = slice(i * 128, (i + 1) * 128)
                    xc = xnT_c[i // 4]
                    Tt = (Tas if which == 0 else Tbs)[i % 2]
                    goff = 0 if which == 0 else 1024
                    for hf in range(2):
                        b = nb()
                        for c in range(8):
                            MM(b.t[:, :], xnT.t[:, c, tok], wg.t[:, c, goff + hf * 512:goff + (hf + 1) * 512], c == 0, c == 7,
                               [xc, wg.c], [b.c])
                        ACT(Tt.t[:, hf * 512:(hf + 1) * 512], b.t[:, :], AF.Sigmoid, [b.c], [Tt.c])

                def f_branch(i, which):
                    Tt = (Tas if which == 0 else Tbs)[i % 2]
                    src = (oabs if which == 0 else obbs)[i % 2]
                    wb = wbh if which == 0 else wbm
                    for hf in range(2):
                        b = nb()
                        for c in range(8):
                            MM(b.t[:, :], src.t[:, c, :], wb.t[:, c, hf * 512:(hf + 1) * 512], c == 0, c == 7, [src.c, wb.c], [b.c])
                        TT("dve", Tt.t[:, hf * 512:(hf + 1) * 512], b.t[:, :], Tt.t[:, hf * 512:(hf + 1) * 512], ALU.mult, [b.c, Tt.c], [Tt.c])
                    if which == 1:
                        TT("pool", ybfs[i % 2].t[:, :], Tas[i % 2].t[:, :], Tbs[i % 2].t[:, :], ALU.add,
                           [Tas[i % 2].c, Tbs[i % 2].c], [ybfs[i % 2].c])

                def b1(i):
                    tok = slice(i * 128, (i + 1) * 128)
                    yb_, Tb_ = ybfs[i % 2], Tbs[i % 2]
                    bt = nb()
                    btb = bt.t[:, :].bitcast(BF16)
                    for c in range(8):
                        TR(btb[:, c * 128:(c + 1) * 128], yb_.t[:, c * 128:(c + 1) * 128], ident_bf, [yb_.c, cb.c], [bt.c])
                    CP("act", yT.t[:, :, :], btb.rearrange("p (c t) -> p c t", c=8), [bt.c], [yT.c])
                    DMA("sp", Tb_.t[:, :], x[tok, :], [], [Tb_.c])

                def b2(i):
                    tok = slice(i * 128, (i + 1) * 128)
                    Ta_, Tb_, yb_ = Tas[i % 2], Tbs[i % 2], ybfs[i % 2]
                    h2bf = h2bfs[i % 2]
                    for hf in range(2):
                        b = nb()
                        for c in range(8):
                            MM(b.t[:, :], yT.t[:, c, :], wo.t[:, c, hf * 512:(hf + 1) * 512], c == 0, c == 7, [yT.c, wo.c], [b.c])
                        TT("dve", Ta_.t[:, hf * 512:(hf + 1) * 512], b.t[:, :], Tb_.t[:, hf * 512:(hf + 1) * 512], ALU.add, [b.c, Tb_.c], [Ta_.c])
                    DMA("sp", x1d[tok, :], Ta_.t[:, :], [Ta_.c], [])
                    ACT(yb_.t[:, :], Ta_.t[:, :], AF.Square, [Ta_.c], [yb_.c, ssq.c], accum_out=ssq.t[:, 0:1])
                    rstd_from(ssq.t[:, 0:1], 128, 1, 1.0 / D, lnv, rstd, [ssq.c])
                    STT(Tb_.t[:, :], Ta_.t[:, :], rstd.t[:, 0:1], g2.t[:, :], ALU.mult, ALU.mult, [Ta_.c, rstd.c, g2.c], [Tb_.c])
                    CP("pool", h2bf.t[:, :], Tb_.t[:, :], [Tb_.c], [h2bf.c])

                def b3(i):
                    Tb_ = Tbs[i % 2]
                    p1, p2 = nb(), nb()
                    for c in range(8):
                        bb = p1 if c < 4 else p2
                        TR(bb.t[:, (c % 4) * 128:(c % 4 + 1) * 128], Tb_.t[:, c * 128:(c + 1) * 128], ident_f, [Tb_.c, cf.c], [bb.c])
                    CP("act", h2T.t[:, 0:4, :], p1.t[:, :].rearrange("p (c t) -> p c t", c=4), [p1.c], [h2T.c])
                    CP("act", h2T.t[:, 4:8, :], p2.t[:, :].rearrange("p (c t) -> p c t", c=4), [p2.c], [h2T.c])

                def b4(i):
                    bl = nb()
                    for c in range(8):
                        MM(bl.t[:, 0:72], h2T.t[:, c, :], wr.t[:, c, :], c == 0, c == 7, [h2T.c, wr.c], [bl.c])
                    TT("dve", lg.t[:, :], bl.t[:, 0:72], br.t[:, :], ALU.add, [bl.c, br.c], [lg.c])
                    em.op("dve", lambda e: e.max(out=g8.t[:, :], in_=lg.t[:, 0:8]), [lg.c], [g8.c])
                    TS("dve", ohg.t[:, :], lg.t[:, 0:8], g8.t[:, 0:1], None, ALU.is_equal, None, [lg.c, g8.c], [ohg.c])
                    TS("dve", ngm.t[:, :], g8.t[:, 0:1], -1.0, None, ALU.mult, None, [g8.c], [ngm.c])
                    ACT(ex.t[:, :], lg.t[:, 0:8], AF.Exp, [lg.c, ngm.c], [ex.c, gs.c], bias=ngm.t[:, 0:1], accum_out=gs.t[:, 0:1])
                    em.op("dve", lambda e: e.reciprocal(out=gw.t[:, :], in_=gs.t[:, :]), [gs.c], [gw.c])
                    TS("dve", pen.t[:, :], ohg.t[:, :], -1.0, 1e30, ALU.add, ALU.mult, [ohg.c], [pen.c])
                    TT("dve", msk.t[:, :].rearrange("p (a b) -> p a b", a=8), lg.t[:, 8:72].rearrange("p (a b) -> p a b", a=8),
                       pen.t[:, :].rearrange("p (a b) -> p a b", b=1).to_broadcast([128, 8, 8]), ALU.add, [lg.c, pen.c], [msk.c])
                    em.op("dve", lambda e: e.max(out=m8.t[:, :], in_=msk.t[:, :]), [msk.c], [m8.c])
                    em.op("dve", lambda e: e.max_index(out=i8.t[:, :], in_max=m8.t[:, :], in_values=msk.t[:, :]), [m8.c, msk.c], [i8.c])
                    CP("dve", idf.t[:, :], i8.t[:, 0:2], [i8.c], [idf.c])
                    TT("dve", dlt.t[:, :], m8.t[:, 1:2], m8.t[:, 0:1], ALU.subtract, [m8.c], [dlt.c])
                    ACT(ed.t[:, :], dlt.t[:, :], AF.Exp, [dlt.c], [ed.c])
                    TS("dve", den.t[:, :], ed.t[:, :], 1.0, None, ALU.add, None, [ed.c], [den.c])
                    em.op("dve", lambda e: e.reciprocal(out=e1.t[:, :], in_=den.t[:, :]), [den.c], [e1.c])
                    TT("dve", e2.t[:, :], ed.t[:, :], e1.t[:, :], ALU.mult, [ed.c, e1.c], [e2.c])
                    TT("dve", wts.t[:, 2 * i:2 * i + 1], e1.t[:, :], gw.t[:, :], ALU.mult, [e1.c, gw.c], [wts.c])
                    TT("dve", wts.t[:, 2 * i + 1:2 * i + 2], e2.t[:, :], gw.t[:, :], ALU.mult, [e2.c, gw.c], [wts.c])
                    TS("dve", oh1.t[:, :], iota, idf.t[:, 0:1], None, ALU.is_equal, None, [cf.c, idf.c], [oh1.c])
                    TS("dve", oh2.t[:, :], iota, idf.t[:, 1:2], None, ALU.is_equal, None, [cf.c, idf.c], [oh2.c])
                    TT("dve", oh.t[:, :], oh1.t[:, :], oh2.t[:, :], ALU.add, [oh1.c, oh2.c], [oh.c])

                def b5(i):
                    h2bf = h2bfs[i % 2]
                    bp_ = nb()
                    MM(bp_.t[:, 0:64], cf.t[:, C_TRIST:C_TRIST + 128], oh.t[:, :], True, False, [cf.c, oh.c], [bp_.c])
                    MM(bp_.t[:, 0:64], cf.t[0:1, C_ONES:C_ONES + 128], base.t[0:1, :], False, True, [cf.c, base.c], [bp_.c])
                    bc_ = nb()
                    MM(bc_.t[0:1, 0:64], cf.t[:, C_ONES:C_ONES + 1], oh.t[:, :], True, True, [cf.c, oh.c], [bc_.c])
                    TT("dve", tmp64.t[:, :], bp_.t[:, 0:64], oh1.t[:, :], ALU.mult, [bp_.c, oh1.c], [tmp64.c])
                    em.op("dve", lambda e: e.reduce_sum(out=sl.t[:, 0:1], in_=tmp64.t[:, :], axis=mybir.AxisListType.X), [tmp64.c], [sl.c])
                    TT("dve", tmp64.t[:, :], bp_.t[:, 0:64], oh2.t[:, :], ALU.mult, [bp_.c, oh2.c], [tmp64.c])
                    em.op("dve", lambda e: e.reduce_sum(out=sl.t[:, 1:2], in_=tmp64.t[:, :], axis=mybir.AxisListType.X), [tmp64.c], [sl.c])
                    TT("dve", base.t[0:1, :], base.t[0:1, :], bc_.t[0:1, 0:64], ALU.add, [base.c, bc_.c], [base.c])
                    TS("dve", ovf.t[:, :], sl.t[:, :], float(CAP), 1e6, ALU.is_ge, ALU.mult, [sl.c], [ovf.c])
                    STT(dsf.t[:, :], idf.t[:, :], float(CAP), sl.t[:, :], ALU.mult, ALU.add, [idf.c, sl.c], [dsf.c])
                    TT("dve", dsf.t[:, :], dsf.t[:, :], ovf.t[:, :], ALU.add, [dsf.c, ovf.c], [dsf.c])
                    TS("dve", dsf.t[:, :], dsf.t[:, :], float(NROWS), None, ALU.min, None, [dsf.c], [dsf.c])
                    CP("dve", dst.t[:, 2 * i:2 * i + 2], dsf.t[:, :], [dsf.c], [dst.c])
                    for k in range(2):
                        em.op("pool", lambda e, k=k, i=i, h2bf=h2bf: e.indirect_dma_start(
                            out=xg[:, :], out_offset=bass.IndirectOffsetOnAxis(ap=dst.t[:, 2 * i + k:2 * i + k + 1], axis=0),
                            in_=h2bf.t[:, :], in_offset=None), [dst.c, h2bf.c], [xg_c], kind="d")

                f_load(0)
                f_gate(0, 0)
                f_branch(0, 0)
                f_gate(0, 1)
                f_branch(0, 1)
                for i in range(NT):
                    nx = i + 1 < NT
                    if nx:
                        f_load(i + 1)
                    b1(i)
                    if nx:
                        f_gate(i + 1, 0)
                    b2(i)
                    if nx:
                        f_branch(i + 1, 0)
                    b3(i)
                    if nx:
                        f_gate(i + 1, 1)
                    b4(i)
                    if nx:
                        f_branch(i + 1, 1)
                    b5(i)
            em.barrier()

        if stage_limit >= 4:
            with contextlib.ExitStack() as st:
                NR = CAP // 128
                w1s = [sbt(st, f"w1s{i}", [128, 8, 512], BF16) for i in range(3)]
                w3s = [sbt(st, f"w3s{i}", [128, 8, 512], BF16) for i in range(3)]
                w2s = [sbt(st, f"w2s{i}", [128, 4, D], BF16) for i in range(3)]
                xgs = [sbt(st, f"xgs{i}", [128, NR, D], BF16) for i in range(2)]
                xgT = [sbt(st, f"xgT{i}", [128, 8, CAP], BF16) for i in range(2)]
                s1s = [sbt(st, f"s1s{i}", [128, CAP], F32) for i in range(2)]
                hid = [sbt(st, f"hid{i}", [128, 4, CAP], BF16) for i in range(2)]
                ysb = [sbt(st, f"ysb{i}", [128, D], F32) for i in range(2)]
                nsi = [0]

                def load_e(ex_):
                    sl_ = ex_ % 3
                    DMA("pool", w1s[sl_].t[:, :, :], ew1[ex_].rearrange("(c p) n -> p c n", p=128), [], [w1s[sl_].c])
                    DMA("pool", w3s[sl_].t[:, :, :], ew3[ex_].rearrange("(c p) n -> p c n", p=128), [], [w3s[sl_].c])
                    DMA("pool", w2s[sl_].t[:, :, :], ew2[ex_].rearrange("(c p) n -> p c n", p=128), [], [w2s[sl_].c])
                    xs = xgs[ex_ % 2]
                    DMA("sp", xs.t[:, :, :], xg[ex_ * CAP:(ex_ + 1) * CAP, :].rearrange("(r p) d -> p r d", p=128), [], [xs.c])

                def front_e(ex_):
                    sl_ = ex_ % 3
                    xs = xgs[ex_ % 2]
                    xT = xgT[ex_ % 2]
                    hd = hid[ex_ % 2]
                    for r in range(NR):
                        bt = nb()
                        btb = bt.t[:, :].bitcast(BF16)
                        for c in range(8):
                            TR(btb[:, c * 128:(c + 1) * 128], xs.t[:, r, c * 128:(c + 1) * 128], ident_bf, [xs.c, cb.c], [bt.c])
                        CP("act" if r == 0 else "dve", xT.t[:, :, r * 128:(r + 1) * 128], btb.rearrange("p (c t) -> p c t", c=8), [bt.c], [xT.c])
                    for fc in range(4):
                        b1, b3 = nb(), nb()
                        for c in range(8):
                            MM(b1.t[:, 0:CAP], w1s[sl_].t[:, c, fc * 128:(fc + 1) * 128], xT.t[:, c, :], c == 0, c == 7, [w1s[sl_].c, xT.c], [b1.c])
                        for c in range(8):
                            MM(b3.t[:, 0:CAP], w3s[sl_].t[:, c, fc * 128:(fc + 1) * 128], xT.t[:, c, :], c == 0, c == 7, [w3s[sl_].c, xT.c], [b3.c])
                        s1 = s1s[nsi[0] % 2]
                        nsi[0] += 1
                        ACT(s1.t[:, :], b1.t[:, 0:CAP], AF.Silu, [b1.c], [s1.c])
                        TT("dve", hd.t[:, fc, :], b3.t[:, 0:CAP], s1.t[:, :], ALU.mult, [b3.c, s1.c], [hd.c])

                def back_e(ex_):
                    sl_ = ex_ % 3
                    hd = hid[ex_ % 2]
                    for r in range(NR):
                        ys = ysb[r % 2]
                        for hf in range(2):
                            b = PS[6 + hf]
                            for fc in range(4):
                                MM(b.t[:, :], hd.t[:, fc, r * 128:(r + 1) * 128], w2s[sl_].t[:, fc, hf * 512:(hf + 1) * 512], fc == 0, fc == 3,
                                   [hd.c, w2s[sl_].c], [b.c])
                            CP("act" if hf == 0 else "dve", ys.t[:, hf * 512:(hf + 1) * 512], b.t[:, :], [b.c], [ys.c])
                        DMA("sp", yb[ex_ * CAP + r * 128:ex_ * CAP + (r + 1) * 128, :], ys.t[:, :], [ys.c], [])

                load_e(0)
                load_e(1)
                front_e(0)
                for ex_ in range(NEXP):
                    if ex_ + 2 < NEXP:
                        load_e(ex_ + 2)
                    if ex_ + 1 < NEXP:
                        front_e(ex_ + 1)
                    back_e(ex_)
            em.barrier()

        if stage_limit >= 5:
            with contextlib.ExitStack() as st:
                y0s = [sbt(st, f"y0s{i}", [128, D], F32) for i in range(4)]
                y1s = [sbt(st, f"y1s{i}", [128, D], F32) for i in range(4)]
                x1s = [sbt(st, f"x1s{i}", [128, D], F32) for i in range(4)]
                accs = [sbt(st, f"acc{i}", [128, D], F32) for i in range(4)]
                junk = sbt(st, "junk5", [128, D], BF16)
                gF = sbt(st, "gF", [128, D], F32)
                ssq = sbt(st, "ssq5", [128, 2], F32)
                lnv = sbt(st, "lnv5", [128, 2], F32)
                rstd = sbt(st, "rstd5", [128, 2], F32)
                DMA("sp", gF.t[:, :], g_fin[0:1, :].to_broadcast([128, D]), [], [gF.c])
                ssqs = [sbt(st, f"ssq5_{i}", [128, 2], F32) for i in range(2)]
                lnvs = [sbt(st, f"lnv5_{i}", [128, 2], F32) for i in range(2)]
                rstds = [sbt(st, f"rstd5_{i}", [128, 2], F32) for i in range(2)]

                def s5_pre(i):
                    tok = slice(i * 128, (i + 1) * 128)
                    y0, y1, x1t, acc = y0s[i % 4], y1s[i % 4], x1s[i % 4], accs[i % 4]
                    for k, yt in ((0, y0), (1, y1)):
                        em.op("pool", lambda e, k=k, i=i, yt=yt: e.indirect_dma_start(
                            out=yt.t[:, :], out_offset=None, in_=yb[:, :],
                            in_offset=bass.IndirectOffsetOnAxis(ap=dst.t[:, 2 * i + k:2 * i + k + 1], axis=0)), [dst.c], [yt.c], kind="d")
                    DMA("sp", x1t.t[:, :], x1d[tok, :], [], [x1t.c])
                    STT(acc.t[:, :], y0.t[:, :], wts.t[:, 2 * i:2 * i + 1], x1t.t[:, :], ALU.mult, ALU.add, [y0.c, wts.c, x1t.c], [acc.c])
                    STT(acc.t[:, :], y1.t[:, :], wts.t[:, 2 * i + 1:2 * i + 2], acc.t[:, :], ALU.mult, ALU.add, [y1.c, wts.c, acc.c], [acc.c])
                    sq_, ln_, rs_ = ssqs[i % 2], lnvs[i % 2], rstds[i % 2]
                    ACT(junk.t[:, :], acc.t[:, :], AF.Square, [acc.c], [junk.c, sq_.c], accum_out=sq_.t[:, 0:1])
                    rstd_from(sq_.t[:, 0:1], 128, 1, 1.0 / D, ln_, rs_, [sq_.c])

                def s5_post(i):
                    tok = slice(i * 128, (i + 1) * 128)
                    acc = accs[i % 4]
                    rs_ = rstds[i % 2]
                    STT(acc.t[:, :], acc.t[:, :], rs_.t[:, 0:1], gF.t[:, :], ALU.mult, ALU.mult, [acc.c, rs_.c, gF.c], [acc.c])
                    DMA("sp", out[tok, :], acc.t[:, :], [acc.c], [])

                s5_pre(0)
                for i in range(T // 128):
                    if i + 1 < T // 128:
                        s5_pre(i + 1)
                    s5_post(i)

        stats = em.emit()
    return nc, stats


_CACHE = {}


def prep_inputs(inputs):
    f = lambda a: np.ascontiguousarray(np.asarray(a, dtype=np.float32))

    def pc(w, nch):
        e, r, n = w.shape
        return np.ascontiguousarray(w.reshape(e, nch, 128, n).transpose(0, 2, 1, 3)).reshape(e, 128, nch * n)
    x = f(inputs["x"])
    positions = np.asarray(inputs["positions"]).astype(np.int32)
    w_in = f(inputs["w_in"][0])
    kr0 = 4096 + 384 + 256
    w_krsw = np.ascontiguousarray(np.concatenate([w_in[:, kr0 + 32:kr0 + 64], w_in[:, kr0:kr0 + 32]], axis=1))
    hl = f(inputs["hg_lower_bound"])
    lbT = np.ascontiguousarray(hl.reshape(2, 8, 128).transpose(2, 0, 1).reshape(128, 16))
    w_uq = f(inputs["mla_w_uq"][0])
    uq3 = w_uq.reshape(384, 8, 192)
    w_uqsw = np.ascontiguousarray(np.concatenate([uq3[:, :, 160:192], uq3[:, :, 128:160]], axis=2).reshape(384, 512))
    shared = {
        "cst": make_consts(),
        "w_in": w_in,
        "w_krsw": w_krsw,
        "g_attn": f(inputs["attn_norm_w"]).reshape(1, D),
        "g_ffn": f(inputs["ffn_norm_w"]).reshape(1, D),
        "g_fin": f(inputs["final_norm_w"]).reshape(1, D),
        "lbT": lbT,
        "g_hg": f(inputs["hg_out_norm_w"]).reshape(128, 1),
        "g_q": np.ascontiguousarray(f(inputs["mla_q_norm_w"]).reshape(3, 128).T),
        "g_kv": np.ascontiguousarray(f(inputs["mla_kv_norm_w"]).reshape(2, 128).T),
        "w_uq": w_uq,
        "w_uqsw": w_uqsw,
        "w_ukv": f(inputs["mla_w_ukv"][0]),
        "w_bh": f(inputs["w_branch_hgrn"][0]),
        "w_bm": f(inputs["w_branch_mla"][0]),
        "w_out": f(inputs["w_out"][0]),
        "w_r": np.ascontiguousarray(np.concatenate([f(inputs["router_group_w"][0]), f(inputs["router_expert_w"][0])], axis=1)),
        "b_r": np.ascontiguousarray(np.concatenate([f(inputs["router_group_b"][0]), f(inputs["router_expert_b"][0])]).reshape(1, 72)),
        "ew1": f(inputs["expert_w1"][0]),
        "ew3": f(inputs["expert_w3"][0]),
        "ew2": f(inputs["expert_w2"][0]),
    }
    in_maps = []
    for c in range(NCORES):
        m = dict(shared)
        m["x"] = np.ascontiguousarray(x[2 * c:2 * c + 2].reshape(T, D))
        m["pos"] = np.ascontiguousarray(positions[2 * c:2 * c + 2].reshape(1, T))
        in_maps.append(m)
    return in_maps


def kernel(**inputs):
    if "nc" not in _CACHE:
        _CACHE["nc"] = build()[0]
    nc = _CACHE["nc"]
    in_maps = prep_inputs(inputs)
    res = run_bass_kernel_spmd(nc, in_maps, core_ids=list(range(NCORES)))
    outs = [np.asarray(r["out"]).reshape(2, SEQ, D) for r in res.results]
    return np.concatenate(outs, axis=0).astype(np.float32)
```
